# Optimizing a Trainium2 kernel written in Bass

```python
import math
import jax, jax.numpy as jnp
from jax import lax
import numpy as np

D_MODEL = 1024
BATCH = 4
SEQ = 8192
DEPTH = 2

HEAD_DIM = 64
A_HEADS = 8
A_BRANCHES = ((128, 1), (512, 4), (2048, 16))
A_BLOCK = 64
B_HEADS = 8
B_KV_HEADS = 2
B_QBLOCK = 128
GRID_W = 64
ROPE_THETA = 10000.0
C_HEADS = 16
C_KV_HEADS = 4
C_RADIUS = 128
C_BLOCK = 128
NUM_BUCKETS = 32
MAX_EXACT = 8
MAX_DISTANCE = 1024
BIAS_HEADS = 16
N_EXPERTS = 16
D_EXPERT = 2048
CAPACITY_FACTOR = 2
EPS = 1e-6
NEG_INF = -1e30
A_W = A_HEADS * HEAD_DIM
B_QW = B_HEADS * HEAD_DIM
B_KVW = B_KV_HEADS * HEAD_DIM
C_QW = C_HEADS * HEAD_DIM
C_KVW = C_KV_HEADS * HEAD_DIM
L0_IN = 3 * A_W + B_QW + 2 * B_KVW
L0_OUT = A_W + B_QW
L1_IN = C_QW + 2 * C_KVW

kernel_name = 'hybrid_dilated_axial_window_ec_encoder'


def rms_norm(x, g):
    xf = x.astype(jnp.float32)
    y = xf * lax.rsqrt(jnp.mean(xf * xf, axis=-1, keepdims=True) + EPS)
    return (y * g.astype(jnp.float32)).astype(x.dtype)


def t5_bucket(rel):
    half = NUM_BUCKETS // 2
    ret = jnp.where(rel > 0, half, 0)
    n = jnp.abs(rel)
    nf = jnp.maximum(n, 1).astype(jnp.float32)
    large = MAX_EXACT + (jnp.log(nf / MAX_EXACT) / math.log(MAX_DISTANCE / MAX_EXACT)
                         * (half - MAX_EXACT)).astype(jnp.int32)
    large = jnp.minimum(large, half - 1)
    return ret + jnp.where(n < MAX_EXACT, n, large)


def local_offsets(block):
    return jnp.arange(3 * block)[None, :] - block - jnp.arange(block)[:, None]


def split_heads(t, n):
    b, s, _ = t.shape
    return t.reshape(b, s, n, HEAD_DIM).transpose(0, 2, 1, 3)


def band_attention(q, k, v, radius, block, bias, sink):
    b, hq, length, hd = q.shape
    hkv = k.shape[1]
    g = hq // hkv
    nb = -(-length // block)
    lp = nb * block
    qp = jnp.pad(q, ((0, 0), (0, 0), (0, lp - length), (0, 0))).reshape(b, hkv, g, nb, block, hd)
    kv_pad = ((0, 0), (0, 0), (block, lp - length + block), (0, 0))

    def windows(t):
        tb = jnp.pad(t, kv_pad).reshape(b, hkv, nb + 2, block, hd)
        return jnp.concatenate([tb[:, :, :-2], tb[:, :, 1:-1], tb[:, :, 2:]], axis=3)

    kw = windows(k)
    vw = windows(v)
    s = jnp.einsum('bhgnqd,bhnkd->bhgnqk', qp, kw).astype(jnp.float32) / math.sqrt(hd)
    s = s + bias.astype(jnp.float32).reshape(hkv, g, 1, block, 3 * block)
    rel = local_offsets(block)
    kpos = jnp.arange(nb)[:, None] * block - block + jnp.arange(3 * block)[None, :]
    valid = (jnp.abs(rel) <= radius)[None] & ((kpos >= 0) & (kpos < length))[:, None, :]
    s = jnp.where(valid, s, NEG_INF)
    m = jnp.max(s, axis=-1)
    if sink is not None:
        sk = sink.astype(jnp.float32).reshape(hkv, g, 1, 1)
        m = jnp.maximum(m, sk)
    p = jnp.exp(s - m[..., None])
    l = jnp.sum(p, axis=-1)
    if sink is not None:
        l = l + jnp.exp(sk - m)
    o = jnp.einsum('bhgnqk,bhnkd->bhgnqd', (p / l[..., None]).astype(v.dtype), vw)
    o = o.reshape(b, hq, lp, hd)[:, :, :length]
    return o, m.reshape(b, hq, lp)[:, :, :length], l.reshape(b, hq, lp)[:, :, :length]


def dilated_attention(q, k, v, rel_bias):
    b, h, s, hd = q.shape
    rel = local_offsets(A_BLOCK)
    outs, ms, ls = [], [], []
    for window, dil in A_BRANCHES:
        radius = window // (2 * dil)
        ls_len = s // dil

        def to_sub(t):
            return t.reshape(b, h, ls_len, dil, hd).transpose(0, 3, 1, 2, 4).reshape(b * dil, h, ls_len, hd)

        bias = rel_bias[:A_HEADS][:, t5_bucket(rel * dil)]
        o, m, l = band_attention(to_sub(q), to_sub(k), to_sub(v), radius, A_BLOCK, bias, None)
        outs.append(o.reshape(b, dil, h, ls_len, hd).transpose(0, 2, 3, 1, 4).reshape(b, h, s, hd))
        ms.append(m.reshape(b, dil, h, ls_len).transpose(0, 2, 3, 1).reshape(b, h, s))
        ls.append(l.reshape(b, dil, h, ls_len).transpose(0, 2, 3, 1).reshape(b, h, s))
    m_all = jnp.stack(ms)
    w = jnp.exp(m_all - jnp.max(m_all, axis=0, keepdims=True)) * jnp.stack(ls)
    o_all = jnp.stack(outs).astype(jnp.float32)
    out = jnp.sum(w[..., None] * o_all, axis=0) / jnp.sum(w, axis=0)[..., None]
    return out.astype(q.dtype)


def axial_rope(x):
    s = x.shape[2]
    rows = s // GRID_W
    row = jnp.broadcast_to(jnp.arange(rows)[:, None], (rows, GRID_W)).reshape(s)
    col = jnp.broadcast_to(jnp.arange(GRID_W)[None, :], (rows, GRID_W)).reshape(s)
    half = HEAD_DIM // 2
    inv = ROPE_THETA ** (-jnp.arange(0, half, 2, dtype=jnp.float32) / half)

    def rot(xh, pos):
        ang = pos.astype(jnp.float32)[:, None] * inv
        cos, sin = jnp.cos(ang), jnp.sin(ang)
        x1, x2 = jnp.split(xh.astype(jnp.float32), 2, axis=-1)
        return jnp.concatenate([x1 * cos - x2 * sin, x1 * sin + x2 * cos], axis=-1)

    return jnp.concatenate([rot(x[..., :half], row), rot(x[..., half:], col)], axis=-1).astype(x.dtype)


def dense_block_attention(q, k, v):
    b, hq, s, hd = q.shape
    hkv = k.shape[1]
    g = hq // hkv
    nqb = s // B_QBLOCK
    qb = q.reshape(b, hkv, g, nqb, B_QBLOCK, hd).transpose(3, 0, 1, 2, 4, 5)

    def one_block(qblk):
        sc = jnp.einsum('bhgqd,bhkd->bhgqk', qblk, k).astype(jnp.float32) / math.sqrt(hd)
        p = jax.nn.softmax(sc, axis=-1)
        return jnp.einsum('bhgqk,bhkd->bhgqd', p.astype(v.dtype), v)

    o = lax.map(one_block, qb)
    return o.transpose(1, 2, 3, 0, 4, 5).reshape(b, hq, s, hd)


def mixer_ab(h, w_in, a_qn, a_kn, b_qn, b_kn, w_out, rel_bias):
    b, s, _ = h.shape
    proj = jnp.einsum('bsd,de->bse', h, w_in)
    cuts = [A_W, 2 * A_W, 3 * A_W, 3 * A_W + B_QW, 3 * A_W + B_QW + B_KVW]
    aq, ak, av, bq, bk, bv = jnp.split(proj, cuts, axis=-1)
    oa = dilated_attention(rms_norm(split_heads(aq, A_HEADS), a_qn),
                           rms_norm(split_heads(ak, A_HEADS), a_kn),
                           split_heads(av, A_HEADS), rel_bias)
    ob = dense_block_attention(axial_rope(rms_norm(split_heads(bq, B_HEADS), b_qn)),
                               axial_rope(rms_norm(split_heads(bk, B_KV_HEADS), b_kn)),
                               split_heads(bv, B_KV_HEADS))
    o = jnp.concatenate([oa, ob], axis=1).transpose(0, 2, 1, 3).reshape(b, s, L0_OUT)
    return jnp.einsum('bse,ed->bsd', o, w_out)


def mixer_c(h, w_in, c_qn, c_kn, sink, w_out, rel_bias):
    b, s, _ = h.shape
    proj = jnp.einsum('bsd,de->bse', h, w_in)
    cq, ck, cv = jnp.split(proj, [C_QW, C_QW + C_KVW], axis=-1)
    bias = rel_bias[:, t5_bucket(local_offsets(C_BLOCK))]
    o, _, _ = band_attention(rms_norm(split_heads(cq, C_HEADS), c_qn),
                             rms_norm(split_heads(ck, C_KV_HEADS), c_kn),
                             split_heads(cv, C_KV_HEADS), C_RADIUS, C_BLOCK, bias, sink)
    o = o.transpose(0, 2, 1, 3).reshape(b, s, C_QW)
    return jnp.einsum('bse,ed->bsd', o, w_out)


def ec_moe(h, w_router, w_gate, w_up, w_down):
    b, s, d = h.shape
    cap = CAPACITY_FACTOR * s // N_EXPERTS
    aff = jax.nn.softmax(jnp.einsum('bsd,de->bse', h, w_router).astype(jnp.float32), axis=-1)
    gates, idx = lax.top_k(jnp.swapaxes(aff, 1, 2), cap)
    xs = jax.vmap(lambda hb, ib: hb[ib])(h, idx)
    act = jax.nn.silu(jnp.einsum('becd,edf->becf', xs, w_gate)) * jnp.einsum('becd,edf->becf', xs, w_up)
    y = jnp.einsum('becf,efd->becd', act, w_down) * gates[..., None].astype(h.dtype)

    def combine(ib, yb):
        return jnp.zeros((s, d), yb.dtype).at[ib.reshape(-1)].add(yb.reshape(-1, d))

    return jax.vmap(combine)(idx, y)


def setup_inputs(seed: int = 0) -> dict:
    key = jax.random.key(seed)
    ks = jax.random.split(key, 25)

    def nrm(k, shape, scale):
        return jax.random.normal(k, shape, jnp.float32) * scale

    def gain(k, n):
        return 1.0 + 0.02 * jax.random.normal(k, (n,), jnp.float32)

    return {
        'x': nrm(ks[0], (BATCH, SEQ, D_MODEL), 1.0),
        'rel_bias': nrm(ks[1], (BIAS_HEADS, NUM_BUCKETS), 0.5),
        'l0_norm_attn': gain(ks[2], D_MODEL),
        'l0_w_in': nrm(ks[3], (D_MODEL, L0_IN), D_MODEL ** -0.5),
        'l0_a_qnorm': gain(ks[4], HEAD_DIM),
        'l0_a_knorm': gain(ks[5], HEAD_DIM),
        'l0_b_qnorm': gain(ks[6], HEAD_DIM),
        'l0_b_knorm': gain(ks[7], HEAD_DIM),
        'l0_w_out': nrm(ks[8], (L0_OUT, D_MODEL), L0_OUT ** -0.5),
        'l0_norm_ffn': gain(ks[9], D_MODEL),
        'l0_router': nrm(ks[10], (D_MODEL, N_EXPERTS), D_MODEL ** -0.5),
        'l0_w_gate': nrm(ks[11], (N_EXPERTS, D_MODEL, D_EXPERT), D_MODEL ** -0.5),
        'l0_w_up': nrm(ks[12], (N_EXPERTS, D_MODEL, D_EXPERT), D_MODEL ** -0.5),
        'l0_w_down': nrm(ks[13], (N_EXPERTS, D_EXPERT, D_MODEL), D_EXPERT ** -0.5),
        'l1_norm_attn': gain(ks[14], D_MODEL),
        'l1_w_in': nrm(ks[15], (D_MODEL, L1_IN), D_MODEL ** -0.5),
        'l1_c_qnorm': gain(ks[16], HEAD_DIM),
        'l1_c_knorm': gain(ks[17], HEAD_DIM),
        'l1_sink': nrm(ks[18], (C_HEADS,), 1.0),
        'l1_w_out': nrm(ks[19], (C_QW, D_MODEL), C_QW ** -0.5),
        'l1_norm_ffn': gain(ks[20], D_MODEL),
        'l1_router': nrm(ks[21], (D_MODEL, N_EXPERTS), D_MODEL ** -0.5),
        'l1_w_gate': nrm(ks[22], (N_EXPERTS, D_MODEL, D_EXPERT), D_MODEL ** -0.5),
        'l1_w_up': nrm(ks[23], (N_EXPERTS, D_MODEL, D_EXPERT), D_MODEL ** -0.5),
        'l1_w_down': nrm(ks[24], (N_EXPERTS, D_EXPERT, D_MODEL), D_EXPERT ** -0.5),
    }


def reference(x, rel_bias,
              l0_norm_attn, l0_w_in, l0_a_qnorm, l0_a_knorm, l0_b_qnorm, l0_b_knorm, l0_w_out,
              l0_norm_ffn, l0_router, l0_w_gate, l0_w_up, l0_w_down,
              l1_norm_attn, l1_w_in, l1_c_qnorm, l1_c_knorm, l1_sink, l1_w_out,
              l1_norm_ffn, l1_router, l1_w_gate, l1_w_up, l1_w_down):
    attn_norms = [l0_norm_attn, l1_norm_attn]
    mixers = [
        lambda h: mixer_ab(h, l0_w_in, l0_a_qnorm, l0_a_knorm, l0_b_qnorm, l0_b_knorm, l0_w_out, rel_bias),
        lambda h: mixer_c(h, l1_w_in, l1_c_qnorm, l1_c_knorm, l1_sink, l1_w_out, rel_bias),
    ]
    ffns = [
        (l0_norm_ffn, l0_router, l0_w_gate, l0_w_up, l0_w_down),
        (l1_norm_ffn, l1_router, l1_w_gate, l1_w_up, l1_w_down),
    ]
    for i in range(DEPTH):
        x = x + mixers[i](rms_norm(x, attn_norms[i]))
        g, wr, wg, wu, wd = ffns[i]
        x = x + ec_moe(rms_norm(x, g), wr, wg, wu, wd)
    return x
```

```python
import numpy as np
import concourse.bass as bass
import concourse.mybir as mybir
from concourse.bass_utils import run_bass_kernel_spmd

F32 = mybir.dt.float32
BF16 = mybir.dt.bfloat16
I32 = mybir.dt.int32
U32 = mybir.dt.uint32
ALU = mybir.AluOpType
AF = mybir.ActivationFunctionType
AX = mybir.AxisListType

ENGS = ("pe", "act", "dve", "pool", "sp")


class Buf:
    __slots__ = ("name", "writers", "readers")

    def __init__(self, name=""):
        self.name = name
        self.writers = []
        self.readers = []


class Op:
    __slots__ = ("eng", "fn", "deps", "is_dma", "marked", "cnt", "sem", "semval", "seq")

    def __init__(self, eng, fn, is_dma):
        self.eng = eng
        self.fn = fn
        self.deps = set()
        self.is_dma = is_dma
        self.marked = False
        self.cnt = 0
        self.sem = None
        self.semval = 0


def _prune(lst, op):
    if not op.is_dma:
        lst[:] = [o for o in lst if o.is_dma or o.eng != op.eng]
    lst.append(op)


class Sched:
    def __init__(self, nc, n_dma_sems=None):
        self.nc = nc
        self.ops = {e: [] for e in ENGS}
        self.dma_ops = {e: [] for e in ENGS}
        self.n_dma_sems = n_dma_sems or {"sp": 24, "act": 8, "pool": 16}
        self.seq = 0
        self.since_barrier = []
        self.regcache = {}

    def getreg(self, eng, val):
        if val not in self.regcache:
            self.regcache[val] = eng.to_reg(val)
        return self.regcache[val]

    def add(self, eng, fn, reads=(), writes=(), pwrites=(), dma=False, extra_deps=()):
        op = Op(eng, fn, dma)
        op.seq = self.seq
        self.seq += 1
        deps = set(extra_deps)
        raw = set()
        for b in reads:
            for w in b.writers:
                deps.add(w)
                raw.add(w)
        for b in writes:
            deps.update(b.writers)
            deps.update(b.readers)
        for b in pwrites:
            deps.update(b.readers)
            for w in b.writers:
                if w.is_dma or w.eng != eng:
                    deps.add(w)
        for d in deps:
            if d is op:
                continue
            if d.is_dma or dma:
                op.deps.add(d)
            elif d.eng != eng:
                op.deps.add(d)
            elif d in raw and eng != "pe":
                op.deps.add(d)
        for b in reads:
            _prune(b.readers, op)
        for b in writes:
            b.writers = [op]
            b.readers = []
        for b in pwrites:
            _prune(b.writers, op)
        if dma:
            q = self.dma_ops[eng]
            K = self.n_dma_sems[eng]
            if len(q) >= K:
                op.deps.add(q[len(q) - K])
            op.cnt = len(q)
            q.append(op)
        self.ops[eng].append(op)
        self.since_barrier.append(op)
        return op

    def barrier(self):
        last = {}
        dl = {}
        for o in self.since_barrier:
            if o.is_dma:
                dl[(o.eng, o.cnt % self.n_dma_sems[o.eng])] = o
            else:
                last[o.eng] = o
        deps = list(last.values()) + list(dl.values())
        self.since_barrier = []
        new = []
        for e in ENGS:
            op = self.add(e, lambda eng: eng.nop(), extra_deps=[d for d in deps])
            new.append(op)
        return new

    def emit(self):
        nc = self.nc
        for e in ENGS:
            for op in self.ops[e]:
                for d in op.deps:
                    d.marked = True
        eng_sem = {e: nc.alloc_semaphore(f"s_{e}") for e in ENGS}
        dma_sems = {e: [nc.alloc_semaphore(f"d_{e}{i}") for i in range(self.n_dma_sems.get(e, 0))]
                    for e in ENGS}
        for e in ENGS:
            c = 0
            for op in self.ops[e]:
                if op.is_dma:
                    continue
                if op.marked:
                    c += 1
                    op.cnt = c
            K = self.n_dma_sems.get(e, 0)
            for i, op in enumerate(self.dma_ops[e]):
                op.sem = dma_sems[e][i % K]
                op.semval = 16 * (i // K + 1)
        self.max_cnt = {e: max([o.cnt for o in self.ops[e]] + [0]) for e in ENGS}

        def run(e, eng):
            seen = {}
            for op in self.ops[e]:
                for d in sorted(op.deps, key=lambda o: o.seq):
                    if d.is_dma:
                        key, val, sem = ("d", id(d.sem)), d.semval, d.sem
                    else:
                        key, val, sem = ("e", d.eng), d.cnt, eng_sem[d.eng]
                    if seen.get(key, 0) < val:
                        eng.wait_ge(sem, val)
                        seen[key] = val
                ins = op.fn(eng)
                if op.is_dma:
                    ins.then_inc(op.sem, 16)
                elif op.marked:
                    ins.then_inc(eng_sem[e], 1)

        with nc.Block() as block:
            @block.tensor
            def _(eng):
                run("pe", eng)

            @block.scalar
            def _(eng):
                run("act", eng)

            @block.vector
            def _(eng):
                run("dve", eng)

            @block.gpsimd
            def _(eng):
                run("pool", eng)

            @block.sync
            def _(eng):
                run("sp", eng)


class Arena:
    def __init__(self, nc, name, nbytes):
        self.t = nc.alloc_sbuf_tensor(name, [128, nbytes // 4], F32)
        self.nbytes = nbytes
        self.off = 0
        self.marks = []

    def alloc(self, shape, dtype, parts=128):
        esz = {F32: 4, BF16: 2, I32: 4, U32: 4}[dtype]
        n = int(np.prod(shape))
        nb = (n * esz + 31) // 32 * 32
        assert self.off + nb <= self.nbytes, f"arena overflow {self.off}+{nb}>{self.nbytes}"
        a = self.t[0:parts, self.off // 4:(self.off + nb) // 4]
        self.off += nb
        if dtype != F32:
            a = a.bitcast(dtype)
        a = a[:, 0:n]
        if len(shape) > 1:
            names = " ".join(f"d{i}" for i in range(len(shape)))
            kw = {f"d{i}": s for i, s in enumerate(shape)}
            a = a.rearrange(f"p ({names}) -> p {names}", **kw)
        return a

    def mark(self):
        self.marks.append(self.off)

    def release(self):
        self.off = self.marks.pop()


NTOK = 8192
D = 1024
NE = 8
CAP = 1024
DF = 2048
ROWW = 524
EPS = 1e-6


class Ctx:
    def __init__(self):
        self.nc = bass.Bass("TRN2", target_bir_lowering=False)
        self.S = Sched(self.nc)
        self.A = Arena(self.nc, "arena", 204 * 1024)
        self.psall = self.nc.alloc_psum_tensor("psall", [128, 4096], F32)
        self.dram = {}

    def scratch(self, name, shape, dt):
        key = (name, tuple(shape), str(dt))
        if key not in self.dram:
            self.dram[key] = self.nc.dram_tensor(name, list(shape), dt).ap()
        return self.dram[key]

    def reset(self):
        self.A.off = 0
        self.A.marks = []


def DMA(out, in_, **kw):
    return lambda e: e.dma_start(out=out, in_=in_, **kw)


def make_ident(S, A):
    identf = A.alloc([128], F32)
    identb = A.alloc([128], BF16)
    bf, bb = Buf("identf"), Buf("identb")
    S.add("pool", lambda e: e.memset(identf, 0.0), writes=[bf])
    S.add("pool", lambda e: e.affine_select(out=identf, in_=identf, pattern=[[-1, 128]],
                                            compare_op=ALU.not_equal, fill=1.0, base=0,
                                            channel_multiplier=1), reads=[bf], writes=[bf])
    S.add("dve", lambda e: e.tensor_copy(out=identb, in_=identf), reads=[bf], writes=[bb])
    return identf, identb, bf, bb


def build_moe(ctx=None, io=None, zero_init=True, wr_sets=None):
    if ctx is None:
        nc = bass.Bass("TRN2", target_bir_lowering=False)
        x = nc.dram_tensor("x", [NTOK, D], F32, kind="ExternalInput").ap()
        gn = nc.dram_tensor("gn", [1, D], F32, kind="ExternalInput").ap()
        wr = nc.dram_tensor("wr", [D, 16], F32, kind="ExternalInput").ap()
        wg = nc.dram_tensor("wg", [NE, D, DF], F32, kind="ExternalInput").ap()
        wu = nc.dram_tensor("wu", [NE, D, DF], F32, kind="ExternalInput").ap()
        wd = nc.dram_tensor("wd", [NE, DF, D], F32, kind="ExternalInput").ap()
        cG = nc.dram_tensor("cG", [128, 128], F32, kind="ExternalInput").ap()
        cL = nc.dram_tensor("cL", [128, 128], F32, kind="ExternalInput").ap()
        part = nc.dram_tensor("part", [NTOK, D], F32, kind="ExternalOutput").ap()
        hbuf = nc.dram_tensor("hbuf", [NTOK, ROWW], F32).ap()
        affd = nc.dram_tensor("affd", [16, NTOK], F32).ap()
        xs = [nc.dram_tensor(f"xs{i}", [CAP, ROWW], F32).ap() for i in range(NE)]
        S = Sched(nc)
        A = Arena(nc, "arena", 204 * 1024)
        ps = [nc.alloc_psum_tensor(f"ps{i}", [128, 512], F32) for i in range(8)]
        part_full, part_eoff = part, 0
    else:
        nc, S, A = ctx.nc, ctx.S, ctx.A
        ctx.reset()
        x, gn, wr, wg, wu, wd, cG, cL, part = (io[k] for k in ("x", "gn", "wr", "wg", "wu", "wd", "cG", "cL", "part"))
        part_full, part_eoff = io.get("part_full", part), io.get("part_eoff", 0)
        hbuf = ctx.scratch("hbuf", [NTOK, ROWW], F32)
        affd = ctx.scratch("affd", [16, NTOK], F32)
        xs = [ctx.scratch(f"xs{i}", [CAP, ROWW], F32) for i in range(NE)]
        ps = [ctx.psall[:, i * 512:(i + 1) * 512] for i in range(8)]
    PB = [Buf(f"ps{i}") for i in range(8)]

    identf, identb, Bif, Bib = make_ident(S, A)
    G_sb = A.alloc([128], F32)
    L_sb = A.alloc([128], F32)
    gb = A.alloc([D], F32)
    gcol = A.alloc([8], F32)
    wr_sb = A.alloc([8, 16], F32)
    zero = A.alloc([D], F32)
    tokid = A.alloc([64], I32)
    Bc = Buf("consts")
    S.add("sp", DMA(G_sb, cG), pwrites=[Bc], dma=True)
    S.add("sp", DMA(L_sb, cL), pwrites=[Bc], dma=True)
    S.add("sp", DMA(gb, gn.partition_broadcast(128)), pwrites=[Bc], dma=True)
    S.add("sp", DMA(gcol, gn.rearrange("o (c p) -> p (o c)", p=128), allow_slow_non_contiguous=True),
          pwrites=[Bc], dma=True)
    wrv = wr.rearrange("(c p) e -> p c e", p=128)
    if wr_sets is None:
        S.add("sp", DMA(wr_sb, wrv), pwrites=[Bc], dma=True)
    else:
        own, oth = wr_sets
        S.add("sp", DMA(wr_sb[:, :, 0:8], wrv[:, :, own:own + 8]), pwrites=[Bc], dma=True)
        S.add("sp", DMA(wr_sb[:, :, 8:16], wrv[:, :, oth:oth + 8]), pwrites=[Bc], dma=True)
    S.add("dve", lambda e: e.memset(zero, 0.0), pwrites=[Bc])
    S.add("pool", lambda e: e.iota(tokid, pattern=[[128, 64]], base=0, channel_multiplier=1), pwrites=[Bc])
    Bpart = Buf("part")
    if zero_init:
        for T in range(NTOK // 128):
            S.add("sp", DMA(part[T * 128:(T + 1) * 128, :], zero), reads=[Bc], pwrites=[Bpart], dma=True)

    posT = A.alloc([4, 128], I32)
    A.mark()
    affT_sb = A.alloc([NTOK], F32)
    BaffT = Buf("affT")
    xts = [A.alloc([D], F32) for _ in range(3)]
    Bxt = [Buf() for _ in range(3)]
    xns = [A.alloc([D], F32) for _ in range(2)]
    Bxn = [Buf() for _ in range(2)]
    hTs = [A.alloc([8, 128], F32) for _ in range(2)]
    BhT = [Buf() for _ in range(2)]
    rts = [A.alloc([ROWW], F32) for _ in range(3)]
    Brt = [Buf() for _ in range(3)]
    sts = [A.alloc([8], F32) for _ in range(4)]
    Bst = [Buf() for _ in range(4)]
    exs = [A.alloc([16], F32) for _ in range(2)]
    Bex = [Buf() for _ in range(2)]
    affs = [A.alloc([16], F32) for _ in range(2)]
    Baf = [Buf() for _ in range(2)]
    junk = A.alloc([D], BF16)
    Bjunk = Buf()
    for T in range(NTOK // 128):
        xt, bxt = xts[T % 3], Bxt[T % 3]
        xn, bxn = xns[T % 2], Bxn[T % 2]
        hT, bhT = hTs[T % 2], BhT[T % 2]
        rt, brt = rts[T % 3], Brt[T % 3]
        st, bst = sts[T % 4], Bst[T % 4]
        ex, bex = exs[T % 2], Bex[T % 2]
        af, baf = affs[T % 2], Baf[T % 2]
        rt_bf = rt.bitcast(BF16)
        rt_i = rt.bitcast(I32)
        S.add("sp", DMA(xt, x[T * 128:(T + 1) * 128, :]), writes=[bxt], dma=True)
        S.add("act", lambda e, xt=xt, st=st: e.activation(out=junk, in_=xt, func=AF.Square, scale=1.0 / 32,
                                                          accum_out=st[:, 0:1]),
              reads=[bxt], writes=[Bjunk, bst])
        S.add("act", lambda e, st=st: e.activation(out=st[:, 1:2], in_=st[:, 0:1], func=AF.Sqrt, bias=EPS, scale=1.0),
              reads=[bst], pwrites=[bst])
        S.add("dve", lambda e, st=st: e.reciprocal(out=st[:, 2:3], in_=st[:, 1:2]), reads=[bst], pwrites=[bst])
        S.add("dve", lambda e, xn=xn, xt=xt, st=st: e.tensor_scalar_mul(out=xn, in0=xt, scalar1=st[:, 2:3]),
              reads=[bxt, bst], writes=[bxn])
        b0 = 2 * (T % 2)
        for c in range(8):
            pb = b0 + c // 4
            S.add("pe", lambda e, pb=pb, c=c, xn=xn: e.transpose(out=ps[pb][:, (c % 4) * 128:(c % 4 + 1) * 128],
                                                                 in_=xn[:, c * 128:(c + 1) * 128], identity=identf),
                  reads=[bxn, Bif], writes=[PB[pb]] if c % 4 == 0 else [], pwrites=[] if c % 4 == 0 else [PB[pb]])
        for c in range(8):
            pb = b0 + c // 4
            src = ps[pb][:, (c % 4) * 128:(c % 4 + 1) * 128]
            if c % 2 == 0:
                S.add("act", lambda e, src=src, c=c, hT=hT: e.activation(out=hT[:, c, :], in_=src, func=AF.Copy,
                                                                         scale=gcol[:, c:c + 1]),
                      reads=[PB[pb], Bc], writes=[bhT] if c == 0 else [], pwrites=[] if c == 0 else [bhT])
            else:
                S.add("dve", lambda e, src=src, c=c, hT=hT: e.tensor_scalar_mul(out=hT[:, c, :], in0=src,
                                                                                scalar1=gcol[:, c:c + 1]),
                      reads=[PB[pb], Bc], pwrites=[bhT])
        pl = 4 + T % 2
        for c in range(8):
            S.add("pe", lambda e, pl=pl, c=c, hT=hT: e.matmul(ps[pl][:, 0:16], lhsT=hT[:, c, :], rhs=wr_sb[:, c, :],
                                                              start=(c == 0), stop=(c == 7)),
                  reads=[bhT, Bc], writes=[PB[pl]] if c == 0 else [], pwrites=[] if c == 0 else [PB[pl]])
        S.add("dve", lambda e, pl=pl, st=st: e.reduce_max(out=st[:, 3:4], in_=ps[pl][:, 0:16], axis=AX.X),
              reads=[PB[pl]], pwrites=[bst])
        S.add("dve", lambda e, st=st: e.tensor_scalar_mul(out=st[:, 4:5], in0=st[:, 3:4], scalar1=-1.0),
              reads=[bst], pwrites=[bst])
        S.add("act", lambda e, pl=pl, st=st, ex=ex: e.activation(out=ex, in_=ps[pl][:, 0:16], func=AF.Exp,
                                                                 bias=st[:, 4:5], scale=1.0, accum_out=st[:, 5:6]),
              reads=[PB[pl], bst], writes=[bex], pwrites=[bst])
        S.add("dve", lambda e, st=st: e.reciprocal(out=st[:, 6:7], in_=st[:, 5:6]), reads=[bst], pwrites=[bst])
        S.add("dve", lambda e, af=af, ex=ex, st=st: e.tensor_scalar_mul(out=af, in0=ex, scalar1=st[:, 6:7]),
              reads=[bex, bst], writes=[baf])
        S.add("dve", lambda e, rt_bf=rt_bf, xn=xn: e.tensor_tensor(out=rt_bf[:, 0:D], in0=xn, in1=gb, op=ALU.mult),
              reads=[bxn, Bc], writes=[brt])
        S.add("act", lambda e, rt=rt, af=af: e.copy(out=rt[:, 512:520], in_=af[:, 0:8]), reads=[baf], pwrites=[brt])
        S.add("dve", lambda e, rt_i=rt_i, T=T: e.tensor_copy(out=rt_i[:, 520:521], in_=tokid[:, T:T + 1]),
              reads=[Bc], pwrites=[brt])
        S.add("act", DMA(hbuf[T * 128:(T + 1) * 128, 0:521], rt[:, 0:521]), reads=[brt], dma=True)
        pa = 6 + T % 2
        S.add("pe", lambda e, pa=pa, af=af: e.transpose(out=ps[pa][0:16, 0:128], in_=af, identity=identf),
              reads=[baf, Bif], writes=[PB[pa]])
        S.add("act", lambda e, pa=pa, T=T: e.copy(out=affT_sb[0:16, T * 128:(T + 1) * 128], in_=ps[pa][0:16, 0:128]),
              reads=[PB[pa]], pwrites=[BaffT])

    Baffd = Buf("affd")
    S.add("sp", DMA(affd, affT_sb[0:16, :]), reads=[BaffT], writes=[Baffd], dma=True)
    a_sb = A.alloc([512], F32)
    Ba = Buf("a")
    S.add("sp", DMA(a_sb, affd[0:8, :].rearrange("e (c j) -> (e c) j", c=16)), reads=[Baffd], writes=[Ba], dma=True)
    msk = A.alloc([512], F32)
    Bm = Buf("msk")
    sc = A.alloc([8], F32)
    Bs = Buf("sc")
    S.add("dve", lambda e: e.memset(sc, 0.0), writes=[Bs])
    pt = 0
    for k in range(30):
        dl = 2.0 ** -(k + 1)
        S.add("dve", lambda e, dl=dl: e.tensor_scalar_add(out=sc[:, 1:2], in0=sc[:, 0:1], scalar1=dl),
              reads=[Bs], pwrites=[Bs])
        S.add("dve", lambda e: e.tensor_single_scalar(out=msk, in_=a_sb, scalar=sc[:, 1:2], op=ALU.is_ge),
              reads=[Ba, Bs], writes=[Bm])
        S.add("dve", lambda e: e.reduce_sum(out=sc[:, 2:3], in_=msk, axis=AX.X), reads=[Bm], pwrites=[Bs])
        S.add("pe", lambda e: e.matmul(ps[pt][:, 0:1], lhsT=G_sb, rhs=sc[:, 2:3], start=True, stop=True),
              reads=[Bs, Bc], writes=[PB[pt]])
        S.add("dve", lambda e: e.tensor_single_scalar(out=sc[:, 3:4], in_=ps[pt][:, 0:1], scalar=CAP - 0.5, op=ALU.is_ge),
              reads=[PB[pt]], pwrites=[Bs])
        S.add("dve", lambda e, dl=dl: e.scalar_tensor_tensor(out=sc[:, 0:1], in0=sc[:, 3:4], scalar=dl, in1=sc[:, 0:1],
                                                            op0=ALU.mult, op1=ALU.add),
              reads=[Bs], pwrites=[Bs])

    ones = A.alloc([512], F32)
    incl = A.alloc([512], F32)
    posm = A.alloc([512], F32)
    Bo, Bi, Bp, BpT = Buf(), Buf(), Buf(), Buf()
    S.add("dve", lambda e: e.memset(ones, 1.0), writes=[Bo])
    S.add("dve", lambda e: e.tensor_single_scalar(out=msk, in_=a_sb, scalar=sc[:, 0:1], op=ALU.is_ge),
          reads=[Ba, Bs], writes=[Bm])
    S.add("dve", lambda e: e.reduce_sum(out=sc[:, 2:3], in_=msk, axis=AX.X), reads=[Bm], pwrites=[Bs])
    S.add("pe", lambda e: e.matmul(ps[pt][:, 0:1], lhsT=L_sb, rhs=sc[:, 2:3], start=True, stop=True),
          reads=[Bs, Bc], writes=[PB[pt]])
    S.add("dve", lambda e: e.tensor_copy(out=sc[:, 4:5], in_=ps[pt][:, 0:1]), reads=[PB[pt]], pwrites=[Bs])
    S.add("dve", lambda e: e.tensor_tensor_scan(out=incl, data0=ones, data1=msk, initial=0.0, op0=ALU.mult, op1=ALU.add),
          reads=[Bo, Bm], writes=[Bi])
    S.add("dve", lambda e: e.tensor_tensor(out=incl, in0=incl, in1=msk, op=ALU.subtract), reads=[Bi, Bm], writes=[Bi])
    S.add("dve", lambda e: e.tensor_scalar(out=posm, in0=incl, scalar1=sc[:, 4:5], scalar2=-4096.0,
                                           op0=ALU.add, op1=ALU.add), reads=[Bi, Bs], writes=[Bp])
    S.add("dve", lambda e: e.tensor_tensor(out=posm, in0=posm, in1=msk, op=ALU.mult), reads=[Bp, Bm], writes=[Bp])
    S.add("dve", lambda e: e.tensor_scalar_add(out=posm, in0=posm, scalar1=4096.0), reads=[Bp], writes=[Bp])
    pq = 1
    for jb in range(4):
        S.add("pe", lambda e, jb=jb: e.transpose(out=ps[pq][:, jb * 128:(jb + 1) * 128],
                                                 in_=posm[:, jb * 128:(jb + 1) * 128], identity=identf),
              reads=[Bp, Bif], writes=[PB[pq]] if jb == 0 else [], pwrites=[] if jb == 0 else [PB[pq]])
    S.add("dve", lambda e: e.tensor_copy(out=posT, in_=ps[pq][:, 0:512].rearrange("p (a b) -> p a b", a=4)),
          reads=[PB[pq]], writes=[BpT])

    S.barrier()
    A.release()
    NRT = 6
    rtl = [A.alloc([ROWW], F32) for _ in range(NRT)]
    Brl = [Buf() for _ in range(NRT)]
    Bxs = [Buf(f"xs{e}") for e in range(NE)]
    NSTG = 4
    stg = [A.alloc([2048], F32) for _ in range(NSTG)]
    Bstg = [Buf() for _ in range(NSTG)]
    wgb = [A.alloc([8, 256], BF16) for _ in range(2)]
    wub = [A.alloc([8, 256], BF16) for _ in range(2)]
    Bwg = [Buf() for _ in range(2)]
    Bwu = [Buf() for _ in range(2)]
    wdb = A.alloc([16, D], BF16)
    Bwd = [Buf() for _ in range(8)]
    xsb = A.alloc([8, ROWW], F32)
    Bxsb = Buf("xsb")
    XT = A.alloc([8, CAP], BF16)
    BXT = Buf("XT")
    AT = A.alloc([16, CAP], BF16)
    BAT = [Buf() for _ in range(16)]
    sgs = [A.alloc([512], F32) for _ in range(2)]
    Bsg = [Buf() for _ in range(2)]
    yts = [A.alloc([D], F32) for _ in range(3)]
    Byt = [Buf() for _ in range(3)]
    pgs = [A.alloc([D], F32) for _ in range(3)]
    Bpg = [Buf() for _ in range(3)]
    pg_i = [0]
    stg_i = [0]
    rt_i_ = [0]
    cast_i = [0]
    sg_i = [0]
    yt_i = [0]
    prev_sc = []

    def cast(out, in_, reads, writes):
        eng = "dve" if cast_i[0] % 3 != 2 else "act"
        cast_i[0] += 1
        if eng == "dve":
            S.add("dve", lambda e: e.tensor_copy(out=out, in_=in_), reads=reads, writes=writes)
        else:
            S.add("act", lambda e: e.copy(out=out, in_=in_), reads=reads, writes=writes)

    def scatter_rows(e_):
        for T in range(NTOK // 128):
            c, jb = T // 4, T % 4
            k = rt_i_[0] % NRT
            rt_i_[0] += 1
            S.add("act", DMA(rtl[k][:, 0:521], hbuf[T * 128:(T + 1) * 128, 0:521]), writes=[Brl[k]], dma=True)
            idx = posT[:, jb, e_ * 16 + c:e_ * 16 + c + 1]
            S.add("pool", lambda e, k=k, idx=idx, e_=e_: e.indirect_dma_start(
                out=xs[e_], out_offset=bass.IndirectOffsetOnAxis(ap=idx, axis=0), in_=rtl[k], in_offset=None,
                bounds_check=S.getreg(e, CAP - 1), oob_is_err=False), reads=[Brl[k], BpT], pwrites=[Bxs[e_]], dma=True)

    scatter_rows(0)
    for e_ in range(NE):
        if e_ + 1 < NE:
            scatter_rows(e_ + 1)
        S.add("sp", DMA(xsb, xs[e_].rearrange("(t p) c -> p t c", p=128)), reads=[Bxs[e_]], writes=[Bxsb], dma=True)
        xsb_bf = xsb.rearrange("p t c -> p (t c)").bitcast(BF16).rearrange("p (t c) -> p t c", t=8)
        xsb_i = xsb.rearrange("p t c -> p (t c)").bitcast(I32).rearrange("p (t c) -> p t c", t=8)
        for dc in range(8):
            for half in range(2):
                pb = 6 + half
                psb = ps[pb].bitcast(BF16)
                for t4 in range(4):
                    t = half * 4 + t4
                    S.add("pe", lambda e, psb=psb, t4=t4, t=t, dc=dc: e.transpose(
                        out=psb[:, t4 * 128:(t4 + 1) * 128], in_=xsb_bf[:, t, dc * 128:(dc + 1) * 128], identity=identb),
                        reads=[Bxsb, Bib], writes=[PB[pb]] if t4 == 0 else [], pwrites=[] if t4 == 0 else [PB[pb]])
                eng = "act" if (dc + half) % 2 == 0 else "dve"
                dst = XT[:, dc, half * 512:(half + 1) * 512]
                if eng == "act":
                    S.add("act", lambda e, psb=psb, dst=dst: e.copy(out=dst, in_=psb[:, 0:512]),
                          reads=[PB[pb]], writes=[BXT] if (dc == 0 and half == 0) else [],
                          pwrites=[] if (dc == 0 and half == 0) else [BXT])
                else:
                    S.add("dve", lambda e, psb=psb, dst=dst: e.tensor_copy(out=dst, in_=psb[:, 0:512]),
                          reads=[PB[pb]], pwrites=[BXT])
        for fq in range(8):
            wsel = fq % 2
            for (wsrc, wdst, bw) in ((wg, wgb[wsel], Bwg[wsel]), (wu, wub[wsel], Bwu[wsel])):
                k = stg_i[0] % NSTG
                stg_i[0] += 1
                sv = stg[k].rearrange("p (c f) -> p c f", c=8)
                S.add("sp", DMA(sv, wsrc[e_].rearrange("(c p) f -> p c f", p=128)[:, :, fq * 256:(fq + 1) * 256]),
                      writes=[Bstg[k]], dma=True)
                cast(wdst, sv, [Bstg[k]], [bw])
            for fl in range(2):
                fc = fq * 2 + fl
                bset = 0 if fc % 2 == 0 else 3
                gb_ = [bset, bset + 1]
                ub_ = [bset + 2, (bset + 3) if bset == 0 else 0]
                if bset == 3:
                    gb_ = [3, 4]
                    ub_ = [5, 2]
                else:
                    gb_ = [0, 1]
                    ub_ = [2, 5]
                for half in range(2):
                    for (wsb, bw, bank) in ((wgb[wsel], Bwg[wsel], gb_[half]), (wub[wsel], Bwu[wsel], ub_[half])):
                        for dc in range(8):
                            S.add("pe", lambda e, wsb=wsb, bank=bank, dc=dc, fl=fl, half=half: e.matmul(
                                ps[bank][:, 0:512], lhsT=wsb[:, dc, fl * 128:(fl + 1) * 128],
                                rhs=XT[:, dc, half * 512:(half + 1) * 512], start=(dc == 0), stop=(dc == 7)),
                                reads=[bw, BXT], writes=[PB[bank]] if dc == 0 else [],
                                pwrites=[] if dc == 0 else [PB[bank]])
                    k = sg_i[0] % 2
                    sg_i[0] += 1
                    S.add("act", lambda e, k=k, bank=gb_[half]: e.activation(out=sgs[k], in_=ps[bank][:, 0:512], func=AF.Silu),
                          reads=[PB[gb_[half]]], writes=[Bsg[k]])
                    S.add("dve", lambda e, k=k, bank=ub_[half], fc=fc, half=half: e.tensor_tensor(
                        out=AT[:, fc, half * 512:(half + 1) * 512], in0=ps[bank][:, 0:512], in1=sgs[k], op=ALU.mult),
                        reads=[PB[ub_[half]], Bsg[k]], writes=[BAT[fc]] if half == 0 else [],
                        pwrites=[] if half == 0 else [BAT[fc]])
        for q in range(8):
            k = stg_i[0] % NSTG
            stg_i[0] += 1
            sv = stg[k].rearrange("p (c f) -> p c f", c=2)
            S.add("sp", DMA(sv, wd[e_].rearrange("(c p) f -> p c f", p=128)[:, q * 2:(q + 1) * 2, :]),
                  writes=[Bstg[k]], dma=True)
            cast(wdb[:, q * 2:(q + 1) * 2, :], sv, [Bstg[k]], [Bwd[q]])
        cur_sc = []
        for t in range(8):
            k = yt_i[0] % 3
            yt_i[0] += 1
            for half in range(2):
                bank = (t * 2 + half) % 6
                for fc in range(16):
                    S.add("pe", lambda e, bank=bank, fc=fc, t=t, half=half: e.matmul(
                        ps[bank][:, 0:512], lhsT=AT[:, fc, t * 128:(t + 1) * 128],
                        rhs=wdb[:, fc, half * 512:(half + 1) * 512], start=(fc == 0), stop=(fc == 15)),
                        reads=[BAT[fc], Bwd[fc // 2]], writes=[PB[bank]] if fc == 0 else [],
                        pwrites=[] if fc == 0 else [PB[bank]])
                gsc = xsb[:, t, 512 + e_:513 + e_]
                if half == 0:
                    S.add("act", lambda e, k=k, bank=bank, gsc=gsc: e.activation(
                        out=yts[k][:, 0:512], in_=ps[bank][:, 0:512], func=AF.Copy, scale=gsc),
                        reads=[PB[bank], Bxsb], writes=[Byt[k]])
                else:
                    S.add("dve", lambda e, k=k, bank=bank, gsc=gsc: e.tensor_scalar_mul(
                        out=yts[k][:, 512:1024], in0=ps[bank][:, 0:512], scalar1=gsc),
                        reads=[PB[bank], Bxsb], pwrites=[Byt[k]])
            idx = xsb_i[:, t, 520:521]
            kg_ = pg_i[0] % 3
            pg_i[0] += 1
            S.add("pool", lambda e, kg_=kg_, idx=idx: e.indirect_dma_start(
                out=pgs[kg_], out_offset=None, in_=part_full,
                in_offset=bass.IndirectOffsetOnAxis(ap=idx, axis=0), element_offset=part_eoff),
                reads=[Bxsb, Bpart], writes=[Bpg[kg_]], dma=True, extra_deps=prev_sc)
            S.add("dve", lambda e, k=k, kg_=kg_: e.tensor_tensor(out=yts[k], in0=yts[k], in1=pgs[kg_], op=ALU.add),
                  reads=[Byt[k], Bpg[kg_]], writes=[Byt[k]])
            op = S.add("pool", lambda e, k=k, idx=idx: e.indirect_dma_start(
                out=part_full, out_offset=bass.IndirectOffsetOnAxis(ap=idx, axis=0), in_=yts[k], in_offset=None,
                element_offset=part_eoff),
                reads=[Byt[k], Bxsb], pwrites=[Bpart], dma=True)
            cur_sc.append(op)
        prev_sc = cur_sc
    S.barrier()
    if ctx is not None:
        return None
    S.emit()
    return nc


import math


def t5_bucket_np(rel):
    rel = np.asarray(rel, np.int64)
    ret = np.where(rel > 0, 16, 0)
    n = np.abs(rel)
    nf = np.maximum(n, 1).astype(np.float32)
    large = 8 + (np.log(nf / np.float32(8)) / np.float32(math.log(128.0)) * np.float32(8)).astype(np.int32)
    large = np.minimum(large, 15)
    return ret + np.where(n < 8, n, large)


def band_consts(cfg):
    ohb = []
    meta = []
    for (d, radius, wins) in cfg["branches"]:
        for (off, bases) in wins:
            for base in bases:
                rho = np.arange(128)
                rel = rho + off - base + 128
                valid = np.abs(rel) <= radius
                bk = t5_bucket_np(rel * d)
                m = np.zeros((32, 128), np.float32)
                m[bk[valid], rho[valid]] = 1.0
                ohb.append(m)
    Z = np.zeros((128, 512), np.float32)
    Z[np.arange(128), np.arange(128) + 128] = 1.0
    p = np.arange(128)
    G = (p[:, None] // 64 == p[None, :] // 64).astype(np.float32)
    return np.stack(ohb), Z, G


def rope_consts():
    half = 32
    inv = (10000.0 ** (-np.arange(0, half, 2, dtype=np.float32) / half)).astype(np.float32)
    t = np.arange(8192)
    row, col = t // 64, t % 64
    cos = np.zeros((64, 8192), np.float32)
    sin = np.zeros((64, 8192), np.float32)
    for f in range(64):
        pos = row if f < 32 else col
        ang = pos.astype(np.float32) * inv[f % 16]
        cos[f] = np.cos(ang)
        sin[f] = np.sin(ang)
    cos = np.concatenate([cos, cos], 0)
    sin = np.concatenate([sin, sin], 0)
    Rl = np.zeros((128, 128), np.float32)
    for f0 in range(0, 128, 32):
        for i in range(16):
            Rl[f0 + 16 + i, f0 + i] = -1.0
            Rl[f0 + i, f0 + 16 + i] = 1.0
    return cos, sin, Rl


def build_attn(cfg, ctx=None, io=None):
    io = io or {}
    if ctx is None:
        nc = bass.Bass("TRN2", target_bir_lowering=False)
    else:
        nc = ctx.nc
        ctx.reset()
    n_ext, own_off, n_own, CH, n_add = cfg["n_ext"], cfg["own_off"], cfg["n_own"], cfg["CH"], cfg["n_add"]
    nh, nkv = cfg["nh"], cfg["nkv"]
    npair, ngrp = nh // 2, nkv // 2
    qmap, kmap, kvq = cfg["qmap"], cfg["kmap"], cfg["kv_of_q"]
    dense = cfg["dense"]
    sinkf = cfg["sink"]
    branches = cfg["branches"]
    npass = sum(len(b) for (_, _, wins) in branches for (_, b) in wins)
    nbw = sum(len(wins) for (_, _, wins) in branches)

    def din(name, shape, dt=F32):
        if name in io:
            return io[name]
        return nc.dram_tensor(name, list(shape), dt, kind="ExternalInput").ap()

    xa = din("xa", [n_ext, D])
    adds = [din(f"pa{i}", [n_ext, D]) for i in range(n_add)]
    gn = din("gn", [1, D])
    wqb = din("wqb", [D, npair * 128])
    wkb = din("wkb", [D, ngrp * 128])
    wvb = din("wvb", [D, nkv * 64])
    wo = din("wo", [D, D])
    gqb = din("gqb", [128, 1])
    gkb = din("gkb", [128, 1])
    gq1 = din("gq1", [1, 64])
    gk1 = din("gk1", [1, 64])
    tblT = din("tblT", [32, nh])
    tblB = din("tblB", [1, nh * 32])
    sink1 = din("sink1", [1, nh])
    cOH = din("cOH", [npass, 32, 128])
    cZ = din("cZ", [128, 512])
    cG = din("cG", [128, 128])
    valid2 = din("valid2", [128, n_ext // 128])
    if dense:
        xk = din("xk", [NTOK, D])
        wqd = din("wqd", [D, 512])
        wkd = din("wkd", [D, 128])
        wvd = din("wvd", [D, 128])
        gqd = din("gqd", [128, 1])
        gkd = din("gkd", [128, 1])
        gqd1 = din("gqd1", [1, 64])
        gkd1 = din("gkd1", [1, 64])
        cosk = din("cosk", [128, NTOK])
        sink_ = din("sink_", [128, NTOK])
        cosq = din("cosq", [128, n_own])
        sinq = din("sinq", [128, n_own])
        cR = din("cR", [128, 128])
    VW = nkv * 65
    OW = nh * 65
    if ctx is None:
        out = nc.dram_tensor("out", [n_own, D], F32, kind="ExternalOutput").ap()
        vbuf = nc.dram_tensor("vbuf", [n_ext, VW], BF16).ap()
        obuf = [nc.dram_tensor(f"obuf{i}", [n_own, OW], F32).ap() for i in range(len(branches))]
        xsum = nc.dram_tensor("xsum", [n_ext, D], F32).ap() if n_add else xa
        S = Sched(nc)
        A = Arena(nc, "arena", 204 * 1024)
        psall = nc.alloc_psum_tensor("psall", [128, 4096], F32)
    else:
        out = io["out"]
        tg = cfg["name"]
        vbuf = ctx.scratch(tg + "vbuf", [n_ext, VW], BF16)
        obuf = [ctx.scratch(tg + f"obuf{i}", [n_own, OW], F32) for i in range(len(branches))]
        xsum = ctx.scratch(tg + "xsum", [n_ext, D], F32) if n_add else xa
        S, A, psall = ctx.S, ctx.A, ctx.psall
    ps = [psall[:, i * 512:(i + 1) * 512] for i in range(8)]
    psb = [p.bitcast(BF16) for p in ps]
    PB = [Buf(f"ps{i}") for i in range(8)]

    identf, identb, Bif, Bib = make_ident(S, A)
    Bc = Buf("consts")

    def cload(shape, src, dt=F32, eng="sp", **kw):
        t = A.alloc(shape, dt)
        S.add(eng, DMA(t, src, **kw), pwrites=[Bc], dma=True)
        return t

    def wload(src, cols):
        t = A.alloc([8, cols], BF16)
        v = src.rearrange("(c p) f -> p c f", p=128)
        for c in range(8):
            S.add("pool", DMA(t[:, c, :], v[:, c, :]), pwrites=[Bc], dma=True)
        return t

    gcol = cload([8], gn.rearrange("o (c p) -> p (o c)", p=128), allow_slow_non_contiguous=True)
    G_bf = cload([128], cG, BF16, "pool")
    Z_bf = cload([512], cZ, BF16, "pool")
    wo_bf = wload(wo, D)
    val_sb = cload([n_ext // 128], valid2)
    gq_c = cload([1], gqb)
    gk_c = cload([1], gkb)
    gqB = cload([64], gq1.partition_broadcast(128))
    gkB = cload([64], gk1.partition_broadcast(128))
    tbB = cload([nh, 32], tblB.partition_broadcast(128))
    skB = cload([nh], sink1.partition_broadcast(128))
    tT = A.alloc([nh], F32)
    S.add("sp", DMA(tT[0:32, :], tblT), pwrites=[Bc], dma=True)
    cst = A.alloc([16], F32)
    MbB = A.alloc([nh], F32)
    SHB = A.alloc([nh], F32)
    skE = A.alloc([nh], F32)
    Bk = Buf("cst")

    def mk_mqk(gA, gB, o):
        S.add("dve", lambda e: e.reduce_max(out=cst[:, o:o + 1], in_=gA, axis=AX.X, apply_absolute_value=True),
              reads=[Bc], pwrites=[Bk])
        S.add("dve", lambda e: e.reduce_max(out=cst[:, o + 1:o + 2], in_=gB, axis=AX.X, apply_absolute_value=True),
              reads=[Bc], pwrites=[Bk])
        S.add("dve", lambda e: e.tensor_tensor(out=cst[:, o + 2:o + 3], in0=cst[:, o:o + 1], in1=cst[:, o + 1:o + 2],
                                               op=ALU.mult), reads=[Bk], pwrites=[Bk])
        S.add("dve", lambda e: e.tensor_scalar_mul(out=cst[:, o + 2:o + 3], in0=cst[:, o + 2:o + 3], scalar1=8.0),
              reads=[Bk], pwrites=[Bk])
        S.add("dve", lambda e: e.tensor_scalar_mul(out=cst[:, o + 3:o + 4], in0=cst[:, o + 2:o + 3], scalar1=-1.0),
              reads=[Bk], pwrites=[Bk])

    mk_mqk(gqB, gkB, 0)
    S.add("dve", lambda e: e.tensor_reduce(out=MbB, in_=tbB, axis=AX.X, op=ALU.max), reads=[Bc], writes=[Buf()])
    BSH = Buf("SH")
    if sinkf:
        S.add("dve", lambda e: e.tensor_tensor(out=SHB, in0=skB, in1=MbB, op=ALU.subtract), reads=[Bc], writes=[BSH])
        S.add("dve", lambda e: e.tensor_scalar_add(out=SHB, in0=SHB, scalar1=cst[:, 3:4]), reads=[BSH, Bk], writes=[BSH])
        S.add("dve", lambda e: e.tensor_scalar_max(out=SHB, in0=SHB, scalar1=0.0), reads=[BSH], writes=[BSH])
        S.add("dve", lambda e: e.tensor_tensor(out=SHB, in0=SHB, in1=MbB, op=ALU.add), reads=[BSH], writes=[BSH])
        S.add("dve", lambda e: e.tensor_tensor(out=skE, in0=skB, in1=SHB, op=ALU.subtract), reads=[BSH, Bc],
              writes=[Buf()])
        S.add("act", lambda e: e.activation(out=skE, in_=skE, func=AF.Exp, bias=cst[:, 3:4], scale=1.0),
              reads=[Bk], pwrites=[BSH])
    else:
        S.add("dve", lambda e: e.tensor_copy(out=SHB, in_=MbB), writes=[BSH])
    etb = A.alloc([nh], BF16)
    etf = A.alloc([nh], F32)
    Bet = Buf("etb")
    S.add("dve", lambda e: e.tensor_tensor(out=etf[0:32, :], in0=tT[0:32, :], in1=SHB[0:32, :], op=ALU.subtract),
          reads=[Bc, BSH], writes=[Bet])
    S.add("act", lambda e: e.activation(out=etb[0:32, :], in_=etf[0:32, :], func=AF.Exp), reads=[Bet], writes=[Bet])

    A.mark()
    oh_bf = A.alloc([npass, 128], BF16)
    S.add("pool", DMA(oh_bf[0:32], cOH.rearrange("n b r -> b n r")), pwrites=[Bc], dma=True)
    tab = A.alloc([npass, nh], BF16)
    Btab = Buf("tab")
    for pi in range(npass):
        S.add("pe", lambda e, pi=pi: e.matmul(ps[4][:, 0:nh], lhsT=oh_bf[0:32, pi, :], rhs=etb[0:32, :], start=True, stop=True),
              reads=[Bc, Bet], writes=[PB[4]])
        S.add("dve", lambda e, pi=pi: e.tensor_copy(out=tab[:, pi, :], in_=ps[4][:, 0:nh]), reads=[PB[4]], pwrites=[Btab])
    A.release()
    A.mark()
    oh_bf = A.alloc([npass, 128], BF16)
    tab = A.alloc([npass, nh], BF16)
    EB = [A.alloc([nh, 128], BF16) for _ in range(nbw)]
    BEB = [Buf(f"EB{i}") for i in range(nbw)]
    nbk = (128 * nh) // 512
    pi = 0
    bw = 0
    for (d, radius, wins) in branches:
        for (off, bases) in wins:
            for qq in range(128):
                for j, base in enumerate(bases):
                    o0 = qq * nh
                    S.add("pe", lambda e, o0=o0, base=base, qq=qq, pj=pi + j, j=j, nb=len(bases): e.matmul(
                        psall[:, o0:o0 + nh], lhsT=Z_bf[:, base - qq:base - qq + 128], rhs=tab[:, pj, :],
                        start=(j == 0), stop=(j == nb - 1)),
                        reads=[Bc, Btab], writes=[PB[k_] for k_ in range(nbk)] if (qq == 0 and j == 0) else [],
                        pwrites=[] if (qq == 0 and j == 0) else [PB[0]])
            S.add("dve", lambda e, bw=bw: e.tensor_copy(
                out=EB[bw], in_=psall[:, 0:128 * nh].rearrange("p (q h) -> p h q", h=nh)),
                reads=[PB[k_] for k_ in range(nbk)], writes=[BEB[bw]])
            pi += len(bases)
            bw += 1

    if cfg.get("stop") == "eb":
        S.barrier()
        S.emit()
        return nc
    NCH = CH // 128
    wq_bf = wload(wqb, npair * 128)
    wk_bf = wload(wkb, ngrp * 128)
    wv_bf = wload(wvb, nkv * 64)
    Kb = A.alloc([ngrp, n_ext], BF16)
    Qb = A.alloc([npair, n_own], BF16)
    BKb, BQb = Buf("Kb"), Buf("Qb")
    A.mark()
    xts = [A.alloc([D], F32) for _ in range(2)]
    Bxt = [Buf() for _ in range(2)]
    xad = [A.alloc([D], F32) for _ in range(2)]
    Bxa = [Buf() for _ in range(2)]
    xnb = [A.alloc([D], BF16) for _ in range(2)]
    Bxn = [Buf() for _ in range(2)]
    hTs = [A.alloc([8, CH], BF16) for _ in range(2)]
    BhT = [Buf() for _ in range(2)]
    sts = [A.alloc([4], F32) for _ in range(4)]
    Bst = [Buf() for _ in range(4)]
    junk = A.alloc([D], BF16)
    Bjunk = Buf()
    sqs = [A.alloc([CH], BF16) for _ in range(2)]
    Bsq = [Buf() for _ in range(2)]
    sds = [A.alloc([CH], F32) for _ in range(2)]
    Bsd = [Buf() for _ in range(2)]
    vsts = [A.alloc([nkv, 65], BF16) for _ in range(2)]
    Bvs = [Buf() for _ in range(2)]
    ctr = {"t": 0, "n": 0, "v": 0}
    Bvbuf = Buf("vbuf")
    Bxsum = Buf("xsum")

    def make_hT(src, tile0, slot, write_sum=False):
        hT, bh = hTs[slot], BhT[slot]
        for ti in range(NCH):
            T = tile0 + ti
            k = ctr["t"] % 2
            ctr["t"] += 1
            xt, bxt = xts[k], Bxt[k]
            st, bst = sts[ctr["t"] % 4], Bst[ctr["t"] % 4]
            S.add("sp", DMA(xt, src[T * 128:(T + 1) * 128, :]), writes=[bxt], dma=True)
            if write_sum and n_add:
                for ai, ad in enumerate(adds):
                    xa_, bxa = xad[ai % 2], Bxa[ai % 2]
                    S.add("act", DMA(xa_, ad[T * 128:(T + 1) * 128, :]), writes=[bxa], dma=True)
                    S.add("dve", lambda e, xt=xt, xa_=xa_: e.tensor_tensor(out=xt, in0=xt, in1=xa_, op=ALU.add),
                          reads=[bxt, bxa], writes=[bxt])
                S.add("sp", DMA(xsum[T * 128:(T + 1) * 128, :], xt), reads=[bxt], pwrites=[Bxsum], dma=True)
            S.add("act", lambda e, xt=xt, st=st, junk=junk: e.activation(out=junk, in_=xt, func=AF.Square, scale=1.0 / 32,
                                                                         accum_out=st[:, 0:1]),
                  reads=[bxt], writes=[Bjunk, bst])
            S.add("act", lambda e, st=st: e.activation(out=st[:, 1:2], in_=st[:, 0:1], func=AF.Sqrt, bias=EPS, scale=1.0),
                  reads=[bst], pwrites=[bst])
            S.add("dve", lambda e, st=st: e.reciprocal(out=st[:, 2:3], in_=st[:, 1:2]), reads=[bst], pwrites=[bst])
            xn, bxn = xnb[k], Bxn[k]
            S.add("dve", lambda e, xn=xn, xt=xt, st=st: e.tensor_scalar_mul(out=xn, in0=xt, scalar1=st[:, 2:3]),
                  reads=[bxt, bst], writes=[bxn])
            pb = k
            for c in range(8):
                S.add("pe", lambda e, pb=pb, c=c, xn=xn: e.transpose(out=psb[pb][:, c * 128:(c + 1) * 128],
                                                                     in_=xn[:, c * 128:(c + 1) * 128], identity=identb),
                      reads=[bxn, Bib], writes=[PB[pb]] if c == 0 else [], pwrites=[] if c == 0 else [PB[pb]])
            for c in range(8):
                src_ = psb[pb][:, c * 128:(c + 1) * 128]
                dst = hT[:, c, ti * 128:(ti + 1) * 128]
                first = (ti == 0 and c == 0)
                if c % 2 == 0:
                    S.add("act", lambda e, src_=src_, dst=dst, c=c: e.activation(out=dst, in_=src_, func=AF.Copy,
                                                                                 scale=gcol[:, c:c + 1]),
                          reads=[PB[pb], Bc], writes=[bh] if first else [], pwrites=[] if first else [bh])
                else:
                    S.add("dve", lambda e, src_=src_, dst=dst, c=c: e.tensor_scalar_mul(out=dst, in0=src_,
                                                                                        scalar1=gcol[:, c:c + 1]),
                          reads=[PB[pb], Bc], pwrites=[bh])
        return hT, bh

    def proj_fm(hT, bh, w_bf, col0, gcolumn, dst, bdst, rope=None):
        k = ctr["n"] % 2
        ctr["n"] += 1
        pq, pss = 2 + k, 4 + k
        for c in range(8):
            S.add("pe", lambda e, c=c: e.matmul(ps[pq][:, 0:CH], lhsT=w_bf[:, c, col0:col0 + 128], rhs=hT[:, c, :],
                                                start=(c == 0), stop=(c == 7)),
                  reads=[bh, Bc], writes=[PB[pq]] if c == 0 else [], pwrites=[] if c == 0 else [PB[pq]])
        sq, bsq, sd, bsd = sqs[k], Bsq[k], sds[k], Bsd[k]
        S.add("act", lambda e: e.activation(out=sq, in_=ps[pq][:, 0:CH], func=AF.Square), reads=[PB[pq]], writes=[bsq])
        S.add("pe", lambda e: e.matmul(ps[pss][:, 0:CH], lhsT=G_bf, rhs=sq, start=True, stop=True),
              reads=[bsq, Bc], writes=[PB[pss]])
        S.add("act", lambda e: e.activation(out=sd, in_=ps[pss][:, 0:CH], func=AF.Sqrt, bias=EPS, scale=1.0 / 64),
              reads=[PB[pss]], writes=[bsd])
        S.add("dve", lambda e: e.reciprocal(out=sd, in_=sd), reads=[bsd], writes=[bsd])
        if rope is None:
            S.add("dve", lambda e: e.scalar_tensor_tensor(out=dst, in0=ps[pq][:, 0:CH], scalar=gcolumn, in1=sd,
                                                          op0=ALU.mult, op1=ALU.mult),
                  reads=[PB[pq], bsd, Bc], pwrites=[bdst])
        else:
            cos_d, sin_d, c0 = rope
            qn, bqn = rp["qn"][k], rp["Bqn"][k]
            cs, bcs, sn, bsn = rp["cs"][k], rp["Bcs"][k], rp["sn"][k], rp["Bsn"][k]
            S.add("sp", DMA(cs, cos_d[:, c0:c0 + CH]), writes=[bcs], dma=True)
            S.add("sp", DMA(sn, sin_d[:, c0:c0 + CH]), writes=[bsn], dma=True)
            S.add("dve", lambda e: e.scalar_tensor_tensor(out=qn, in0=ps[pq][:, 0:CH], scalar=gcolumn, in1=sd,
                                                          op0=ALU.mult, op1=ALU.mult),
                  reads=[PB[pq], bsd, Bc], writes=[bqn])
            S.add("pe", lambda e: e.matmul(ps[6][:, 0:CH], lhsT=rp["R"], rhs=qn, start=True, stop=True),
                  reads=[bqn, Bc], writes=[PB[6]])
            S.add("dve", lambda e: e.tensor_tensor(out=cs, in0=qn, in1=cs, op=ALU.mult), reads=[bqn, bcs], writes=[bcs])
            S.add("dve", lambda e: e.tensor_tensor(out=sn, in0=ps[6][:, 0:CH], in1=sn, op=ALU.mult),
                  reads=[PB[6], bsn], writes=[bsn])
            S.add("dve", lambda e: e.tensor_tensor(out=dst, in0=cs, in1=sn, op=ALU.add), reads=[bcs, bsn], pwrites=[bdst])

    def proj_v(hT, bh, ti, w_bf, ncols, dst_fn):
        for c in range(8):
            S.add("pe", lambda e, c=c: e.matmul(ps[7][:, 0:ncols], lhsT=hT[:, c, ti * 128:(ti + 1) * 128],
                                                rhs=w_bf[:, c, 0:ncols], start=(c == 0), stop=(c == 7)),
                  reads=[bh, Bc], writes=[PB[7]] if c == 0 else [], pwrites=[] if c == 0 else [PB[7]])
        dst_fn(ps[7][:, 0:ncols])

    own_lo, own_hi = own_off, own_off + n_own
    for ch in range(n_ext // CH):
        hT, bh = make_hT(xa, ch * NCH, ch % 2, write_sum=True)
        for g in range(ngrp):
            proj_fm(hT, bh, wk_bf, g * 128, gk_c[:, 0:1], Kb[:, g, ch * CH:(ch + 1) * CH], BKb)
        for ti in range(NCH):
            T = ch * NCH + ti
            k = ctr["v"] % 2
            ctr["v"] += 1
            vs, bvs = vsts[k], Bvs[k]

            def put(psv, vs=vs, bvs=bvs, T=T):
                S.add("act", lambda e: e.copy(out=vs[:, :, 0:64], in_=psv.rearrange("p (h d) -> p h d", h=nkv)),
                      reads=[PB[7]], writes=[bvs])
                S.add("dve", lambda e: e.tensor_copy(out=vs[:, :, 64],
                                                     in_=val_sb[:, T:T + 1].to_broadcast([128, nkv])),
                      reads=[Bc], pwrites=[bvs])
                S.add("act", DMA(vbuf[T * 128:(T + 1) * 128, :], vs.rearrange("p h d -> p (h d)")), reads=[bvs],
                      pwrites=[Bvbuf], dma=True)
            proj_v(hT, bh, ti, wv_bf, nkv * 64, put)
        if own_lo <= ch * CH < own_hi:
            for p_ in range(npair):
                proj_fm(hT, bh, wq_bf, p_ * 128, gq_c[:, 0:1],
                        Qb[:, p_, ch * CH - own_lo:(ch + 1) * CH - own_lo], BQb)
    S.barrier()
    A.release()
    if cfg.get("stop") == "pass1":
        S.barrier()
        S.emit()
        return nc

    A.mark()
    NW = 6
    vws = [A.alloc([nkv, 65], BF16) for _ in range(NW)]
    Bvw = [Buf() for _ in range(NW)]
    pts = [A.alloc([512], BF16) for _ in range(3)]
    Bpt = [Buf() for _ in range(3)]
    PTs = [A.alloc([512], BF16) for _ in range(3)]
    BPT = [Buf() for _ in range(3)]
    osts = [A.alloc([nh, 65], F32) for _ in range(2)]
    Bos = [Buf() for _ in range(2)]
    Bob = [Buf(f"obuf{i}") for i in range(len(branches))]
    ngr = nh // 4
    cw = {"w": 0, "p": 0, "s": 0, "o": 0, "t": 0}
    bw0 = 0
    units = []
    for bi, (d, radius, wins) in enumerate(branches):
        nt = n_own // d // 128
        for r in range(d):
            for i in range(nt):
                if cfg.get("band_limit") is not None and cw["t"] >= cfg["band_limit"]:
                    break
                cw["t"] += 1
                tile = dict(bi=bi, d=d, wins=wins, s0=own_off // d + 128 * i, q0=r + d * 128 * i, r=r, bw0=bw0,
                            ko=cw["t"] % 2)
                for g in range(ngr):
                    for wi in range(len(wins)):
                        units.append((tile, g, wi))
        bw0 += len(wins)
    LAG = 2
    ust = {}
    for step in range(len(units) + LAG):
        if step < len(units):
            tile, g, wi = units[step]
            d, wins, q0 = tile["d"], tile["wins"], tile["q0"]
            if g == 0 and wi == 0:
                vt = []
                for (off, bases) in wins:
                    kw_ = cw["w"] % NW
                    cw["w"] += 1
                    u0 = tile["r"] + d * (tile["s0"] + off)
                    S.add("sp", DMA(vws[kw_].rearrange("p h d -> p (h d)"), vbuf[u0:u0 + d * 127 + 1:d, :]),
                          reads=[Bvbuf], writes=[Bvw[kw_]], dma=True)
                    vt.append((kw_, u0))
                tile["vt"] = vt
            if wi == 0:
                tile[("ob", g)] = 4 + cw["o"] % 4
                cw["o"] += 1
            kw_, u0 = tile["vt"][wi]
            sb = cw["s"] % 4
            cw["s"] += 1
            for j in range(4):
                hq = g * 4 + j
                pr, hf = qmap[hq]
                kg, khf = kmap[kvq[hq]]
                assert hf == khf
                S.add("pe", lambda e, sb=sb, j=j, hf=hf, kg=kg, pr=pr, u0=u0, q0=q0, d=d: e.matmul(
                    ps[sb][:, j * 128:(j + 1) * 128],
                    lhsT=Kb[hf * 64:(hf + 1) * 64, kg, u0:u0 + d * 127 + 1:d],
                    rhs=Qb[hf * 64:(hf + 1) * 64, pr, q0:q0 + d * 127 + 1:d], start=True, stop=True),
                    reads=[BKb, BQb], writes=[PB[sb]] if j == 0 else [], pwrites=[] if j == 0 else [PB[sb]])
            kp = cw["p"] % 3
            cw["p"] += 1
            S.add("act", lambda e, kp=kp, sb=sb: e.activation(out=pts[kp], in_=ps[sb][:, 0:512], func=AF.Exp,
                                                              bias=cst[:, 3:4], scale=0.125),
                  reads=[PB[sb], Bk], writes=[Bpt[kp]])
            ebv = EB[tile["bw0"] + wi][:, g * 4:(g + 1) * 4, :].rearrange("p h q -> p (h q)")
            S.add("dve", lambda e, kp=kp, ebv=ebv: e.tensor_tensor(out=PTs[kp], in0=pts[kp], in1=ebv, op=ALU.mult),
                  reads=[Bpt[kp], BEB[tile["bw0"] + wi]], writes=[BPT[kp]])
            ust[step] = kp
        if step >= LAG:
            tile, g, wi = units[step - LAG]
            kp = ust.pop(step - LAG)
            d, wins, q0 = tile["d"], tile["wins"], tile["q0"]
            kw_, u0 = tile["vt"][wi]
            ob = tile[("ob", g)]
            ost, bos = osts[tile["ko"]], Bos[tile["ko"]]
            for j in range(4):
                hq = g * 4 + j
                S.add("pe", lambda e, ob=ob, j=j, kp=kp, kw_=kw_, kv=kvq[hq], wi=wi, nw=len(wins): e.matmul(
                    ps[ob][:, j * 65:(j + 1) * 65], lhsT=PTs[kp][:, j * 128:(j + 1) * 128],
                    rhs=vws[kw_][:, kv, :], start=(wi == 0 and j == 0), stop=(wi == nw - 1)),
                    reads=[BPT[kp], Bvw[kw_]],
                    writes=[PB[ob]] if (wi == 0 and j == 0) else [],
                    pwrites=[] if (wi == 0 and j == 0) else [PB[ob]])
            if wi == len(wins) - 1:
                S.add("act", lambda e, ob=ob, g=g, ost=ost: e.copy(
                    out=ost[:, g * 4:(g + 1) * 4, :].rearrange("p h d -> p (h d)"), in_=ps[ob][:, 0:260]),
                    reads=[PB[ob]], writes=[bos] if g == 0 else [], pwrites=[] if g == 0 else [bos])
                if g == ngr - 1:
                    S.add("act", DMA(obuf[tile["bi"]][q0:q0 + d * 127 + 1:d, :], ost.rearrange("p h d -> p (h d)")),
                          reads=[bos], pwrites=[Bob[tile["bi"]]], dma=True)
    S.barrier()
    A.release()
    A.release()

    if cfg.get("stop") == "band":
        S.barrier()
        S.emit()
        return nc
    OA = A.alloc([4, D], BF16)
    BOA = Buf("OA")
    if dense:
        wqd_bf = wload(wqd, 512)
        wkd_bf = wload(wkd, 128)
        wvd_bf = wload(wvd, 128)
        gqd_c = cload([1], gqd)
        gkd_c = cload([1], gkd)
        gqdB = cload([64], gqd1.partition_broadcast(128))
        gkdB = cload([64], gkd1.partition_broadcast(128))
        R_bf = cload([128], cR, BF16, "pool")
        mk_mqk(gqdB, gkdB, 4)
        Kd = A.alloc([NTOK], BF16)
        Qd = A.alloc([4, n_own], BF16)
        V1d = A.alloc([NTOK // 128, 2, 65], BF16)
        BKd, BQd, BVd = Buf("Kd"), Buf("Qd"), Buf("V1d")
        S.add("dve", lambda e: e.memset(V1d[:, :, :, 64], 1.0), pwrites=[BVd])
        A.mark()
        xts[:] = [A.alloc([D], F32) for _ in range(2)]
        xnb[:] = [A.alloc([D], BF16) for _ in range(2)]
        hTs[:] = [A.alloc([8, CH], BF16) for _ in range(2)]
        sts[:] = [A.alloc([4], F32) for _ in range(4)]
        junk = A.alloc([D], BF16)
        sqs[:] = [A.alloc([CH], BF16) for _ in range(2)]
        sds[:] = [A.alloc([CH], F32) for _ in range(2)]
        rp = {"qn": [A.alloc([CH], BF16) for _ in range(2)], "Bqn": [Buf(), Buf()],
              "cs": [A.alloc([CH], F32) for _ in range(2)], "Bcs": [Buf(), Buf()],
              "sn": [A.alloc([CH], F32) for _ in range(2)], "Bsn": [Buf(), Buf()], "R": R_bf}
        saved_nadd = n_add
        for ch in range(n_own // CH):
            hT, bh = make_hT(xsum, (own_off + ch * CH) // 128, ch % 2)
            for p_ in range(4):
                proj_fm(hT, bh, wqd_bf, p_ * 128, gqd_c[:, 0:1], Qd[:, p_, ch * CH:(ch + 1) * CH], BQd,
                        rope=(cosq, sinq, ch * CH))
        for ch in range(NTOK // CH):
            hT, bh = make_hT(xk, ch * NCH, ch % 2)
            proj_fm(hT, bh, wkd_bf, 0, gkd_c[:, 0:1], Kd[:, ch * CH:(ch + 1) * CH], BKd, rope=(cosk, sink_, ch * CH))
            for ti in range(NCH):
                T = ch * NCH + ti

                def putd(psv, T=T):
                    S.add("act", lambda e: e.copy(out=V1d[:, T, :, 0:64], in_=psv.rearrange("p (h d) -> p h d", h=2)),
                          reads=[PB[7]], pwrites=[BVd])
                proj_v(hT, bh, ti, wvd_bf, 128, putd)
        S.barrier()
        A.release()

    if cfg.get("stop") == "densep":
        S.barrier()
        S.emit()
        return nc
    A.mark()
    pts2 = [A.alloc([512], BF16) for _ in range(4)]
    Bpt2 = [Buf() for _ in range(4)]
    obl = [[A.alloc([nh, 65], F32) for _ in range(len(branches))] for _ in range(2)]
    Bol = [[Buf() for _ in range(len(branches))] for _ in range(2)]
    rcs = [A.alloc([16], F32) for _ in range(2)]
    Brc = [Buf() for _ in range(2)]
    OT = A.alloc([8, 512], BF16)
    BOT = Buf("OT")
    xrs = [A.alloc([D], F32) for _ in range(2)]
    Bxr = [Buf() for _ in range(2)]
    cd = {"s": 0, "p": 0, "o": 0, "l": 0, "x": 0}
    band_col0 = cfg["band_col0"]
    for chq in range(n_own // 512):
        first_oa = [True]

        def oa_w():
            if first_oa[0]:
                first_oa[0] = False
                return dict(writes=[BOA])
            return dict(pwrites=[BOA])
        if dense:
            NK = NTOK // 128
            units = [(hq, kt) for hq in range(8) for kt in range(NK)]
            LAG = 2
            ust = {}
            obh = {}
            for step in range(len(units) + LAG):
                if step < len(units):
                    hq, kt = units[step]
                    pr, hf = hq % 4, hq // 4
                    if kt == 0:
                        obh[hq] = 4 + cd["o"] % 2
                        cd["o"] += 1
                    sb = cd["s"] % 4
                    cd["s"] += 1
                    S.add("pe", lambda e, sb=sb, hf=hf, pr=pr, kt=kt, chq=chq: e.matmul(
                        ps[sb][:, 0:512], lhsT=Kd[hf * 64:(hf + 1) * 64, kt * 128:(kt + 1) * 128],
                        rhs=Qd[hf * 64:(hf + 1) * 64, pr, chq * 512:(chq + 1) * 512], start=True, stop=True),
                        reads=[BKd, BQd], writes=[PB[sb]])
                    kp = cd["p"] % 4
                    cd["p"] += 1
                    S.add("act", lambda e, kp=kp, sb=sb: e.activation(out=pts2[kp], in_=ps[sb][:, 0:512], func=AF.Exp,
                                                                      bias=cst[:, 7:8], scale=0.125),
                          reads=[PB[sb], Bk], writes=[Bpt2[kp]])
                    ust[step] = kp
                if step >= LAG:
                    hq, kt = units[step - LAG]
                    pr, hf = hq % 4, hq // 4
                    kp = ust.pop(step - LAG)
                    ob = obh[hq]
                    for sub in range(4):
                        S.add("pe", lambda e, ob=ob, sub=sub, kp=kp, kt=kt, hf=hf: e.matmul(
                            ps[ob][:, sub * 65:(sub + 1) * 65], lhsT=pts2[kp][:, sub * 128:(sub + 1) * 128],
                            rhs=V1d[:, kt, hf, :], start=(kt == 0 and sub == 0), stop=(kt == NK - 1)),
                            reads=[Bpt2[kp], BVd],
                            writes=[PB[ob]] if (kt == 0 and sub == 0) else [],
                            pwrites=[] if (kt == 0 and sub == 0) else [PB[ob]])
                    if kt == NK - 1:
                        kr = cd["l"] % 2
                        cd["l"] += 1
                        ov = ps[ob][:, 0:260].rearrange("p (s d) -> p s d", s=4)
                        S.add("dve", lambda e, kr=kr, ov=ov: e.reciprocal(out=rcs[kr][:, 0:4], in_=ov[:, :, 64]),
                              reads=[PB[ob]], writes=[Brc[kr]])
                        col = cfg["dense_col0"] + hq * 64
                        S.add("dve", lambda e, kr=kr, ov=ov, col=col: e.tensor_tensor(
                            out=OA[:, :, col:col + 64], in0=ov[:, :, 0:64],
                            in1=rcs[kr][:, 0:4].unsqueeze(2).to_broadcast([128, 4, 64]), op=ALU.mult),
                            reads=[PB[ob], Brc[kr]], **oa_w())
        for sub in range(4):
            t = chq * 4 + sub
            ko = cd["x"] % 2
            cd["x"] += 1
            for bi in range(len(branches)):
                S.add("sp", DMA(obl[ko][bi].rearrange("p h d -> p (h d)"), obuf[bi][t * 128:(t + 1) * 128, :]),
                      reads=[Bob[bi]], writes=[Bol[ko][bi]], dma=True)
            o0, b0_ = obl[ko][0], Bol[ko][0]
            for bi in range(1, len(branches)):
                S.add("dve", lambda e, o0=o0, o1=obl[ko][bi]: e.tensor_tensor(out=o0, in0=o0, in1=o1, op=ALU.add),
                      reads=[b0_, Bol[ko][bi]], writes=[b0_])
            if sinkf:
                S.add("dve", lambda e, o0=o0: e.tensor_tensor(out=o0[:, :, 64], in0=o0[:, :, 64], in1=skE, op=ALU.add),
                      reads=[b0_, BSH], writes=[b0_])
            kr = cd["l"] % 2
            cd["l"] += 1
            S.add("dve", lambda e, kr=kr, o0=o0: e.reciprocal(out=rcs[kr][:, 0:nh], in_=o0[:, :, 64]),
                  reads=[b0_], writes=[Brc[kr]])
            S.add("dve", lambda e, kr=kr, o0=o0, sub=sub: e.tensor_tensor(
                out=OA[:, sub, band_col0:band_col0 + nh * 64].rearrange("p (h d) -> p h d", h=nh), in0=o0[:, :, 0:64],
                in1=rcs[kr][:, 0:nh].unsqueeze(2).to_broadcast([128, nh, 64]), op=ALU.mult),
                reads=[b0_, Brc[kr]], **oa_w())
        for c8 in range(8):
            pb = 6 + c8 % 2
            for sub in range(4):
                S.add("pe", lambda e, pb=pb, sub=sub, c8=c8: e.transpose(
                    out=psb[pb][:, sub * 128:(sub + 1) * 128], in_=OA[:, sub, c8 * 128:(c8 + 1) * 128], identity=identb),
                    reads=[BOA, Bib], writes=[PB[pb]] if sub == 0 else [], pwrites=[] if sub == 0 else [PB[pb]])
            if c8 % 2 == 0:
                S.add("act", lambda e, pb=pb, c8=c8: e.copy(out=OT[:, c8, :], in_=psb[pb][:, 0:512]),
                      reads=[PB[pb]], writes=[BOT] if c8 == 0 else [], pwrites=[] if c8 == 0 else [BOT])
            else:
                S.add("dve", lambda e, pb=pb, c8=c8: e.tensor_copy(out=OT[:, c8, :], in_=psb[pb][:, 0:512]),
                      reads=[PB[pb]], pwrites=[BOT])
        for sub in range(4):
            t = chq * 4 + sub
            kx = (chq * 4 + sub) % 2
            xr, bxr = xrs[kx], Bxr[kx]
            S.add("sp", DMA(xr, xsum[own_off + t * 128:own_off + (t + 1) * 128, :]), reads=[Bxsum], writes=[bxr], dma=True)
            for hf in range(2):
                pb = (sub * 2 + hf) % 4
                for c8 in range(8):
                    S.add("pe", lambda e, pb=pb, c8=c8, sub=sub, hf=hf: e.matmul(
                        ps[pb][:, 0:512], lhsT=OT[:, c8, sub * 128:(sub + 1) * 128],
                        rhs=wo_bf[:, c8, hf * 512:(hf + 1) * 512], start=(c8 == 0), stop=(c8 == 7)),
                        reads=[BOT, Bc], writes=[PB[pb]] if c8 == 0 else [], pwrites=[] if c8 == 0 else [PB[pb]])
                S.add("dve", lambda e, pb=pb, xr=xr, hf=hf: e.tensor_tensor(
                    out=xr[:, hf * 512:(hf + 1) * 512], in0=ps[pb][:, 0:512], in1=xr[:, hf * 512:(hf + 1) * 512],
                    op=ALU.add), reads=[PB[pb], bxr], writes=[bxr])
            S.add("sp", DMA(out[t * 128:(t + 1) * 128, :], xr), reads=[bxr], dma=True)
    S.barrier()
    if ctx is not None:
        return None
    S.emit()
    return nc


def build_add3(n_rows):
    nc = bass.Bass("TRN2", target_bir_lowering=False)
    srcs = [nc.dram_tensor(n, [n_rows, D], F32, kind="ExternalInput").ap() for n in ("xa", "p0", "p1")]
    out = nc.dram_tensor("out", [n_rows, D], F32, kind="ExternalOutput").ap()
    S = Sched(nc)
    A = Arena(nc, "arena", 64 * 1024)
    bufs = [[A.alloc([D], F32) for _ in range(3)] for _ in range(3)]
    Bb = [[Buf() for _ in range(3)] for _ in range(3)]
    for T in range(n_rows // 128):
        k = T % 3
        for i, eng in enumerate(("sp", "act", "sp")):
            S.add(eng, DMA(bufs[k][i], srcs[i][T * 128:(T + 1) * 128, :]), writes=[Bb[k][i]], dma=True)
        S.add("dve", lambda e, k=k: e.tensor_tensor(out=bufs[k][0], in0=bufs[k][0], in1=bufs[k][1], op=ALU.add),
              reads=[Bb[k][0], Bb[k][1]], writes=[Bb[k][0]])
        S.add("dve", lambda e, k=k: e.tensor_tensor(out=bufs[k][0], in0=bufs[k][0], in1=bufs[k][2], op=ALU.add),
              reads=[Bb[k][0], Bb[k][2]], writes=[Bb[k][0]])
        S.add("act", DMA(out[T * 128:(T + 1) * 128, :], bufs[k][0]), reads=[Bb[k][0]], dma=True)
    S.barrier()
    S.emit()
    return nc


A_WINS = [(-64, [128]), (64, [255])]
CFG_L0 = dict(name="l0", n_ext=6144, own_off=1024, n_own=4096, CH=512, n_add=0, nh=8, nkv=8,
              qmap=[(h % 4, h // 4) for h in range(8)], kmap=[(h % 4, h // 4) for h in range(8)],
              kv_of_q=list(range(8)),
              branches=[(1, 64, A_WINS), (4, 64, A_WINS), (16, 64, A_WINS)],
              sink=False, dense=True, band_col0=0, dense_col0=512)
CFG_L1 = dict(name="l1", n_ext=4352, own_off=128, n_own=4096, CH=128, n_add=2, nh=16, nkv=4,
              qmap=[(((hq // 4) // 2) * 4 + hq % 4, (hq // 4) % 2) for hq in range(16)],
              kmap=[(i // 2, i % 2) for i in range(4)],
              kv_of_q=[hq // 4 for hq in range(16)],
              branches=[(1, 128, [(-128, [128]), (0, [255, 127]), (128, [255])])],
              sink=True, dense=None, band_col0=0, dense_col0=0)


def _ext_rows(a, t0, own_off, n_ext):
    lo = t0 - own_off
    out = np.zeros((n_ext, a.shape[1]), a.dtype)
    s, e = max(lo, 0), min(lo + n_ext, a.shape[0])
    out[s - lo:e - lo] = a[s:e]
    return out


def _valid2(t0, own_off, n_ext):
    u = np.arange(n_ext) + t0 - own_off
    v = ((u >= 0) & (u < NTOK)).astype(np.float32)
    return np.ascontiguousarray(v.reshape(n_ext // 128, 128).T)


def attn_inputs(cfg, consts, half, xa_full, adds_full, gn, wq, wk, wv, wo, gq, gk, tbl, sink, dense_in=None):
    t0 = half * 4096
    n_ext, own_off = cfg["n_ext"], cfg["own_off"]
    nh = cfg["nh"]
    npair = nh // 2
    wqb = np.zeros((D, npair * 128), np.float32)
    for hq, (p_, s_) in enumerate(cfg["qmap"]):
        wqb[:, p_ * 128 + s_ * 64:p_ * 128 + s_ * 64 + 64] = wq[:, hq * 64:(hq + 1) * 64]
    wkb = np.zeros((D, (cfg["nkv"] // 2) * 128), np.float32)
    for kv, (g_, s_) in enumerate(cfg["kmap"]):
        wkb[:, g_ * 128 + s_ * 64:g_ * 128 + s_ * 64 + 64] = wk[:, kv * 64:(kv + 1) * 64]
    ohb, Z, G = consts
    m = dict(xa=_ext_rows(xa_full, t0, own_off, n_ext), gn=gn[None], wqb=wqb, wkb=wkb,
             wvb=np.ascontiguousarray(wv), wo=np.ascontiguousarray(wo),
             gqb=np.tile(gq, 2)[:, None].copy(), gkb=np.tile(gk, 2)[:, None].copy(), gq1=gq[None].copy(), gk1=gk[None].copy(),
             tblT=np.ascontiguousarray(tbl[:nh].T), tblB=np.ascontiguousarray(tbl[:nh].reshape(1, nh * 32)),
             sink1=(sink[None].copy() if sink is not None else np.zeros((1, nh), np.float32)),
             cOH=ohb, cZ=Z, cG=G, valid2=_valid2(t0, own_off, n_ext))
    for i, a in enumerate(adds_full):
        m[f"pa{i}"] = _ext_rows(a, t0, own_off, n_ext)
    if dense_in is not None:
        xk, wqd_, wkd_, wvd_, gqd_, gkd_, (cos, sin, Rl) = dense_in
        wqd = np.zeros((D, 512), np.float32)
        for hq in range(8):
            r_, i_ = hq % 4, hq // 4
            wqd[:, (r_ * 2 + i_) * 64:(r_ * 2 + i_) * 64 + 64] = wqd_[:, hq * 64:(hq + 1) * 64]
        m.update(xk=xk, wqd=wqd, wkd=np.ascontiguousarray(wkd_), wvd=np.ascontiguousarray(wvd_),
                 gqd=np.tile(gqd_, 2)[:, None].copy(), gkd=np.tile(gkd_, 2)[:, None].copy(),
                 gqd1=gqd_[None].copy(), gkd1=gkd_[None].copy(), cosk=cos, sink_=sin,
                 cosq=np.ascontiguousarray(cos[:, t0:t0 + 4096]), sinq=np.ascontiguousarray(sin[:, t0:t0 + 4096]), cR=Rl)
    return m


def moe_inputs(half, x_full, gn, router, wg, wu, wd):
    p = np.arange(128)
    cG = (p[:, None] // 16 == p[None, :] // 16).astype(np.float32)
    cL = ((p[:, None] // 16 == p[None, :] // 16) & (p[:, None] < p[None, :])).astype(np.float32)
    perm = np.concatenate([np.arange(8 * half, 8 * half + 8), np.arange(8 * (1 - half), 8 * (1 - half) + 8)])
    return dict(x=x_full, gn=gn[None], wr=np.ascontiguousarray(router[:, perm]),
                wg=np.ascontiguousarray(wg[8 * half:8 * half + 8]), wu=np.ascontiguousarray(wu[8 * half:8 * half + 8]),
                wd=np.ascontiguousarray(wd[8 * half:8 * half + 8]), cG=cG, cL=cL)


CFG_L1F = dict(CFG_L1, n_add=1)


def fused_host_inputs(inp, b):
    x = inp["x"][b]
    m = {}
    xpad = np.zeros((NTOK + 2048, D), np.float32)
    xpad[1024:1024 + NTOK] = x
    m["xpad"] = xpad
    cons0, cons1 = band_consts(CFG_L0), band_consts(CFG_L1)
    cos, sin, Rl = rope_consts()
    w0, w1 = inp["l0_w_in"], inp["l1_w_in"]
    zx = np.zeros((NTOK, D), np.float32)
    for h in range(2):
        a0 = attn_inputs(CFG_L0, cons0, h, zx, [], inp["l0_norm_attn"], w0[:, 0:512], w0[:, 512:1024], w0[:, 1024:1536],
                         inp["l0_w_out"], inp["l0_a_qnorm"], inp["l0_a_knorm"], inp["rel_bias"], None,
                         dense_in=(zx, w0[:, 1536:2048], w0[:, 2048:2176], w0[:, 2176:2304], inp["l0_b_qnorm"],
                                   inp["l0_b_knorm"], (cos, sin, Rl)))
        a1 = attn_inputs(CFG_L1F, cons1, h, zx, [zx], inp["l1_norm_attn"], w1[:, 0:1024], w1[:, 1024:1280],
                         w1[:, 1280:1536], inp["l1_w_out"], inp["l1_c_qnorm"], inp["l1_c_knorm"], inp["rel_bias"],
                         inp["l1_sink"])
        m[f"a0_valid2_{h}"] = a0["valid2"]
        m[f"a1_valid2_{h}"] = a1["valid2"]
        if h == 0:
            for k, v in a0.items():
                if k not in ("xa", "xk", "valid2", "cosq", "sinq"):
                    m["a0_" + k] = v
            for k, v in a1.items():
                if k not in ("xa", "pa0", "valid2"):
                    m["a1_" + k] = v
    p = np.arange(128)
    m["cG16"] = (p[:, None] // 16 == p[None, :] // 16).astype(np.float32)
    m["cL16"] = ((p[:, None] // 16 == p[None, :] // 16) & (p[:, None] < p[None, :])).astype(np.float32)
    for l in range(2):
        m[f"m{l}_gn"] = inp[f"l{l}_norm_ffn"][None]
        m[f"m{l}_wr"] = inp[f"l{l}_router"]
        m[f"m{l}_wg"] = inp[f"l{l}_w_gate"]
        m[f"m{l}_wu"] = inp[f"l{l}_w_up"]
        m[f"m{l}_wd"] = inp[f"l{l}_w_down"]
    return {k: np.ascontiguousarray(v) for k, v in m.items()}


def build_fused(tmpl):
    ctx = Ctx()
    nc, S, A = ctx.nc, ctx.S, ctx.A
    npdt = {np.dtype(np.float32): F32, np.dtype(np.int32): I32}
    E = {k: nc.dram_tensor(k, list(v.shape), npdt[v.dtype], kind="ExternalInput").ap() for k, v in tmpl.items()}
    out = nc.dram_tensor("out", [NTOK, D], F32, kind="ExternalOutput").ap()
    x1p = ctx.scratch("x1p", [NTOK + 256, D], F32)
    p0p = ctx.scratch("p0p", [NTOK + 256, D], F32)
    x3 = ctx.scratch("x3", [NTOK, D], F32)
    p1 = ctx.scratch("p1", [NTOK, D], F32)
    zt = A.alloc([D], F32)
    Bz = Buf()
    S.add("dve", lambda e: e.memset(zt, 0.0), writes=[Bz])
    for t in (x1p, p0p):
        S.add("sp", DMA(t[0:128, :], zt), reads=[Bz], dma=True)
        S.add("sp", DMA(t[128 + NTOK:256 + NTOK, :], zt), reads=[Bz], dma=True)
    S.barrier()
    xpad = E["xpad"]
    a0 = {k[3:]: v for k, v in E.items() if k.startswith("a0_")}
    a1 = {k[3:]: v for k, v in E.items() if k.startswith("a1_")}
    for h in range(2):
        io = dict(a0)
        io.update(xa=xpad[h * 4096:h * 4096 + 6144, :], xk=xpad[1024:1024 + NTOK, :], valid2=a0[f"valid2_{h}"],
                  cosq=a0["cosk"][:, h * 4096:(h + 1) * 4096], sinq=a0["sink_"][:, h * 4096:(h + 1) * 4096],
                  out=x1p[128 + h * 4096:128 + (h + 1) * 4096, :])
        build_attn(CFG_L0, ctx, io)
    for s_ in range(2):
        build_moe(ctx, dict(x=x1p[128:128 + NTOK, :], gn=E["m0_gn"], wr=E["m0_wr"], wg=E["m0_wg"][8 * s_:8 * s_ + 8],
                            wu=E["m0_wu"][8 * s_:8 * s_ + 8], wd=E["m0_wd"][8 * s_:8 * s_ + 8], cG=E["cG16"],
                            cL=E["cL16"], part=p0p[128:128 + NTOK, :], part_full=p0p, part_eoff=128 * D),
                  zero_init=(s_ == 0),
                  wr_sets=(8 * s_, 8 * (1 - s_)))
    for h in range(2):
        io = dict(a1)
        io.update(xa=x1p[h * 4096:h * 4096 + 4352, :], pa0=p0p[h * 4096:h * 4096 + 4352, :],
                  valid2=a1[f"valid2_{h}"], out=x3[h * 4096:(h + 1) * 4096, :])
        build_attn(CFG_L1F, ctx, io)
    for s_ in range(2):
        build_moe(ctx, dict(x=x3, gn=E["m1_gn"], wr=E["m1_wr"], wg=E["m1_wg"][8 * s_:8 * s_ + 8],
                            wu=E["m1_wu"][8 * s_:8 * s_ + 8], wd=E["m1_wd"][8 * s_:8 * s_ + 8], cG=E["cG16"],
                            cL=E["cL16"], part=p1), zero_init=(s_ == 0), wr_sets=(8 * s_, 8 * (1 - s_)))
    ctx.reset()
    bufs = [[A.alloc([D], F32) for _ in range(2)] for _ in range(3)]
    Bb = [[Buf() for _ in range(2)] for _ in range(3)]
    for T in range(NTOK // 128):
        k = T % 3
        S.add("sp", DMA(bufs[k][0], x3[T * 128:(T + 1) * 128, :]), writes=[Bb[k][0]], dma=True)
        S.add("act", DMA(bufs[k][1], p1[T * 128:(T + 1) * 128, :]), writes=[Bb[k][1]], dma=True)
        S.add("dve", lambda e, k=k: e.tensor_tensor(out=bufs[k][0], in0=bufs[k][0], in1=bufs[k][1], op=ALU.add),
              reads=[Bb[k][0], Bb[k][1]], writes=[Bb[k][0]])
        S.add("sp", DMA(out[T * 128:(T + 1) * 128, :], bufs[k][0]), reads=[Bb[k][0]], dma=True)
    S.barrier()
    S.emit()
    return nc


def kernel(**inp):
    inp = {k: np.asarray(v) for k, v in inp.items()}
    nb = inp["x"].shape[0]
    maps = [fused_host_inputs(inp, b) for b in range(nb)]
    nc = build_fused(maps[0])
    res = run_bass_kernel_spmd(nc, maps, core_ids=list(range(nb)))
    return np.stack([res.results[b]["out"] for b in range(nb)]).astype(np.float32)
```

```python
import numpy as np
import concourse.bass as bass
import concourse.mybir as mybir
from concourse.bass_utils import run_bass_kernel_spmd

F32 = mybir.dt.float32
BF16 = mybir.dt.bfloat16
I32 = mybir.dt.int32
U32 = mybir.dt.uint32
ALU = mybir.AluOpType
AF = mybir.ActivationFunctionType
AX = mybir.AxisListType

ENGS = ("pe", "act", "dve", "pool", "sp")


class Buf:
    __slots__ = ("name", "writers", "readers")

    def __init__(self, name=""):
        self.name = name
        self.writers = []
        self.readers = []


class Op:
    __slots__ = ("eng", "fn", "deps", "is_dma", "marked", "cnt", "sem", "semval", "seq")

    def __init__(self, eng, fn, is_dma):
        self.eng = eng
        self.fn = fn
        self.deps = set()
        self.is_dma = is_dma
        self.marked = False
        self.cnt = 0
        self.sem = None
        self.semval = 0


def _prune(lst, op):
    if not op.is_dma:
        lst[:] = [o for o in lst if o.is_dma or o.eng != op.eng]
    lst.append(op)


class Sched:
    def __init__(self, nc, n_dma_sems=None):
        self.nc = nc
        self.ops = {e: [] for e in ENGS}
        self.dma_ops = {e: [] for e in ENGS}
        self.n_dma_sems = n_dma_sems or {"sp": 24, "act": 8, "pool": 16}
        self.seq = 0
        self.since_barrier = []
        self.regcache = {}

    def getreg(self, eng, val):
        if val not in self.regcache:
            self.regcache[val] = eng.to_reg(val)
        return self.regcache[val]

    def add(self, eng, fn, reads=(), writes=(), pwrites=(), dma=False, extra_deps=()):
        op = Op(eng, fn, dma)
        op.seq = self.seq
        self.seq += 1
        deps = set(extra_deps)
        raw = set()
        for b in reads:
            for w in b.writers:
                deps.add(w)
                raw.add(w)
        for b in writes:
            deps.update(b.writers)
            deps.update(b.readers)
        for b in pwrites:
            deps.update(b.readers)
            for w in b.writers:
                if w.is_dma or w.eng != eng:
                    deps.add(w)
        for d in deps:
            if d is op:
                continue
            if d.is_dma or dma:
                op.deps.add(d)
            elif d.eng != eng:
                op.deps.add(d)
            elif d in raw and eng != "pe":
                op.deps.add(d)
        for b in reads:
            _prune(b.readers, op)
        for b in writes:
            b.writers = [op]
            b.readers = []
        for b in pwrites:
            _prune(b.writers, op)
        if dma:
            q = self.dma_ops[eng]
            K = self.n_dma_sems[eng]
            if len(q) >= K:
                op.deps.add(q[len(q) - K])
            op.cnt = len(q)
            q.append(op)
        self.ops[eng].append(op)
        self.since_barrier.append(op)
        return op

    def barrier(self):
        last = {}
        dl = {}
        for o in self.since_barrier:
            if o.is_dma:
                dl[(o.eng, o.cnt % self.n_dma_sems[o.eng])] = o
            else:
                last[o.eng] = o
        deps = list(last.values()) + list(dl.values())
        self.since_barrier = []
        new = []
        for e in ENGS:
            op = self.add(e, lambda eng: eng.nop(), extra_deps=[d for d in deps])
            new.append(op)
        return new

    def emit(self):
        nc = self.nc
        for e in ENGS:
            for op in self.ops[e]:
                for d in op.deps:
                    d.marked = True
        eng_sem = {e: nc.alloc_semaphore(f"s_{e}") for e in ENGS}
        dma_sems = {e: [nc.alloc_semaphore(f"d_{e}{i}") for i in range(self.n_dma_sems.get(e, 0))]
                    for e in ENGS}
        for e in ENGS:
            c = 0
            for op in self.ops[e]:
                if op.is_dma:
                    continue
                if op.marked:
                    c += 1
                    op.cnt = c
            K = self.n_dma_sems.get(e, 0)
            for i, op in enumerate(self.dma_ops[e]):
                op.sem = dma_sems[e][i % K]
                op.semval = 16 * (i // K + 1)
        self.max_cnt = {e: max([o.cnt for o in self.ops[e]] + [0]) for e in ENGS}

        def run(e, eng):
            seen = {}
            for op in self.ops[e]:
                for d in sorted(op.deps, key=lambda o: o.seq):
                    if d.is_dma:
                        key, val, sem = ("d", id(d.sem)), d.semval, d.sem
                    else:
                        key, val, sem = ("e", d.eng), d.cnt, eng_sem[d.eng]
                    if seen.get(key, 0) < val:
                        eng.wait_ge(sem, val)
                        seen[key] = val
                ins = op.fn(eng)
                if op.is_dma:
                    ins.then_inc(op.sem, 16)
                elif op.marked:
                    ins.then_inc(eng_sem[e], 1)

        with nc.Block() as block:
            @block.tensor
            def _(eng):
                run("pe", eng)

            @block.scalar
            def _(eng):
                run("act", eng)

            @block.vector
            def _(eng):
                run("dve", eng)

            @block.gpsimd
            def _(eng):
                run("pool", eng)

            @block.sync
            def _(eng):
                run("sp", eng)


class Arena:
    def __init__(self, nc, name, nbytes):
        self.t = nc.alloc_sbuf_tensor(name, [128, nbytes // 4], F32)
        self.nbytes = nbytes
        self.off = 0
        self.marks = []

    def alloc(self, shape, dtype, parts=128):
        esz = {F32: 4, BF16: 2, I32: 4, U32: 4}[dtype]
        n = int(np.prod(shape))
        nb = (n * esz + 31) // 32 * 32
        assert self.off + nb <= self.nbytes, f"arena overflow {self.off}+{nb}>{self.nbytes}"
        a = self.t[0:parts, self.off // 4:(self.off + nb) // 4]
        self.off += nb
        if dtype != F32:
            a = a.bitcast(dtype)
        a = a[:, 0:n]
        if len(shape) > 1:
            names = " ".join(f"d{i}" for i in range(len(shape)))
            kw = {f"d{i}": s for i, s in enumerate(shape)}
            a = a.rearrange(f"p ({names}) -> p {names}", **kw)
        return a

    def mark(self):
        self.marks.append(self.off)

    def release(self):
        self.off = self.marks.pop()


NTOK = 8192
D = 1024
NE = 8
CAP = 1024
DF = 2048
ROWW = 524
EPS = 1e-6


class Ctx:
    def __init__(self):
        self.nc = bass.Bass("TRN2", target_bir_lowering=False)
        self.S = Sched(self.nc)
        self.A = Arena(self.nc, "arena", 204 * 1024)
        self.psall = self.nc.alloc_psum_tensor("psall", [128, 4096], F32)
        self.dram = {}

    def scratch(self, name, shape, dt):
        key = (name, tuple(shape), str(dt))
        if key not in self.dram:
            self.dram[key] = self.nc.dram_tensor(name, list(shape), dt).ap()
        return self.dram[key]

    def reset(self):
        self.A.off = 0
        self.A.marks = []


def DMA(out, in_, **kw):
    return lambda e: e.dma_start(out=out, in_=in_, **kw)


def make_ident(S, A):
    identf = A.alloc([128], F32)
    identb = A.alloc([128], BF16)
    bf, bb = Buf("identf"), Buf("identb")
    S.add("pool", lambda e: e.memset(identf, 0.0), writes=[bf])
    S.add("pool", lambda e: e.affine_select(out=identf, in_=identf, pattern=[[-1, 128]],
                                            compare_op=ALU.not_equal, fill=1.0, base=0,
                                            channel_multiplier=1), reads=[bf], writes=[bf])
    S.add("dve", lambda e: e.tensor_copy(out=identb, in_=identf), reads=[bf], writes=[bb])
    return identf, identb, bf, bb


def build_moe(ctx=None, io=None, zero_init=True, wr_sets=None, sets=None):
    fused16 = sets is not None
    sets = sets if fused16 else (0,)
    RW = 532 if fused16 else ROWW
    GW = 16 if fused16 else 8
    TOKW = 512 + GW
    if ctx is None:
        nc = bass.Bass("TRN2", target_bir_lowering=False)
        x = nc.dram_tensor("x", [NTOK, D], F32, kind="ExternalInput").ap()
        gn = nc.dram_tensor("gn", [1, D], F32, kind="ExternalInput").ap()
        wr = nc.dram_tensor("wr", [D, 16], F32, kind="ExternalInput").ap()
        wg = nc.dram_tensor("wg", [NE, D, DF], F32, kind="ExternalInput").ap()
        wu = nc.dram_tensor("wu", [NE, D, DF], F32, kind="ExternalInput").ap()
        wd = nc.dram_tensor("wd", [NE, DF, D], F32, kind="ExternalInput").ap()
        cG = nc.dram_tensor("cG", [128, 128], F32, kind="ExternalInput").ap()
        cL = nc.dram_tensor("cL", [128, 128], F32, kind="ExternalInput").ap()
        part = nc.dram_tensor("part", [NTOK, D], F32, kind="ExternalOutput").ap()
        hbuf = nc.dram_tensor("hbuf", [NTOK, ROWW], F32).ap()
        affd = nc.dram_tensor("affd", [16, NTOK], F32).ap()
        xs = [nc.dram_tensor(f"xs{i}", [CAP, ROWW], F32).ap() for i in range(NE)]
        S = Sched(nc)
        A = Arena(nc, "arena", 204 * 1024)
        ps = [nc.alloc_psum_tensor(f"ps{i}", [128, 512], F32) for i in range(8)]
        part_full, part_eoff = part, 0
    else:
        nc, S, A = ctx.nc, ctx.S, ctx.A
        ctx.reset()
        x, gn, wr, wg, wu, wd, cG, cL, part = (io[k] for k in ("x", "gn", "wr", "wg", "wu", "wd", "cG", "cL", "part"))
        part_full, part_eoff = io.get("part_full", part), io.get("part_eoff", 0)
        hbuf = ctx.scratch("hbuf16", [NTOK, RW], F32)
        affd = ctx.scratch("affd", [16, NTOK], F32)
        xs = [ctx.scratch(f"xs16_{i}", [CAP, RW], F32) for i in range(NE)]
        ps = [ctx.psall[:, i * 512:(i + 1) * 512] for i in range(8)]
    PB = [Buf(f"ps{i}") for i in range(8)]

    identf, identb, Bif, Bib = make_ident(S, A)
    G_sb = A.alloc([128], F32)
    L_sb = A.alloc([128], F32)
    gb = A.alloc([D], F32)
    gcol = A.alloc([8], F32)
    wr_sb = A.alloc([8, 16], F32)
    zero = A.alloc([D], F32)
    tokid = A.alloc([64], I32)
    Bc = Buf("consts")
    S.add("sp", DMA(G_sb, cG), pwrites=[Bc], dma=True)
    S.add("sp", DMA(L_sb, cL), pwrites=[Bc], dma=True)
    S.add("sp", DMA(gb, gn.partition_broadcast(128)), pwrites=[Bc], dma=True)
    S.add("sp", DMA(gcol, gn.rearrange("o (c p) -> p (o c)", p=128), allow_slow_non_contiguous=True),
          pwrites=[Bc], dma=True)
    wrv = wr.rearrange("(c p) e -> p c e", p=128)
    if wr_sets is None:
        S.add("sp", DMA(wr_sb, wrv), pwrites=[Bc], dma=True)
    else:
        own, oth = wr_sets
        S.add("sp", DMA(wr_sb[:, :, 0:8], wrv[:, :, own:own + 8]), pwrites=[Bc], dma=True)
        S.add("sp", DMA(wr_sb[:, :, 8:16], wrv[:, :, oth:oth + 8]), pwrites=[Bc], dma=True)
    S.add("dve", lambda e: e.memset(zero, 0.0), pwrites=[Bc])
    S.add("pool", lambda e: e.iota(tokid, pattern=[[128, 64]], base=0, channel_multiplier=1), pwrites=[Bc])
    Bpart = Buf("part")
    if zero_init:
        for T in range(NTOK // 128):
            S.add("sp", DMA(part[T * 128:(T + 1) * 128, :], zero), reads=[Bc], pwrites=[Bpart], dma=True)

    posT = A.alloc([4, 128], I32)
    A.mark()
    affT_sb = A.alloc([NTOK], F32)
    BaffT = Buf("affT")
    xts = [A.alloc([D], F32) for _ in range(3)]
    Bxt = [Buf() for _ in range(3)]
    xns = [A.alloc([D], F32) for _ in range(2)]
    Bxn = [Buf() for _ in range(2)]
    hTs = [A.alloc([8, 128], F32) for _ in range(2)]
    BhT = [Buf() for _ in range(2)]
    rts = [A.alloc([RW], F32) for _ in range(3)]
    Brt = [Buf() for _ in range(3)]
    sts = [A.alloc([8], F32) for _ in range(4)]
    Bst = [Buf() for _ in range(4)]
    exs = [A.alloc([16], F32) for _ in range(2)]
    Bex = [Buf() for _ in range(2)]
    affs = [A.alloc([16], F32) for _ in range(2)]
    Baf = [Buf() for _ in range(2)]
    junk = A.alloc([D], BF16)
    Bjunk = Buf()
    for T in range(NTOK // 128):
        xt, bxt = xts[T % 3], Bxt[T % 3]
        xn, bxn = xns[T % 2], Bxn[T % 2]
        hT, bhT = hTs[T % 2], BhT[T % 2]
        rt, brt = rts[T % 3], Brt[T % 3]
        st, bst = sts[T % 4], Bst[T % 4]
        ex, bex = exs[T % 2], Bex[T % 2]
        af, baf = affs[T % 2], Baf[T % 2]
        rt_bf = rt.bitcast(BF16)
        rt_i = rt.bitcast(I32)
        S.add("sp", DMA(xt, x[T * 128:(T + 1) * 128, :]), writes=[bxt], dma=True)
        S.add("act", lambda e, xt=xt, st=st: e.activation(out=junk, in_=xt, func=AF.Square, scale=1.0 / 32,
                                                          accum_out=st[:, 0:1]),
              reads=[bxt], writes=[Bjunk, bst])
        S.add("act", lambda e, st=st: e.activation(out=st[:, 1:2], in_=st[:, 0:1], func=AF.Sqrt, bias=EPS, scale=1.0),
              reads=[bst], pwrites=[bst])
        S.add("dve", lambda e, st=st: e.reciprocal(out=st[:, 2:3], in_=st[:, 1:2]), reads=[bst], pwrites=[bst])
        S.add("dve", lambda e, xn=xn, xt=xt, st=st: e.tensor_scalar_mul(out=xn, in0=xt, scalar1=st[:, 2:3]),
              reads=[bxt, bst], writes=[bxn])
        b0 = 2 * (T % 2)
        for c in range(8):
            pb = b0 + c // 4
            S.add("pe", lambda e, pb=pb, c=c, xn=xn: e.transpose(out=ps[pb][:, (c % 4) * 128:(c % 4 + 1) * 128],
                                                                 in_=xn[:, c * 128:(c + 1) * 128], identity=identf),
                  reads=[bxn, Bif], writes=[PB[pb]] if c % 4 == 0 else [], pwrites=[] if c % 4 == 0 else [PB[pb]])
        for c in range(8):
            pb = b0 + c // 4
            src = ps[pb][:, (c % 4) * 128:(c % 4 + 1) * 128]
            if c % 2 == 0:
                S.add("act", lambda e, src=src, c=c, hT=hT: e.activation(out=hT[:, c, :], in_=src, func=AF.Copy,
                                                                         scale=gcol[:, c:c + 1]),
                      reads=[PB[pb], Bc], writes=[bhT] if c == 0 else [], pwrites=[] if c == 0 else [bhT])
            else:
                S.add("dve", lambda e, src=src, c=c, hT=hT: e.tensor_scalar_mul(out=hT[:, c, :], in0=src,
                                                                                scalar1=gcol[:, c:c + 1]),
                      reads=[PB[pb], Bc], pwrites=[bhT])
        pl = 4 + T % 2
        for c in range(8):
            S.add("pe", lambda e, pl=pl, c=c, hT=hT: e.matmul(ps[pl][:, 0:16], lhsT=hT[:, c, :], rhs=wr_sb[:, c, :],
                                                              start=(c == 0), stop=(c == 7)),
                  reads=[bhT, Bc], writes=[PB[pl]] if c == 0 else [], pwrites=[] if c == 0 else [PB[pl]])
        S.add("dve", lambda e, pl=pl, st=st: e.reduce_max(out=st[:, 3:4], in_=ps[pl][:, 0:16], axis=AX.X),
              reads=[PB[pl]], pwrites=[bst])
        S.add("dve", lambda e, st=st: e.tensor_scalar_mul(out=st[:, 4:5], in0=st[:, 3:4], scalar1=-1.0),
              reads=[bst], pwrites=[bst])
        S.add("act", lambda e, pl=pl, st=st, ex=ex: e.activation(out=ex, in_=ps[pl][:, 0:16], func=AF.Exp,
                                                                 bias=st[:, 4:5], scale=1.0, accum_out=st[:, 5:6]),
              reads=[PB[pl], bst], writes=[bex], pwrites=[bst])
        S.add("dve", lambda e, st=st: e.reciprocal(out=st[:, 6:7], in_=st[:, 5:6]), reads=[bst], pwrites=[bst])
        S.add("dve", lambda e, af=af, ex=ex, st=st: e.tensor_scalar_mul(out=af, in0=ex, scalar1=st[:, 6:7]),
              reads=[bex, bst], writes=[baf])
        S.add("dve", lambda e, rt_bf=rt_bf, xn=xn: e.tensor_tensor(out=rt_bf[:, 0:D], in0=xn, in1=gb, op=ALU.mult),
              reads=[bxn, Bc], writes=[brt])
        S.add("act", lambda e, rt=rt, af=af: e.copy(out=rt[:, 512:512 + GW], in_=af[:, 0:GW]), reads=[baf], pwrites=[brt])
        S.add("dve", lambda e, rt_i=rt_i, T=T: e.tensor_copy(out=rt_i[:, TOKW:TOKW + 1], in_=tokid[:, T:T + 1]),
              reads=[Bc], pwrites=[brt])
        S.add("act", DMA(hbuf[T * 128:(T + 1) * 128, 0:TOKW + 1], rt[:, 0:TOKW + 1]), reads=[brt], dma=True)
        pa = 6 + T % 2
        S.add("pe", lambda e, pa=pa, af=af: e.transpose(out=ps[pa][0:16, 0:128], in_=af, identity=identf),
              reads=[baf, Bif], writes=[PB[pa]])
        S.add("act", lambda e, pa=pa, T=T: e.copy(out=affT_sb[0:16, T * 128:(T + 1) * 128], in_=ps[pa][0:16, 0:128]),
              reads=[PB[pa]], pwrites=[BaffT])

    Baffd = Buf("affd")
    S.add("sp", DMA(affd, affT_sb[0:16, :]), reads=[BaffT], writes=[Baffd], dma=True)
    S.barrier()
    A.release()
    for s_ in sets:
        eofs = 8 * s_ if fused16 else 0
        gofs = eofs
        prev_sc = []
        A.mark()
        a_sb = A.alloc([512], F32)
        Ba = Buf("a")
        S.add("sp", DMA(a_sb, affd[eofs:eofs + 8, :].rearrange("e (c j) -> (e c) j", c=16)), reads=[Baffd], writes=[Ba], dma=True)
        msk = A.alloc([512], F32)
        Bm = Buf("msk")
        sc = A.alloc([8], F32)
        Bs = Buf("sc")
        S.add("dve", lambda e: e.memset(sc, 0.0), writes=[Bs])
        pt = 0
        for k in range(30):
            dl = 2.0 ** -(k + 1)
            S.add("dve", lambda e, dl=dl: e.tensor_scalar_add(out=sc[:, 1:2], in0=sc[:, 0:1], scalar1=dl),
                  reads=[Bs], pwrites=[Bs])
            S.add("dve", lambda e: e.tensor_single_scalar(out=msk, in_=a_sb, scalar=sc[:, 1:2], op=ALU.is_ge),
                  reads=[Ba, Bs], writes=[Bm])
            S.add("dve", lambda e: e.reduce_sum(out=sc[:, 2:3], in_=msk, axis=AX.X), reads=[Bm], pwrites=[Bs])
            S.add("pe", lambda e: e.matmul(ps[pt][:, 0:1], lhsT=G_sb, rhs=sc[:, 2:3], start=True, stop=True),
                  reads=[Bs, Bc], writes=[PB[pt]])
            S.add("dve", lambda e: e.tensor_single_scalar(out=sc[:, 3:4], in_=ps[pt][:, 0:1], scalar=CAP - 0.5, op=ALU.is_ge),
                  reads=[PB[pt]], pwrites=[Bs])
            S.add("dve", lambda e, dl=dl: e.scalar_tensor_tensor(out=sc[:, 0:1], in0=sc[:, 3:4], scalar=dl, in1=sc[:, 0:1],
                                                                op0=ALU.mult, op1=ALU.add),
                  reads=[Bs], pwrites=[Bs])

        ones = A.alloc([512], F32)
        incl = A.alloc([512], F32)
        posm = A.alloc([512], F32)
        Bo, Bi, Bp, BpT = Buf(), Buf(), Buf(), Buf()
        S.add("dve", lambda e: e.memset(ones, 1.0), writes=[Bo])
        S.add("dve", lambda e: e.tensor_single_scalar(out=msk, in_=a_sb, scalar=sc[:, 0:1], op=ALU.is_ge),
              reads=[Ba, Bs], writes=[Bm])
        S.add("dve", lambda e: e.reduce_sum(out=sc[:, 2:3], in_=msk, axis=AX.X), reads=[Bm], pwrites=[Bs])
        S.add("pe", lambda e: e.matmul(ps[pt][:, 0:1], lhsT=L_sb, rhs=sc[:, 2:3], start=True, stop=True),
              reads=[Bs, Bc], writes=[PB[pt]])
        S.add("dve", lambda e: e.tensor_copy(out=sc[:, 4:5], in_=ps[pt][:, 0:1]), reads=[PB[pt]], pwrites=[Bs])
        S.add("dve", lambda e: e.tensor_tensor_scan(out=incl, data0=ones, data1=msk, initial=0.0, op0=ALU.mult, op1=ALU.add),
              reads=[Bo, Bm], writes=[Bi])
        S.add("dve", lambda e: e.tensor_tensor(out=incl, in0=incl, in1=msk, op=ALU.subtract), reads=[Bi, Bm], writes=[Bi])
        S.add("dve", lambda e: e.tensor_scalar(out=posm, in0=incl, scalar1=sc[:, 4:5], scalar2=-4096.0,
                                               op0=ALU.add, op1=ALU.add), reads=[Bi, Bs], writes=[Bp])
        S.add("dve", lambda e: e.tensor_tensor(out=posm, in0=posm, in1=msk, op=ALU.mult), reads=[Bp, Bm], writes=[Bp])
        S.add("dve", lambda e: e.tensor_scalar_add(out=posm, in0=posm, scalar1=4096.0), reads=[Bp], writes=[Bp])
        pq = 1
        for jb in range(4):
            S.add("pe", lambda e, jb=jb: e.transpose(out=ps[pq][:, jb * 128:(jb + 1) * 128],
                                                     in_=posm[:, jb * 128:(jb + 1) * 128], identity=identf),
                  reads=[Bp, Bif], writes=[PB[pq]] if jb == 0 else [], pwrites=[] if jb == 0 else [PB[pq]])
        S.add("dve", lambda e: e.tensor_copy(out=posT, in_=ps[pq][:, 0:512].rearrange("p (a b) -> p a b", a=4)),
              reads=[PB[pq]], writes=[BpT])

        S.barrier()
        A.release()
        A.mark()
        import os as _os
        if _os.environ.get("MOE_STOP") == "3":
            S.emit()
            return nc
        NRT = 6
        rtl = [A.alloc([RW], F32) for _ in range(NRT)]
        Brl = [Buf() for _ in range(NRT)]
        Bxs = [Buf(f"xs{e}") for e in range(NE)]
        NSTG = 4
        stg = [A.alloc([2048], F32) for _ in range(NSTG)]
        Bstg = [Buf() for _ in range(NSTG)]
        wgb = [A.alloc([8, 256], BF16) for _ in range(2)]
        wub = [A.alloc([8, 256], BF16) for _ in range(2)]
        Bwg = [Buf() for _ in range(2)]
        Bwu = [Buf() for _ in range(2)]
        wdb = A.alloc([16, D], BF16)
        Bwd = [Buf() for _ in range(8)]
        xsb = A.alloc([8, RW], F32)
        Bxsb = Buf("xsb")
        XT = A.alloc([8, CAP], BF16)
        BXT = Buf("XT")
        AT = A.alloc([16, CAP], BF16)
        BAT = [Buf() for _ in range(16)]
        sgs = [A.alloc([512], F32) for _ in range(2)]
        Bsg = [Buf() for _ in range(2)]
        yts = [A.alloc([D], F32) for _ in range(3)]
        Byt = [Buf() for _ in range(3)]
        pgs = [A.alloc([D], F32) for _ in range(3)]
        Bpg = [Buf() for _ in range(3)]
        pg_i = [0]
        stg_i = [0]
        rt_i_ = [0]
        cast_i = [0]
        sg_i = [0]
        yt_i = [0]
        prev_sc = []

        def cast(out, in_, reads, writes):
            eng = "dve" if cast_i[0] % 3 != 2 else "act"
            cast_i[0] += 1
            if eng == "dve":
                S.add("dve", lambda e: e.tensor_copy(out=out, in_=in_), reads=reads, writes=writes)
            else:
                S.add("act", lambda e: e.copy(out=out, in_=in_), reads=reads, writes=writes)

        def scatter_rows(e_):
            for T in range(NTOK // 128):
                c, jb = T // 4, T % 4
                k = rt_i_[0] % NRT
                rt_i_[0] += 1
                S.add("act", DMA(rtl[k][:, 0:TOKW + 1], hbuf[T * 128:(T + 1) * 128, 0:TOKW + 1]), writes=[Brl[k]], dma=True)
                idx = posT[:, jb, e_ * 16 + c:e_ * 16 + c + 1]
                S.add("pool", lambda e, k=k, idx=idx, e_=e_: e.indirect_dma_start(
                    out=xs[e_], out_offset=bass.IndirectOffsetOnAxis(ap=idx, axis=0), in_=rtl[k], in_offset=None,
                    bounds_check=S.getreg(e, CAP - 1), oob_is_err=False), reads=[Brl[k], BpT], pwrites=[Bxs[e_]], dma=True)

        scatter_rows(0)
        for e_ in range(NE):
            if e_ + 1 < NE:
                scatter_rows(e_ + 1)
            S.add("sp", DMA(xsb, xs[e_].rearrange("(t p) c -> p t c", p=128)), reads=[Bxs[e_]], writes=[Bxsb], dma=True)
            xsb_bf = xsb.rearrange("p t c -> p (t c)").bitcast(BF16).rearrange("p (t c) -> p t c", t=8)
            xsb_i = xsb.rearrange("p t c -> p (t c)").bitcast(I32).rearrange("p (t c) -> p t c", t=8)
            for dc in range(8):
                for half in range(2):
                    pb = 6 + half
                    psb = ps[pb].bitcast(BF16)
                    for t4 in range(4):
                        t = half * 4 + t4
                        S.add("pe", lambda e, psb=psb, t4=t4, t=t, dc=dc: e.transpose(
                            out=psb[:, t4 * 128:(t4 + 1) * 128], in_=xsb_bf[:, t, dc * 128:(dc + 1) * 128], identity=identb),
                            reads=[Bxsb, Bib], writes=[PB[pb]] if t4 == 0 else [], pwrites=[] if t4 == 0 else [PB[pb]])
                    eng = "act" if (dc + half) % 2 == 0 else "dve"
                    dst = XT[:, dc, half * 512:(half + 1) * 512]
                    if eng == "act":
                        S.add("act", lambda e, psb=psb, dst=dst: e.copy(out=dst, in_=psb[:, 0:512]),
                              reads=[PB[pb]], writes=[BXT] if (dc == 0 and half == 0) else [],
                              pwrites=[] if (dc == 0 and half == 0) else [BXT])
                    else:
                        S.add("dve", lambda e, psb=psb, dst=dst: e.tensor_copy(out=dst, in_=psb[:, 0:512]),
                              reads=[PB[pb]], pwrites=[BXT])
            for fq in range(8):
                wsel = fq % 2
                for (wsrc, wdst, bw) in ((wg, wgb[wsel], Bwg[wsel]), (wu, wub[wsel], Bwu[wsel])):
                    k = stg_i[0] % NSTG
                    stg_i[0] += 1
                    sv = stg[k].rearrange("p (c f) -> p c f", c=8)
                    S.add("sp", DMA(sv, wsrc[eofs + e_].rearrange("(c p) f -> p c f", p=128)[:, :, fq * 256:(fq + 1) * 256]),
                          writes=[Bstg[k]], dma=True)
                    cast(wdst, sv, [Bstg[k]], [bw])
                for fl in range(2):
                    fc = fq * 2 + fl
                    bset = 0 if fc % 2 == 0 else 3
                    gb_ = [bset, bset + 1]
                    ub_ = [bset + 2, (bset + 3) if bset == 0 else 0]
                    if bset == 3:
                        gb_ = [4, 5]
                        ub_ = [6, 7]
                    else:
                        gb_ = [0, 1]
                        ub_ = [2, 3]
                    for half in range(2):
                        for (wsb, bw, bank) in ((wgb[wsel], Bwg[wsel], gb_[half]), (wub[wsel], Bwu[wsel], ub_[half])):
                            for dc in range(8):
                                S.add("pe", lambda e, wsb=wsb, bank=bank, dc=dc, fl=fl, half=half: e.matmul(
                                    ps[bank][:, 0:512], lhsT=wsb[:, dc, fl * 128:(fl + 1) * 128],
                                    rhs=XT[:, dc, half * 512:(half + 1) * 512], start=(dc == 0), stop=(dc == 7)),
                                    reads=[bw, BXT], writes=[PB[bank]] if dc == 0 else [],
                                    pwrites=[] if dc == 0 else [PB[bank]])
                        k = sg_i[0] % 2
                        sg_i[0] += 1
                        S.add("act", lambda e, k=k, bank=gb_[half]: e.activation(out=sgs[k], in_=ps[bank][:, 0:512], func=AF.Silu),
                              reads=[PB[gb_[half]]], writes=[Bsg[k]])
                        S.add("dve", lambda e, k=k, bank=ub_[half], fc=fc, half=half: e.tensor_tensor(
                            out=AT[:, fc, half * 512:(half + 1) * 512], in0=ps[bank][:, 0:512], in1=sgs[k], op=ALU.mult),
                            reads=[PB[ub_[half]], Bsg[k]], writes=[BAT[fc]] if half == 0 else [],
                            pwrites=[] if half == 0 else [BAT[fc]])
            for q in range(8):
                k = stg_i[0] % NSTG
                stg_i[0] += 1
                sv = stg[k].rearrange("p (c f) -> p c f", c=2)
                S.add("sp", DMA(sv, wd[eofs + e_].rearrange("(c p) f -> p c f", p=128)[:, q * 2:(q + 1) * 2, :]),
                      writes=[Bstg[k]], dma=True)
                cast(wdb[:, q * 2:(q + 1) * 2, :], sv, [Bstg[k]], [Bwd[q]])
            cur_sc = []
            for t in range(8):
                k = yt_i[0] % 3
                yt_i[0] += 1
                for half in range(2):
                    bank = (t * 2 + half) % 6
                    for fc in range(16):
                        S.add("pe", lambda e, bank=bank, fc=fc, t=t, half=half: e.matmul(
                            ps[bank][:, 0:512], lhsT=AT[:, fc, t * 128:(t + 1) * 128],
                            rhs=wdb[:, fc, half * 512:(half + 1) * 512], start=(fc == 0), stop=(fc == 15)),
                            reads=[BAT[fc], Bwd[fc // 2]], writes=[PB[bank]] if fc == 0 else [],
                            pwrites=[] if fc == 0 else [PB[bank]])
                    gsc = xsb[:, t, 512 + gofs + e_:513 + gofs + e_]
                    if half == 0:
                        S.add("act", lambda e, k=k, bank=bank, gsc=gsc: e.activation(
                            out=yts[k][:, 0:512], in_=ps[bank][:, 0:512], func=AF.Copy, scale=gsc),
                            reads=[PB[bank], Bxsb], writes=[Byt[k]])
                    else:
                        S.add("dve", lambda e, k=k, bank=bank, gsc=gsc: e.tensor_scalar_mul(
                            out=yts[k][:, 512:1024], in0=ps[bank][:, 0:512], scalar1=gsc),
                            reads=[PB[bank], Bxsb], pwrites=[Byt[k]])
                idx = xsb_i[:, t, TOKW:TOKW + 1]
                kg_ = pg_i[0] % 3
                pg_i[0] += 1
                S.add("pool", lambda e, kg_=kg_, idx=idx: e.indirect_dma_start(
                    out=pgs[kg_], out_offset=None, in_=part_full,
                    in_offset=bass.IndirectOffsetOnAxis(ap=idx, axis=0), element_offset=part_eoff),
                    reads=[Bxsb, Bpart], writes=[Bpg[kg_]], dma=True, extra_deps=prev_sc)
                S.add("dve", lambda e, k=k, kg_=kg_: e.tensor_tensor(out=yts[k], in0=yts[k], in1=pgs[kg_], op=ALU.add),
                      reads=[Byt[k], Bpg[kg_]], writes=[Byt[k]])
                op = S.add("pool", lambda e, k=k, idx=idx: e.indirect_dma_start(
                    out=part_full, out_offset=bass.IndirectOffsetOnAxis(ap=idx, axis=0), in_=yts[k], in_offset=None,
                    element_offset=part_eoff),
                    reads=[Byt[k], Bxsb], pwrites=[Bpart], dma=True)
                cur_sc.append(op)
            prev_sc = cur_sc
        S.barrier()
        A.release()
    S.barrier()
    if ctx is not None:
        return None
    S.emit()
    return nc


import math


def t5_bucket_np(rel):
    rel = np.asarray(rel, np.int64)
    ret = np.where(rel > 0, 16, 0)
    n = np.abs(rel)
    nf = np.maximum(n, 1).astype(np.float32)
    large = 8 + (np.log(nf / np.float32(8)) / np.float32(math.log(128.0)) * np.float32(8)).astype(np.int32)
    large = np.minimum(large, 15)
    return ret + np.where(n < 8, n, large)


def band_consts(cfg):
    ohb = []
    meta = []
    for (d, radius, wins) in cfg["branches"]:
        for (off, bases) in wins:
            for base in bases:
                rho = np.arange(128)
                rel = rho + off - base + 128
                valid = np.abs(rel) <= radius
                bk = t5_bucket_np(rel * d)
                m = np.zeros((32, 128), np.float32)
                m[bk[valid], rho[valid]] = 1.0
                ohb.append(m)
    Z = np.zeros((128, 512), np.float32)
    Z[np.arange(128), np.arange(128) + 128] = 1.0
    p = np.arange(128)
    G = (p[:, None] // 64 == p[None, :] // 64).astype(np.float32)
    return np.stack(ohb), Z, G


def rope_consts():
    half = 32
    inv = (10000.0 ** (-np.arange(0, half, 2, dtype=np.float32) / half)).astype(np.float32)
    t = np.arange(8192)
    row, col = t // 64, t % 64
    cos = np.zeros((64, 8192), np.float32)
    sin = np.zeros((64, 8192), np.float32)
    for f in range(64):
        pos = row if f < 32 else col
        ang = pos.astype(np.float32) * inv[f % 16]
        cos[f] = np.cos(ang)
        sin[f] = np.sin(ang)
    cos = np.concatenate([cos, cos], 0)
    sin = np.concatenate([sin, sin], 0)
    Rl = np.zeros((128, 128), np.float32)
    for f0 in range(0, 128, 32):
        for i in range(16):
            Rl[f0 + 16 + i, f0 + i] = -1.0
            Rl[f0 + i, f0 + 16 + i] = 1.0
    return cos, sin, Rl


def build_attn(cfg, ctx=None, io=None):
    io = io or {}
    if ctx is None:
        nc = bass.Bass("TRN2", target_bir_lowering=False)
    else:
        nc = ctx.nc
        ctx.reset()
    n_ext, own_off, n_own, CH, n_add = cfg["n_ext"], cfg["own_off"], cfg["n_own"], cfg["CH"], cfg["n_add"]
    nh, nkv = cfg["nh"], cfg["nkv"]
    npair, ngrp = nh // 2, nkv // 2
    qmap, kmap, kvq = cfg["qmap"], cfg["kmap"], cfg["kv_of_q"]
    dense = cfg["dense"]
    sinkf = cfg["sink"]
    branches = cfg["branches"]
    npass = sum(len(b) for (_, _, wins) in branches for (_, b) in wins)
    nbw = sum(len(wins) for (_, _, wins) in branches)

    def din(name, shape, dt=F32):
        if name in io:
            return io[name]
        return nc.dram_tensor(name, list(shape), dt, kind="ExternalInput").ap()

    xa = din("xa", [n_ext, D])
    adds = [din(f"pa{i}", [n_ext, D]) for i in range(n_add)]
    gn = din("gn", [1, D])
    wqb = din("wqb", [D, npair * 128])
    wkb = din("wkb", [D, ngrp * 128])
    wvb = din("wvb", [D, nkv * 64])
    wo = din("wo", [D, D])
    gqb = din("gqb", [128, 1])
    gkb = din("gkb", [128, 1])
    gq1 = din("gq1", [1, 64])
    gk1 = din("gk1", [1, 64])
    tblT = din("tblT", [32, nh])
    tblB = din("tblB", [1, nh * 32])
    sink1 = din("sink1", [1, nh])
    cOH = din("cOH", [npass, 32, 128])
    cZ = din("cZ", [128, 512])
    cG = din("cG", [128, 128])
    valid2 = din("valid2", [128, n_ext // 128])
    if dense:
        xk = din("xk", [NTOK, D])
        wqd = din("wqd", [D, 512])
        wkd = din("wkd", [D, 128])
        wvd = din("wvd", [D, 128])
        gqd = din("gqd", [128, 1])
        gkd = din("gkd", [128, 1])
        gqd1 = din("gqd1", [1, 64])
        gkd1 = din("gkd1", [1, 64])
        cosk = din("cosk", [128, NTOK])
        sink_ = din("sink_", [128, NTOK])
        cosq = din("cosq", [128, n_own])
        sinq = din("sinq", [128, n_own])
        cR = din("cR", [128, 128])
    VW = nkv * 65
    OW = nh * 65
    if ctx is None:
        out = nc.dram_tensor("out", [n_own, D], F32, kind="ExternalOutput").ap()
        vbuf = nc.dram_tensor("vbuf", [n_ext, VW], BF16).ap()
        obuf = [nc.dram_tensor(f"obuf{i}", [n_own, OW], F32).ap() for i in range(len(branches))]
        xsum = nc.dram_tensor("xsum", [n_ext, D], F32).ap() if n_add else xa
        S = Sched(nc)
        A = Arena(nc, "arena", 204 * 1024)
        psall = nc.alloc_psum_tensor("psall", [128, 4096], F32)
    else:
        out = io["out"]
        tg = cfg["name"]
        vbuf = ctx.scratch(tg + "vbuf", [n_ext, VW], BF16)
        obuf = [ctx.scratch(tg + f"obuf{i}", [n_own, OW], F32) for i in range(len(branches))]
        xsum = ctx.scratch(tg + "xsum", [n_ext, D], F32) if n_add else xa
        S, A, psall = ctx.S, ctx.A, ctx.psall
    ps = [psall[:, i * 512:(i + 1) * 512] for i in range(8)]
    psb = [p.bitcast(BF16) for p in ps]
    PB = [Buf(f"ps{i}") for i in range(8)]

    identf, identb, Bif, Bib = make_ident(S, A)
    Bc = Buf("consts")

    def cload(shape, src, dt=F32, eng="sp", **kw):
        t = A.alloc(shape, dt)
        S.add(eng, DMA(t, src, **kw), pwrites=[Bc], dma=True)
        return t

    def wload(src, cols):
        t = A.alloc([8, cols], BF16)
        v = src.rearrange("(c p) f -> p c f", p=128)
        for c in range(8):
            S.add("pool", DMA(t[:, c, :], v[:, c, :]), pwrites=[Bc], dma=True)
        return t

    gcol = cload([8], gn.rearrange("o (c p) -> p (o c)", p=128), allow_slow_non_contiguous=True)
    G_bf = cload([128], cG, BF16, "pool")
    Z_bf = cload([512], cZ, BF16, "pool")
    wo_bf = wload(wo, D)
    val_sb = cload([n_ext // 128], valid2)
    gq_c = cload([1], gqb)
    gk_c = cload([1], gkb)
    gqB = cload([64], gq1.partition_broadcast(128))
    gkB = cload([64], gk1.partition_broadcast(128))
    tbB = cload([nh, 32], tblB.partition_broadcast(128))
    skB = cload([nh], sink1.partition_broadcast(128))
    tT = A.alloc([nh], F32)
    S.add("sp", DMA(tT[0:32, :], tblT), pwrites=[Bc], dma=True)
    cst = A.alloc([16], F32)
    MbB = A.alloc([nh], F32)
    SHB = A.alloc([nh], F32)
    skE = A.alloc([nh], F32)
    Bk = Buf("cst")

    def mk_mqk(gA, gB, o):
        S.add("dve", lambda e: e.reduce_max(out=cst[:, o:o + 1], in_=gA, axis=AX.X, apply_absolute_value=True),
              reads=[Bc], pwrites=[Bk])
        S.add("dve", lambda e: e.reduce_max(out=cst[:, o + 1:o + 2], in_=gB, axis=AX.X, apply_absolute_value=True),
              reads=[Bc], pwrites=[Bk])
        S.add("dve", lambda e: e.tensor_tensor(out=cst[:, o + 2:o + 3], in0=cst[:, o:o + 1], in1=cst[:, o + 1:o + 2],
                                               op=ALU.mult), reads=[Bk], pwrites=[Bk])
        S.add("dve", lambda e: e.tensor_scalar_mul(out=cst[:, o + 2:o + 3], in0=cst[:, o + 2:o + 3], scalar1=8.0),
              reads=[Bk], pwrites=[Bk])
        S.add("dve", lambda e: e.tensor_scalar_mul(out=cst[:, o + 3:o + 4], in0=cst[:, o + 2:o + 3], scalar1=-1.0),
              reads=[Bk], pwrites=[Bk])

    mk_mqk(gqB, gkB, 0)
    BMb, BskE = Buf("Mb"), Buf("skE")
    S.add("dve", lambda e: e.tensor_reduce(out=MbB, in_=tbB, axis=AX.X, op=ALU.max), reads=[Bc], writes=[BMb])
    BSH = Buf("SH")
    if sinkf:
        S.add("dve", lambda e: e.tensor_tensor(out=SHB, in0=skB, in1=MbB, op=ALU.subtract), reads=[Bc, BMb], writes=[BSH])
        S.add("dve", lambda e: e.tensor_scalar_add(out=SHB, in0=SHB, scalar1=cst[:, 3:4]), reads=[BSH, Bk], writes=[BSH])
        S.add("dve", lambda e: e.tensor_scalar_max(out=SHB, in0=SHB, scalar1=0.0), reads=[BSH], writes=[BSH])
        S.add("dve", lambda e: e.tensor_tensor(out=SHB, in0=SHB, in1=MbB, op=ALU.add), reads=[BSH, BMb], writes=[BSH])
        S.add("dve", lambda e: e.tensor_tensor(out=skE, in0=skB, in1=SHB, op=ALU.subtract), reads=[BSH, Bc],
              writes=[BskE])
        S.add("act", lambda e: e.activation(out=skE, in_=skE, func=AF.Exp, bias=cst[:, 3:4], scale=1.0),
              reads=[Bk, BskE], writes=[BskE])
    else:
        S.add("dve", lambda e: e.tensor_copy(out=SHB, in_=MbB), reads=[BMb], writes=[BSH])
    etb = A.alloc([nh], BF16)
    etf = A.alloc([nh], F32)
    Bet = Buf("etb")
    S.add("dve", lambda e: e.tensor_tensor(out=etf[0:32, :], in0=tT[0:32, :], in1=SHB[0:32, :], op=ALU.subtract),
          reads=[Bc, BSH], writes=[Bet])
    S.add("act", lambda e: e.activation(out=etb[0:32, :], in_=etf[0:32, :], func=AF.Exp), reads=[Bet], writes=[Bet])

    A.mark()
    oh_bf = A.alloc([npass, 128], BF16)
    S.add("pool", DMA(oh_bf[0:32], cOH.rearrange("n b r -> b n r")), pwrites=[Bc], dma=True)
    tab = A.alloc([npass, nh], BF16)
    Btab = Buf("tab")
    for pi in range(npass):
        S.add("pe", lambda e, pi=pi: e.matmul(ps[4][:, 0:nh], lhsT=oh_bf[0:32, pi, :], rhs=etb[0:32, :], start=True, stop=True),
              reads=[Bc, Bet], writes=[PB[4]])
        S.add("dve", lambda e, pi=pi: e.tensor_copy(out=tab[:, pi, :], in_=ps[4][:, 0:nh]), reads=[PB[4]], pwrites=[Btab])
    A.release()
    A.mark()
    oh_bf = A.alloc([npass, 128], BF16)
    tab = A.alloc([npass, nh], BF16)
    EB = [A.alloc([nh, 128], BF16) for _ in range(nbw)]
    BEB = [Buf(f"EB{i}") for i in range(nbw)]
    nbk = (128 * nh) // 512
    pi = 0
    bw = 0
    for (d, radius, wins) in branches:
        for (off, bases) in wins:
            for qq in range(128):
                for j, base in enumerate(bases):
                    o0 = qq * nh
                    S.add("pe", lambda e, o0=o0, base=base, qq=qq, pj=pi + j, j=j, nb=len(bases): e.matmul(
                        psall[:, o0:o0 + nh], lhsT=Z_bf[:, base - qq:base - qq + 128], rhs=tab[:, pj, :],
                        start=(j == 0), stop=(j == nb - 1)),
                        reads=[Bc, Btab], writes=[PB[k_] for k_ in range(nbk)] if (qq == 0 and j == 0) else [],
                        pwrites=[] if (qq == 0 and j == 0) else [PB[0]])
            S.add("dve", lambda e, bw=bw: e.tensor_copy(
                out=EB[bw], in_=psall[:, 0:128 * nh].rearrange("p (q h) -> p h q", h=nh)),
                reads=[PB[k_] for k_ in range(nbk)], writes=[BEB[bw]])
            pi += len(bases)
            bw += 1

    if cfg.get("stop") == "eb":
        S.barrier()
        S.emit()
        return nc
    NCH = CH // 128
    wq_bf = wload(wqb, npair * 128)
    wk_bf = wload(wkb, ngrp * 128)
    wv_bf = wload(wvb, nkv * 64)
    Kb = A.alloc([ngrp, n_ext], BF16)
    Qb = A.alloc([npair, n_own], BF16)
    BKb, BQb = Buf("Kb"), Buf("Qb")
    A.mark()
    xts = [A.alloc([D], F32) for _ in range(2)]
    Bxt = [Buf() for _ in range(2)]
    xad = [A.alloc([D], F32) for _ in range(2)]
    Bxa = [Buf() for _ in range(2)]
    xnb = [A.alloc([D], BF16) for _ in range(2)]
    Bxn = [Buf() for _ in range(2)]
    hTs = [A.alloc([8, CH], BF16) for _ in range(2)]
    BhT = [Buf() for _ in range(2)]
    sts = [A.alloc([4], F32) for _ in range(4)]
    Bst = [Buf() for _ in range(4)]
    junk = A.alloc([D], BF16)
    Bjunk = Buf()
    sqs = [A.alloc([CH], BF16) for _ in range(2)]
    Bsq = [Buf() for _ in range(2)]
    sds = [A.alloc([CH], F32) for _ in range(2)]
    Bsd = [Buf() for _ in range(2)]
    vsts = [A.alloc([nkv, 65], BF16) for _ in range(2)]
    Bvs = [Buf() for _ in range(2)]
    ctr = {"t": 0, "n": 0, "v": 0}
    Bvbuf = Buf("vbuf")
    Bxsum = Buf("xsum")

    def make_hT(src, tile0, slot, write_sum=False):
        hT, bh = hTs[slot], BhT[slot]
        for ti in range(NCH):
            T = tile0 + ti
            k = ctr["t"] % 2
            ctr["t"] += 1
            xt, bxt = xts[k], Bxt[k]
            st, bst = sts[ctr["t"] % 4], Bst[ctr["t"] % 4]
            S.add("sp", DMA(xt, src[T * 128:(T + 1) * 128, :]), writes=[bxt], dma=True)
            if write_sum and n_add:
                for ai, ad in enumerate(adds):
                    xa_, bxa = xad[ai % 2], Bxa[ai % 2]
                    S.add("act", DMA(xa_, ad[T * 128:(T + 1) * 128, :]), writes=[bxa], dma=True)
                    S.add("dve", lambda e, xt=xt, xa_=xa_: e.tensor_tensor(out=xt, in0=xt, in1=xa_, op=ALU.add),
                          reads=[bxt, bxa], writes=[bxt])
                S.add("sp", DMA(xsum[T * 128:(T + 1) * 128, :], xt), reads=[bxt], pwrites=[Bxsum], dma=True)
            S.add("act", lambda e, xt=xt, st=st, junk=junk: e.activation(out=junk, in_=xt, func=AF.Square, scale=1.0 / 32,
                                                                         accum_out=st[:, 0:1]),
                  reads=[bxt], writes=[Bjunk, bst])
            S.add("act", lambda e, st=st: e.activation(out=st[:, 1:2], in_=st[:, 0:1], func=AF.Sqrt, bias=EPS, scale=1.0),
                  reads=[bst], pwrites=[bst])
            S.add("dve", lambda e, st=st: e.reciprocal(out=st[:, 2:3], in_=st[:, 1:2]), reads=[bst], pwrites=[bst])
            xn, bxn = xnb[k], Bxn[k]
            S.add("dve", lambda e, xn=xn, xt=xt, st=st: e.tensor_scalar_mul(out=xn, in0=xt, scalar1=st[:, 2:3]),
                  reads=[bxt, bst], writes=[bxn])
            pb = k
            for c in range(8):
                S.add("pe", lambda e, pb=pb, c=c, xn=xn: e.transpose(out=psb[pb][:, c * 128:(c + 1) * 128],
                                                                     in_=xn[:, c * 128:(c + 1) * 128], identity=identb),
                      reads=[bxn, Bib], writes=[PB[pb]] if c == 0 else [], pwrites=[] if c == 0 else [PB[pb]])
            for c in range(8):
                src_ = psb[pb][:, c * 128:(c + 1) * 128]
                dst = hT[:, c, ti * 128:(ti + 1) * 128]
                first = (ti == 0 and c == 0)
                if c % 2 == 0:
                    S.add("act", lambda e, src_=src_, dst=dst, c=c: e.activation(out=dst, in_=src_, func=AF.Copy,
                                                                                 scale=gcol[:, c:c + 1]),
                          reads=[PB[pb], Bc], writes=[bh] if first else [], pwrites=[] if first else [bh])
                else:
                    S.add("dve", lambda e, src_=src_, dst=dst, c=c: e.tensor_scalar_mul(out=dst, in0=src_,
                                                                                        scalar1=gcol[:, c:c + 1]),
                          reads=[PB[pb], Bc], pwrites=[bh])
        return hT, bh

    def proj_fm(hT, bh, w_bf, col0, gcolumn, dst, bdst, rope=None):
        k = ctr["n"] % 2
        ctr["n"] += 1
        pq, pss = 2 + k, 4 + k
        for c in range(8):
            S.add("pe", lambda e, c=c: e.matmul(ps[pq][:, 0:CH], lhsT=w_bf[:, c, col0:col0 + 128], rhs=hT[:, c, :],
                                                start=(c == 0), stop=(c == 7)),
                  reads=[bh, Bc], writes=[PB[pq]] if c == 0 else [], pwrites=[] if c == 0 else [PB[pq]])
        sq, bsq, sd, bsd = sqs[k], Bsq[k], sds[k], Bsd[k]
        S.add("act", lambda e: e.activation(out=sq, in_=ps[pq][:, 0:CH], func=AF.Square), reads=[PB[pq]], writes=[bsq])
        S.add("pe", lambda e: e.matmul(ps[pss][:, 0:CH], lhsT=G_bf, rhs=sq, start=True, stop=True),
              reads=[bsq, Bc], writes=[PB[pss]])
        S.add("act", lambda e: e.activation(out=sd, in_=ps[pss][:, 0:CH], func=AF.Sqrt, bias=EPS, scale=1.0 / 64),
              reads=[PB[pss]], writes=[bsd])
        S.add("dve", lambda e: e.reciprocal(out=sd, in_=sd), reads=[bsd], writes=[bsd])
        if rope is None:
            S.add("dve", lambda e: e.scalar_tensor_tensor(out=dst, in0=ps[pq][:, 0:CH], scalar=gcolumn, in1=sd,
                                                          op0=ALU.mult, op1=ALU.mult),
                  reads=[PB[pq], bsd, Bc], pwrites=[bdst])
        else:
            cos_d, sin_d, c0 = rope
            qn, bqn = rp["qn"][k], rp["Bqn"][k]
            cs, bcs, sn, bsn = rp["cs"][k], rp["Bcs"][k], rp["sn"][k], rp["Bsn"][k]
            S.add("sp", DMA(cs, cos_d[:, c0:c0 + CH]), writes=[bcs], dma=True)
            S.add("sp", DMA(sn, sin_d[:, c0:c0 + CH]), writes=[bsn], dma=True)
            S.add("dve", lambda e: e.scalar_tensor_tensor(out=qn, in0=ps[pq][:, 0:CH], scalar=gcolumn, in1=sd,
                                                          op0=ALU.mult, op1=ALU.mult),
                  reads=[PB[pq], bsd, Bc], writes=[bqn])
            S.add("pe", lambda e: e.matmul(ps[6][:, 0:CH], lhsT=rp["R"], rhs=qn, start=True, stop=True),
                  reads=[bqn, Bc], writes=[PB[6]])
            S.add("dve", lambda e: e.tensor_tensor(out=cs, in0=qn, in1=cs, op=ALU.mult), reads=[bqn, bcs], writes=[bcs])
            S.add("dve", lambda e: e.tensor_tensor(out=sn, in0=ps[6][:, 0:CH], in1=sn, op=ALU.mult),
                  reads=[PB[6], bsn], writes=[bsn])
            S.add("dve", lambda e: e.tensor_tensor(out=dst, in0=cs, in1=sn, op=ALU.add), reads=[bcs, bsn], pwrites=[bdst])

    def proj_v(hT, bh, ti, w_bf, ncols, dst_fn):
        for c in range(8):
            S.add("pe", lambda e, c=c: e.matmul(ps[7][:, 0:ncols], lhsT=hT[:, c, ti * 128:(ti + 1) * 128],
                                                rhs=w_bf[:, c, 0:ncols], start=(c == 0), stop=(c == 7)),
                  reads=[bh, Bc], writes=[PB[7]] if c == 0 else [], pwrites=[] if c == 0 else [PB[7]])
        dst_fn(ps[7][:, 0:ncols])

    own_lo, own_hi = own_off, own_off + n_own
    for ch in range(n_ext // CH):
        hT, bh = make_hT(xa, ch * NCH, ch % 2, write_sum=True)
        for g in range(ngrp):
            proj_fm(hT, bh, wk_bf, g * 128, gk_c[:, 0:1], Kb[:, g, ch * CH:(ch + 1) * CH], BKb)
        for ti in range(NCH):
            T = ch * NCH + ti
            k = ctr["v"] % 2
            ctr["v"] += 1
            vs, bvs = vsts[k], Bvs[k]

            def put(psv, vs=vs, bvs=bvs, T=T):
                S.add("act", lambda e: e.copy(out=vs[:, :, 0:64], in_=psv.rearrange("p (h d) -> p h d", h=nkv)),
                      reads=[PB[7]], writes=[bvs])
                S.add("dve", lambda e: e.tensor_copy(out=vs[:, :, 64],
                                                     in_=val_sb[:, T:T + 1].to_broadcast([128, nkv])),
                      reads=[Bc], pwrites=[bvs])
                S.add("act", DMA(vbuf[T * 128:(T + 1) * 128, :], vs.rearrange("p h d -> p (h d)")), reads=[bvs],
                      pwrites=[Bvbuf], dma=True)
            proj_v(hT, bh, ti, wv_bf, nkv * 64, put)
        if own_lo <= ch * CH < own_hi:
            for p_ in range(npair):
                proj_fm(hT, bh, wq_bf, p_ * 128, gq_c[:, 0:1],
                        Qb[:, p_, ch * CH - own_lo:(ch + 1) * CH - own_lo], BQb)
    S.barrier()
    A.release()
    if cfg.get("stop") == "pass1":
        S.barrier()
        S.emit()
        return nc

    A.mark()
    NW = 6
    vws = [A.alloc([nkv, 65], BF16) for _ in range(NW)]
    Bvw = [Buf() for _ in range(NW)]
    pts = [A.alloc([512], BF16) for _ in range(3)]
    Bpt = [Buf() for _ in range(3)]
    PTs = [A.alloc([512], BF16) for _ in range(3)]
    BPT = [Buf() for _ in range(3)]
    osts = [A.alloc([nh, 65], F32) for _ in range(2)]
    Bos = [Buf() for _ in range(2)]
    Bob = [Buf(f"obuf{i}") for i in range(len(branches))]
    ngr = nh // 4
    cw = {"w": 0, "p": 0, "s": 0, "o": 0, "t": 0}
    bw0 = 0
    units = []
    for bi, (d, radius, wins) in enumerate(branches):
        nt = n_own // d // 128
        for r in range(d):
            for i in range(nt):
                if cfg.get("band_limit") is not None and cw["t"] >= cfg["band_limit"]:
                    break
                cw["t"] += 1
                tile = dict(bi=bi, d=d, wins=wins, s0=own_off // d + 128 * i, q0=r + d * 128 * i, r=r, bw0=bw0,
                            ko=cw["t"] % 2)
                for g in range(ngr):
                    for wi in range(len(wins)):
                        units.append((tile, g, wi))
        bw0 += len(wins)
    LAG = 2
    ust = {}
    for step in range(len(units) + LAG):
        if step < len(units):
            tile, g, wi = units[step]
            d, wins, q0 = tile["d"], tile["wins"], tile["q0"]
            if g == 0 and wi == 0:
                vt = []
                for (off, bases) in wins:
                    kw_ = cw["w"] % NW
                    cw["w"] += 1
                    u0 = tile["r"] + d * (tile["s0"] + off)
                    S.add("sp", DMA(vws[kw_].rearrange("p h d -> p (h d)"), vbuf[u0:u0 + d * 127 + 1:d, :]),
                          reads=[Bvbuf], writes=[Bvw[kw_]], dma=True)
                    vt.append((kw_, u0))
                tile["vt"] = vt
            if wi == 0:
                tile[("ob", g)] = 4 + cw["o"] % 4
                cw["o"] += 1
            kw_, u0 = tile["vt"][wi]
            sb = cw["s"] % 4
            cw["s"] += 1
            for j in range(4):
                hq = g * 4 + j
                pr, hf = qmap[hq]
                kg, khf = kmap[kvq[hq]]
                assert hf == khf
                S.add("pe", lambda e, sb=sb, j=j, hf=hf, kg=kg, pr=pr, u0=u0, q0=q0, d=d: e.matmul(
                    ps[sb][:, j * 128:(j + 1) * 128],
                    lhsT=Kb[hf * 64:(hf + 1) * 64, kg, u0:u0 + d * 127 + 1:d],
                    rhs=Qb[hf * 64:(hf + 1) * 64, pr, q0:q0 + d * 127 + 1:d], start=True, stop=True),
                    reads=[BKb, BQb], writes=[PB[sb]] if j == 0 else [], pwrites=[] if j == 0 else [PB[sb]])
            kp = cw["p"] % 3
            cw["p"] += 1
            S.add("act", lambda e, kp=kp, sb=sb: e.activation(out=pts[kp], in_=ps[sb][:, 0:512], func=AF.Exp,
                                                              bias=cst[:, 3:4], scale=0.125),
                  reads=[PB[sb], Bk], writes=[Bpt[kp]])
            ebv = EB[tile["bw0"] + wi][:, g * 4:(g + 1) * 4, :].rearrange("p h q -> p (h q)")
            S.add("dve", lambda e, kp=kp, ebv=ebv: e.tensor_tensor(out=PTs[kp], in0=pts[kp], in1=ebv, op=ALU.mult),
                  reads=[Bpt[kp], BEB[tile["bw0"] + wi]], writes=[BPT[kp]])
            ust[step] = kp
        if step >= LAG:
            tile, g, wi = units[step - LAG]
            kp = ust.pop(step - LAG)
            d, wins, q0 = tile["d"], tile["wins"], tile["q0"]
            kw_, u0 = tile["vt"][wi]
            ob = tile[("ob", g)]
            ost, bos = osts[tile["ko"]], Bos[tile["ko"]]
            for j in range(4):
                hq = g * 4 + j
                S.add("pe", lambda e, ob=ob, j=j, kp=kp, kw_=kw_, kv=kvq[hq], wi=wi, nw=len(wins): e.matmul(
                    ps[ob][:, j * 65:(j + 1) * 65], lhsT=PTs[kp][:, j * 128:(j + 1) * 128],
                    rhs=vws[kw_][:, kv, :], start=(wi == 0 and j == 0), stop=(wi == nw - 1)),
                    reads=[BPT[kp], Bvw[kw_]],
                    writes=[PB[ob]] if (wi == 0 and j == 0) else [],
                    pwrites=[] if (wi == 0 and j == 0) else [PB[ob]])
            if wi == len(wins) - 1:
                S.add("act", lambda e, ob=ob, g=g, ost=ost: e.copy(
                    out=ost[:, g * 4:(g + 1) * 4, :].rearrange("p h d -> p (h d)"), in_=ps[ob][:, 0:260]),
                    reads=[PB[ob]], writes=[bos] if g == 0 else [], pwrites=[] if g == 0 else [bos])
                if g == ngr - 1:
                    S.add("act", DMA(obuf[tile["bi"]][q0:q0 + d * 127 + 1:d, :], ost.rearrange("p h d -> p (h d)")),
                          reads=[bos], pwrites=[Bob[tile["bi"]]], dma=True)
    S.barrier()
    A.release()
    A.release()

    if cfg.get("stop") == "band":
        S.barrier()
        S.emit()
        return nc
    OA = A.alloc([4, D], BF16)
    BOA = Buf("OA")
    if dense:
        wqd_bf = wload(wqd, 512)
        wkd_bf = wload(wkd, 128)
        wvd_bf = wload(wvd, 128)
        gqd_c = cload([1], gqd)
        gkd_c = cload([1], gkd)
        gqdB = cload([64], gqd1.partition_broadcast(128))
        gkdB = cload([64], gkd1.partition_broadcast(128))
        R_bf = cload([128], cR, BF16, "pool")
        mk_mqk(gqdB, gkdB, 4)
        Kd = A.alloc([NTOK], BF16)
        Qd = A.alloc([4, n_own], BF16)
        V1d = A.alloc([NTOK // 128, 2, 65], BF16)
        BKd, BQd, BVd = Buf("Kd"), Buf("Qd"), Buf("V1d")
        S.add("dve", lambda e: e.memset(V1d[:, :, :, 64], 1.0), pwrites=[BVd])
        A.mark()
        xts[:] = [A.alloc([D], F32) for _ in range(2)]
        xnb[:] = [A.alloc([D], BF16) for _ in range(2)]
        hTs[:] = [A.alloc([8, CH], BF16) for _ in range(2)]
        sts[:] = [A.alloc([4], F32) for _ in range(4)]
        junk = A.alloc([D], BF16)
        sqs[:] = [A.alloc([CH], BF16) for _ in range(2)]
        sds[:] = [A.alloc([CH], F32) for _ in range(2)]
        rp = {"qn": [A.alloc([CH], BF16) for _ in range(2)], "Bqn": [Buf(), Buf()],
              "cs": [A.alloc([CH], F32) for _ in range(2)], "Bcs": [Buf(), Buf()],
              "sn": [A.alloc([CH], F32) for _ in range(2)], "Bsn": [Buf(), Buf()], "R": R_bf}
        saved_nadd = n_add
        for ch in range(n_own // CH):
            hT, bh = make_hT(xsum, (own_off + ch * CH) // 128, ch % 2)
            for p_ in range(4):
                proj_fm(hT, bh, wqd_bf, p_ * 128, gqd_c[:, 0:1], Qd[:, p_, ch * CH:(ch + 1) * CH], BQd,
                        rope=(cosq, sinq, ch * CH))
        for ch in range(NTOK // CH):
            hT, bh = make_hT(xk, ch * NCH, ch % 2)
            proj_fm(hT, bh, wkd_bf, 0, gkd_c[:, 0:1], Kd[:, ch * CH:(ch + 1) * CH], BKd, rope=(cosk, sink_, ch * CH))
            for ti in range(NCH):
                T = ch * NCH + ti

                def putd(psv, T=T):
                    S.add("act", lambda e: e.copy(out=V1d[:, T, :, 0:64], in_=psv.rearrange("p (h d) -> p h d", h=2)),
                          reads=[PB[7]], pwrites=[BVd])
                proj_v(hT, bh, ti, wvd_bf, 128, putd)
        S.barrier()
        A.release()

    if cfg.get("stop") == "densep":
        S.barrier()
        S.emit()
        return nc
    A.mark()
    pts2 = [A.alloc([512], BF16) for _ in range(4)]
    Bpt2 = [Buf() for _ in range(4)]
    obl = [[A.alloc([nh, 65], F32) for _ in range(len(branches))] for _ in range(2)]
    Bol = [[Buf() for _ in range(len(branches))] for _ in range(2)]
    rcs = [A.alloc([16], F32) for _ in range(2)]
    Brc = [Buf() for _ in range(2)]
    OT = A.alloc([8, 512], BF16)
    BOT = Buf("OT")
    xrs = [A.alloc([D], F32) for _ in range(2)]
    Bxr = [Buf() for _ in range(2)]
    cd = {"s": 0, "p": 0, "o": 0, "l": 0, "x": 0}
    band_col0 = cfg["band_col0"]
    for chq in range(n_own // 512):
        first_oa = [True]

        def oa_w():
            if first_oa[0]:
                first_oa[0] = False
                return dict(writes=[BOA])
            return dict(pwrites=[BOA])
        if dense:
            NK = NTOK // 128
            units = [(hq, kt) for hq in range(8) for kt in range(NK)]
            LAG = 2
            ust = {}
            obh = {}
            for step in range(len(units) + LAG):
                if step < len(units):
                    hq, kt = units[step]
                    pr, hf = hq % 4, hq // 4
                    if kt == 0:
                        obh[hq] = 4 + cd["o"] % 2
                        cd["o"] += 1
                    sb = cd["s"] % 4
                    cd["s"] += 1
                    S.add("pe", lambda e, sb=sb, hf=hf, pr=pr, kt=kt, chq=chq: e.matmul(
                        ps[sb][:, 0:512], lhsT=Kd[hf * 64:(hf + 1) * 64, kt * 128:(kt + 1) * 128],
                        rhs=Qd[hf * 64:(hf + 1) * 64, pr, chq * 512:(chq + 1) * 512], start=True, stop=True),
                        reads=[BKd, BQd], writes=[PB[sb]])
                    kp = cd["p"] % 4
                    cd["p"] += 1
                    S.add("act", lambda e, kp=kp, sb=sb: e.activation(out=pts2[kp], in_=ps[sb][:, 0:512], func=AF.Exp,
                                                                      bias=cst[:, 7:8], scale=0.125),
                          reads=[PB[sb], Bk], writes=[Bpt2[kp]])
                    ust[step] = kp
                if step >= LAG:
                    hq, kt = units[step - LAG]
                    pr, hf = hq % 4, hq // 4
                    kp = ust.pop(step - LAG)
                    ob = obh[hq]
                    for sub in range(4):
                        S.add("pe", lambda e, ob=ob, sub=sub, kp=kp, kt=kt, hf=hf: e.matmul(
                            ps[ob][:, sub * 65:(sub + 1) * 65], lhsT=pts2[kp][:, sub * 128:(sub + 1) * 128],
                            rhs=V1d[:, kt, hf, :], start=(kt == 0 and sub == 0), stop=(kt == NK - 1)),
                            reads=[Bpt2[kp], BVd],
                            writes=[PB[ob]] if (kt == 0 and sub == 0) else [],
                            pwrites=[] if (kt == 0 and sub == 0) else [PB[ob]])
                    if kt == NK - 1:
                        kr = cd["l"] % 2
                        cd["l"] += 1
                        ov = ps[ob][:, 0:260].rearrange("p (s d) -> p s d", s=4)
                        S.add("dve", lambda e, kr=kr, ov=ov: e.reciprocal(out=rcs[kr][:, 0:4], in_=ov[:, :, 64]),
                              reads=[PB[ob]], writes=[Brc[kr]])
                        col = cfg["dense_col0"] + hq * 64
                        S.add("dve", lambda e, kr=kr, ov=ov, col=col: e.tensor_tensor(
                            out=OA[:, :, col:col + 64], in0=ov[:, :, 0:64],
                            in1=rcs[kr][:, 0:4].unsqueeze(2).to_broadcast([128, 4, 64]), op=ALU.mult),
                            reads=[PB[ob], Brc[kr]], **oa_w())
        for sub in range(4):
            t = chq * 4 + sub
            ko = cd["x"] % 2
            cd["x"] += 1
            for bi in range(len(branches)):
                S.add("sp", DMA(obl[ko][bi].rearrange("p h d -> p (h d)"), obuf[bi][t * 128:(t + 1) * 128, :]),
                      reads=[Bob[bi]], writes=[Bol[ko][bi]], dma=True)
            o0, b0_ = obl[ko][0], Bol[ko][0]
            for bi in range(1, len(branches)):
                S.add("dve", lambda e, o0=o0, o1=obl[ko][bi]: e.tensor_tensor(out=o0, in0=o0, in1=o1, op=ALU.add),
                      reads=[b0_, Bol[ko][bi]], writes=[b0_])
            if sinkf:
                S.add("dve", lambda e, o0=o0: e.tensor_tensor(out=o0[:, :, 64], in0=o0[:, :, 64], in1=skE, op=ALU.add),
                      reads=[b0_, BSH, BskE], writes=[b0_])
            kr = cd["l"] % 2
            cd["l"] += 1
            S.add("dve", lambda e, kr=kr, o0=o0: e.reciprocal(out=rcs[kr][:, 0:nh], in_=o0[:, :, 64]),
                  reads=[b0_], writes=[Brc[kr]])
            S.add("dve", lambda e, kr=kr, o0=o0, sub=sub: e.tensor_tensor(
                out=OA[:, sub, band_col0:band_col0 + nh * 64].rearrange("p (h d) -> p h d", h=nh), in0=o0[:, :, 0:64],
                in1=rcs[kr][:, 0:nh].unsqueeze(2).to_broadcast([128, nh, 64]), op=ALU.mult),
                reads=[b0_, Brc[kr]], **oa_w())
        for c8 in range(8):
            pb = 6 + c8 % 2
            for sub in range(4):
                S.add("pe", lambda e, pb=pb, sub=sub, c8=c8: e.transpose(
                    out=psb[pb][:, sub * 128:(sub + 1) * 128], in_=OA[:, sub, c8 * 128:(c8 + 1) * 128], identity=identb),
                    reads=[BOA, Bib], writes=[PB[pb]] if sub == 0 else [], pwrites=[] if sub == 0 else [PB[pb]])
            if c8 % 2 == 0:
                S.add("act", lambda e, pb=pb, c8=c8: e.copy(out=OT[:, c8, :], in_=psb[pb][:, 0:512]),
                      reads=[PB[pb]], writes=[BOT] if c8 == 0 else [], pwrites=[] if c8 == 0 else [BOT])
            else:
                S.add("dve", lambda e, pb=pb, c8=c8: e.tensor_copy(out=OT[:, c8, :], in_=psb[pb][:, 0:512]),
                      reads=[PB[pb]], pwrites=[BOT])
        for sub in range(4):
            t = chq * 4 + sub
            kx = (chq * 4 + sub) % 2
            xr, bxr = xrs[kx], Bxr[kx]
            S.add("sp", DMA(xr, xsum[own_off + t * 128:own_off + (t + 1) * 128, :]), reads=[Bxsum], writes=[bxr], dma=True)
            for hf in range(2):
                pb = (sub * 2 + hf) % 4
                for c8 in range(8):
                    S.add("pe", lambda e, pb=pb, c8=c8, sub=sub, hf=hf: e.matmul(
                        ps[pb][:, 0:512], lhsT=OT[:, c8, sub * 128:(sub + 1) * 128],
                        rhs=wo_bf[:, c8, hf * 512:(hf + 1) * 512], start=(c8 == 0), stop=(c8 == 7)),
                        reads=[BOT, Bc], writes=[PB[pb]] if c8 == 0 else [], pwrites=[] if c8 == 0 else [PB[pb]])
                S.add("dve", lambda e, pb=pb, xr=xr, hf=hf: e.tensor_tensor(
                    out=xr[:, hf * 512:(hf + 1) * 512], in0=ps[pb][:, 0:512], in1=xr[:, hf * 512:(hf + 1) * 512],
                    op=ALU.add), reads=[PB[pb], bxr], writes=[bxr])
            S.add("sp", DMA(out[t * 128:(t + 1) * 128, :], xr), reads=[bxr], dma=True)
    S.barrier()
    if ctx is not None:
        return None
    S.emit()
    return nc


def build_add3(n_rows):
    nc = bass.Bass("TRN2", target_bir_lowering=False)
    srcs = [nc.dram_tensor(n, [n_rows, D], F32, kind="ExternalInput").ap() for n in ("xa", "p0", "p1")]
    out = nc.dram_tensor("out", [n_rows, D], F32, kind="ExternalOutput").ap()
    S = Sched(nc)
    A = Arena(nc, "arena", 64 * 1024)
    bufs = [[A.alloc([D], F32) for _ in range(3)] for _ in range(3)]
    Bb = [[Buf() for _ in range(3)] for _ in range(3)]
    for T in range(n_rows // 128):
        k = T % 3
        for i, eng in enumerate(("sp", "act", "sp")):
            S.add(eng, DMA(bufs[k][i], srcs[i][T * 128:(T + 1) * 128, :]), writes=[Bb[k][i]], dma=True)
        S.add("dve", lambda e, k=k: e.tensor_tensor(out=bufs[k][0], in0=bufs[k][0], in1=bufs[k][1], op=ALU.add),
              reads=[Bb[k][0], Bb[k][1]], writes=[Bb[k][0]])
        S.add("dve", lambda e, k=k: e.tensor_tensor(out=bufs[k][0], in0=bufs[k][0], in1=bufs[k][2], op=ALU.add),
              reads=[Bb[k][0], Bb[k][2]], writes=[Bb[k][0]])
        S.add("act", DMA(out[T * 128:(T + 1) * 128, :], bufs[k][0]), reads=[Bb[k][0]], dma=True)
    S.barrier()
    S.emit()
    return nc


A_WINS = [(-64, [128]), (64, [255])]
CFG_L0 = dict(name="l0", n_ext=6144, own_off=1024, n_own=4096, CH=512, n_add=0, nh=8, nkv=8,
              qmap=[(h % 4, h // 4) for h in range(8)], kmap=[(h % 4, h // 4) for h in range(8)],
              kv_of_q=list(range(8)),
              branches=[(1, 64, A_WINS), (4, 64, A_WINS), (16, 64, A_WINS)],
              sink=False, dense=True, band_col0=0, dense_col0=512)
CFG_L1 = dict(name="l1", n_ext=4352, own_off=128, n_own=4096, CH=128, n_add=2, nh=16, nkv=4,
              qmap=[(((hq // 4) // 2) * 4 + hq % 4, (hq // 4) % 2) for hq in range(16)],
              kmap=[(i // 2, i % 2) for i in range(4)],
              kv_of_q=[hq // 4 for hq in range(16)],
              branches=[(1, 128, [(-128, [128]), (0, [255, 127]), (128, [255])])],
              sink=True, dense=None, band_col0=0, dense_col0=0)


def _ext_rows(a, t0, own_off, n_ext):
    lo = t0 - own_off
    out = np.zeros((n_ext, a.shape[1]), a.dtype)
    s, e = max(lo, 0), min(lo + n_ext, a.shape[0])
    out[s - lo:e - lo] = a[s:e]
    return out


def _valid2(t0, own_off, n_ext):
    u = np.arange(n_ext) + t0 - own_off
    v = ((u >= 0) & (u < NTOK)).astype(np.float32)
    return np.ascontiguousarray(v.reshape(n_ext // 128, 128).T)


def attn_inputs(cfg, consts, half, xa_full, adds_full, gn, wq, wk, wv, wo, gq, gk, tbl, sink, dense_in=None):
    t0 = half * 4096
    n_ext, own_off = cfg["n_ext"], cfg["own_off"]
    nh = cfg["nh"]
    npair = nh // 2
    wqb = np.zeros((D, npair * 128), np.float32)
    for hq, (p_, s_) in enumerate(cfg["qmap"]):
        wqb[:, p_ * 128 + s_ * 64:p_ * 128 + s_ * 64 + 64] = wq[:, hq * 64:(hq + 1) * 64]
    wkb = np.zeros((D, (cfg["nkv"] // 2) * 128), np.float32)
    for kv, (g_, s_) in enumerate(cfg["kmap"]):
        wkb[:, g_ * 128 + s_ * 64:g_ * 128 + s_ * 64 + 64] = wk[:, kv * 64:(kv + 1) * 64]
    ohb, Z, G = consts
    m = dict(xa=_ext_rows(xa_full, t0, own_off, n_ext), gn=gn[None], wqb=wqb, wkb=wkb,
             wvb=np.ascontiguousarray(wv), wo=np.ascontiguousarray(wo),
             gqb=np.tile(gq, 2)[:, None].copy(), gkb=np.tile(gk, 2)[:, None].copy(), gq1=gq[None].copy(), gk1=gk[None].copy(),
             tblT=np.ascontiguousarray(tbl[:nh].T), tblB=np.ascontiguousarray(tbl[:nh].reshape(1, nh * 32)),
             sink1=(sink[None].copy() if sink is not None else np.zeros((1, nh), np.float32)),
             cOH=ohb, cZ=Z, cG=G, valid2=_valid2(t0, own_off, n_ext))
    for i, a in enumerate(adds_full):
        m[f"pa{i}"] = _ext_rows(a, t0, own_off, n_ext)
    if dense_in is not None:
        xk, wqd_, wkd_, wvd_, gqd_, gkd_, (cos, sin, Rl) = dense_in
        wqd = np.zeros((D, 512), np.float32)
        for hq in range(8):
            r_, i_ = hq % 4, hq // 4
            wqd[:, (r_ * 2 + i_) * 64:(r_ * 2 + i_) * 64 + 64] = wqd_[:, hq * 64:(hq + 1) * 64]
        m.update(xk=xk, wqd=wqd, wkd=np.ascontiguousarray(wkd_), wvd=np.ascontiguousarray(wvd_),
                 gqd=np.tile(gqd_, 2)[:, None].copy(), gkd=np.tile(gkd_, 2)[:, None].copy(),
                 gqd1=gqd_[None].copy(), gkd1=gkd_[None].copy(), cosk=cos, sink_=sin,
                 cosq=np.ascontiguousarray(cos[:, t0:t0 + 4096]), sinq=np.ascontiguousarray(sin[:, t0:t0 + 4096]), cR=Rl)
    return m


def moe_inputs(half, x_full, gn, router, wg, wu, wd):
    p = np.arange(128)
    cG = (p[:, None] // 16 == p[None, :] // 16).astype(np.float32)
    cL = ((p[:, None] // 16 == p[None, :] // 16) & (p[:, None] < p[None, :])).astype(np.float32)
    perm = np.concatenate([np.arange(8 * half, 8 * half + 8), np.arange(8 * (1 - half), 8 * (1 - half) + 8)])
    return dict(x=x_full, gn=gn[None], wr=np.ascontiguousarray(router[:, perm]),
                wg=np.ascontiguousarray(wg[8 * half:8 * half + 8]), wu=np.ascontiguousarray(wu[8 * half:8 * half + 8]),
                wd=np.ascontiguousarray(wd[8 * half:8 * half + 8]), cG=cG, cL=cL)


CFG_L1F = dict(CFG_L1, n_add=1)


def fused_host_inputs(inp, b):
    x = inp["x"][b]
    m = {}
    xpad = np.zeros((NTOK + 2048, D), np.float32)
    xpad[1024:1024 + NTOK] = x
    m["xpad"] = xpad
    cons0, cons1 = band_consts(CFG_L0), band_consts(CFG_L1)
    cos, sin, Rl = rope_consts()
    w0, w1 = inp["l0_w_in"], inp["l1_w_in"]
    zx = np.zeros((NTOK, D), np.float32)
    for h in range(2):
        a0 = attn_inputs(CFG_L0, cons0, h, zx, [], inp["l0_norm_attn"], w0[:, 0:512], w0[:, 512:1024], w0[:, 1024:1536],
                         inp["l0_w_out"], inp["l0_a_qnorm"], inp["l0_a_knorm"], inp["rel_bias"], None,
                         dense_in=(zx, w0[:, 1536:2048], w0[:, 2048:2176], w0[:, 2176:2304], inp["l0_b_qnorm"],
                                   inp["l0_b_knorm"], (cos, sin, Rl)))
        a1 = attn_inputs(CFG_L1F, cons1, h, zx, [zx], inp["l1_norm_attn"], w1[:, 0:1024], w1[:, 1024:1280],
                         w1[:, 1280:1536], inp["l1_w_out"], inp["l1_c_qnorm"], inp["l1_c_knorm"], inp["rel_bias"],
                         inp["l1_sink"])
        m[f"a0_valid2_{h}"] = a0["valid2"]
        m[f"a1_valid2_{h}"] = a1["valid2"]
        if h == 0:
            for k, v in a0.items():
                if k not in ("xa", "xk", "valid2", "cosq", "sinq"):
                    m["a0_" + k] = v
            for k, v in a1.items():
                if k not in ("xa", "pa0", "valid2"):
                    m["a1_" + k] = v
    p = np.arange(128)
    m["cG16"] = (p[:, None] // 16 == p[None, :] // 16).astype(np.float32)
    m["cL16"] = ((p[:, None] // 16 == p[None, :] // 16) & (p[:, None] < p[None, :])).astype(np.float32)
    for l in range(2):
        m[f"m{l}_gn"] = inp[f"l{l}_norm_ffn"][None]
        m[f"m{l}_wr"] = inp[f"l{l}_router"]
        m[f"m{l}_wg"] = inp[f"l{l}_w_gate"]
        m[f"m{l}_wu"] = inp[f"l{l}_w_up"]
        m[f"m{l}_wd"] = inp[f"l{l}_w_down"]
    return {k: np.ascontiguousarray(v) for k, v in m.items()}


def build_fused(tmpl):
    ctx = Ctx()
    nc, S, A = ctx.nc, ctx.S, ctx.A
    npdt = {np.dtype(np.float32): F32, np.dtype(np.int32): I32}
    E = {k: nc.dram_tensor(k, list(v.shape), npdt[v.dtype], kind="ExternalInput").ap() for k, v in tmpl.items()}
    out = nc.dram_tensor("out", [NTOK, D], F32, kind="ExternalOutput").ap()
    x1p = ctx.scratch("x1p", [NTOK + 256, D], F32)
    p0p = ctx.scratch("p0p", [NTOK + 256, D], F32)
    x3 = ctx.scratch("x3", [NTOK, D], F32)
    p1 = ctx.scratch("p1", [NTOK, D], F32)
    zt = A.alloc([D], F32)
    Bz = Buf()
    S.add("dve", lambda e: e.memset(zt, 0.0), writes=[Bz])
    for t in (x1p, p0p):
        S.add("sp", DMA(t[0:128, :], zt), reads=[Bz], dma=True)
        S.add("sp", DMA(t[128 + NTOK:256 + NTOK, :], zt), reads=[Bz], dma=True)
    S.barrier()
    xpad = E["xpad"]
    a0 = {k[3:]: v for k, v in E.items() if k.startswith("a0_")}
    a1 = {k[3:]: v for k, v in E.items() if k.startswith("a1_")}
    for h in range(2):
        io = dict(a0)
        io.update(xa=xpad[h * 4096:h * 4096 + 6144, :], xk=xpad[1024:1024 + NTOK, :], valid2=a0[f"valid2_{h}"],
                  cosq=a0["cosk"][:, h * 4096:(h + 1) * 4096], sinq=a0["sink_"][:, h * 4096:(h + 1) * 4096],
                  out=x1p[128 + h * 4096:128 + (h + 1) * 4096, :])
        build_attn(CFG_L0, ctx, io)
    build_moe(ctx, dict(x=x1p[128:128 + NTOK, :], gn=E["m0_gn"], wr=E["m0_wr"], wg=E["m0_wg"], wu=E["m0_wu"],
                        wd=E["m0_wd"], cG=E["cG16"], cL=E["cL16"], part=p0p[128:128 + NTOK, :], part_full=p0p,
                        part_eoff=128 * D), zero_init=True, sets=(0, 1))
    for h in range(2):
        io = dict(a1)
        io.update(xa=x1p[h * 4096:h * 4096 + 4352, :], pa0=p0p[h * 4096:h * 4096 + 4352, :],
                  valid2=a1[f"valid2_{h}"], out=x3[h * 4096:(h + 1) * 4096, :])
        build_attn(CFG_L1F, ctx, io)
    build_moe(ctx, dict(x=x3, gn=E["m1_gn"], wr=E["m1_wr"], wg=E["m1_wg"], wu=E["m1_wu"], wd=E["m1_wd"],
                        cG=E["cG16"], cL=E["cL16"], part=p1), zero_init=True, sets=(0, 1))
    ctx.reset()
    bufs = [[A.alloc([D], F32) for _ in range(2)] for _ in range(3)]
    Bb = [[Buf() for _ in range(2)] for _ in range(3)]
    for T in range(NTOK // 128):
        k = T % 3
        S.add("sp", DMA(bufs[k][0], x3[T * 128:(T + 1) * 128, :]), writes=[Bb[k][0]], dma=True)
        S.add("act", DMA(bufs[k][1], p1[T * 128:(T + 1) * 128, :]), writes=[Bb[k][1]], dma=True)
        S.add("dve", lambda e, k=k: e.tensor_tensor(out=bufs[k][0], in0=bufs[k][0], in1=bufs[k][1], op=ALU.add),
              reads=[Bb[k][0], Bb[k][1]], writes=[Bb[k][0]])
        S.add("sp", DMA(out[T * 128:(T + 1) * 128, :], bufs[k][0]), reads=[Bb[k][0]], dma=True)
    S.barrier()
    S.emit()
    return nc


def kernel(**inp):
    inp = {k: np.asarray(v) for k, v in inp.items()}
    nb = inp["x"].shape[0]
    maps = [fused_host_inputs(inp, b) for b in range(nb)]
    nc = build_fused(maps[0])
    res = run_bass_kernel_spmd(nc, maps, core_ids=list(range(nb)))
    return np.stack([res.results[b]["out"] for b in range(nb)]).astype(np.float32)
```

```python
import numpy as np
import concourse.bass as bass
import concourse.mybir as mybir
from concourse.bass_utils import run_bass_kernel_spmd

F32 = mybir.dt.float32
BF16 = mybir.dt.bfloat16
I32 = mybir.dt.int32
U32 = mybir.dt.uint32
ALU = mybir.AluOpType
AF = mybir.ActivationFunctionType
AX = mybir.AxisListType

ENGS = ("pe", "act", "dve", "pool", "sp")


class Buf:
    __slots__ = ("name", "writers", "readers")

    def __init__(self, name=""):
        self.name = name
        self.writers = []
        self.readers = []


class Op:
    __slots__ = ("eng", "fn", "deps", "is_dma", "marked", "cnt", "sem", "semval", "seq")

    def __init__(self, eng, fn, is_dma):
        self.eng = eng
        self.fn = fn
        self.deps = set()
        self.is_dma = is_dma
        self.marked = False
        self.cnt = 0
        self.sem = None
        self.semval = 0


def _prune(lst, op):
    if not op.is_dma:
        lst[:] = [o for o in lst if o.is_dma or o.eng != op.eng]
    lst.append(op)


class Sched:
    def __init__(self, nc, n_dma_sems=None):
        self.nc = nc
        self.ops = {e: [] for e in ENGS}
        self.dma_ops = {e: [] for e in ENGS}
        self.n_dma_sems = n_dma_sems or {"sp": 24, "act": 8, "pool": 16}
        self.seq = 0
        self.since_barrier = []
        self.regcache = {}

    def getreg(self, eng, val):
        if val not in self.regcache:
            self.regcache[val] = eng.to_reg(val)
        return self.regcache[val]

    def add(self, eng, fn, reads=(), writes=(), pwrites=(), dma=False, extra_deps=()):
        op = Op(eng, fn, dma)
        op.seq = self.seq
        self.seq += 1
        deps = set(extra_deps)
        raw = set()
        for b in reads:
            for w in b.writers:
                deps.add(w)
                raw.add(w)
        for b in writes:
            deps.update(b.writers)
            deps.update(b.readers)
        for b in pwrites:
            deps.update(b.readers)
            for w in b.writers:
                if w.is_dma or w.eng != eng:
                    deps.add(w)
        for d in deps:
            if d is op:
                continue
            if d.is_dma or dma:
                op.deps.add(d)
            elif d.eng != eng:
                op.deps.add(d)
            elif d in raw and eng != "pe":
                op.deps.add(d)
        for b in reads:
            _prune(b.readers, op)
        for b in writes:
            b.writers = [op]
            b.readers = []
        for b in pwrites:
            _prune(b.writers, op)
        if dma:
            q = self.dma_ops[eng]
            K = self.n_dma_sems[eng]
            if len(q) >= K:
                op.deps.add(q[len(q) - K])
            op.cnt = len(q)
            q.append(op)
        self.ops[eng].append(op)
        self.since_barrier.append(op)
        return op

    def barrier(self):
        last = {}
        dl = {}
        for o in self.since_barrier:
            if o.is_dma:
                dl[(o.eng, o.cnt % self.n_dma_sems[o.eng])] = o
            else:
                last[o.eng] = o
        deps = list(last.values()) + list(dl.values())
        self.since_barrier = []
        new = []
        for e in ENGS:
            op = self.add(e, lambda eng: eng.nop(), extra_deps=[d for d in deps])
            new.append(op)
        return new

    def emit(self):
        nc = self.nc
        for e in ENGS:
            for op in self.ops[e]:
                for d in op.deps:
                    d.marked = True
        eng_sem = {e: nc.alloc_semaphore(f"s_{e}") for e in ENGS}
        dma_sems = {e: [nc.alloc_semaphore(f"d_{e}{i}") for i in range(self.n_dma_sems.get(e, 0))]
                    for e in ENGS}
        for e in ENGS:
            c = 0
            for op in self.ops[e]:
                if op.is_dma:
                    continue
                if op.marked:
                    c += 1
                    op.cnt = c
            K = self.n_dma_sems.get(e, 0)
            for i, op in enumerate(self.dma_ops[e]):
                op.sem = dma_sems[e][i % K]
                op.semval = 16 * (i // K + 1)
        self.max_cnt = {e: max([o.cnt for o in self.ops[e]] + [0]) for e in ENGS}

        def run(e, eng):
            seen = {}
            for op in self.ops[e]:
                for d in sorted(op.deps, key=lambda o: o.seq):
                    if d.is_dma:
                        key, val, sem = ("d", id(d.sem)), d.semval, d.sem
                    else:
                        key, val, sem = ("e", d.eng), d.cnt, eng_sem[d.eng]
                    if seen.get(key, 0) < val:
                        eng.wait_ge(sem, val)
                        seen[key] = val
                ins = op.fn(eng)
                if op.is_dma:
                    ins.then_inc(op.sem, 16)
                elif op.marked:
                    ins.then_inc(eng_sem[e], 1)

        with nc.Block() as block:
            @block.tensor
            def _(eng):
                run("pe", eng)

            @block.scalar
            def _(eng):
                run("act", eng)

            @block.vector
            def _(eng):
                run("dve", eng)

            @block.gpsimd
            def _(eng):
                run("pool", eng)

            @block.sync
            def _(eng):
                run("sp", eng)


class Arena:
    def __init__(self, nc, name, nbytes):
        self.t = nc.alloc_sbuf_tensor(name, [128, nbytes // 4], F32)
        self.nbytes = nbytes
        self.off = 0
        self.marks = []

    def alloc(self, shape, dtype, parts=128):
        esz = {F32: 4, BF16: 2, I32: 4, U32: 4}[dtype]
        n = int(np.prod(shape))
        nb = (n * esz + 31) // 32 * 32
        assert self.off + nb <= self.nbytes, f"arena overflow {self.off}+{nb}>{self.nbytes}"
        a = self.t[0:parts, self.off // 4:(self.off + nb) // 4]
        self.off += nb
        if dtype != F32:
            a = a.bitcast(dtype)
        a = a[:, 0:n]
        if len(shape) > 1:
            names = " ".join(f"d{i}" for i in range(len(shape)))
            kw = {f"d{i}": s for i, s in enumerate(shape)}
            a = a.rearrange(f"p ({names}) -> p {names}", **kw)
        return a

    def mark(self):
        self.marks.append(self.off)

    def release(self):
        self.off = self.marks.pop()


NTOK = 8192
D = 1024
NE = 8
CAP = 1024
DF = 2048
ROWW = 524
EPS = 1e-6


class Ctx:
    def __init__(self):
        self.nc = bass.Bass("TRN2", target_bir_lowering=False)
        self.S = Sched(self.nc)
        self.A = Arena(self.nc, "arena", 204 * 1024)
        self.psall = self.nc.alloc_psum_tensor("psall", [128, 4096], F32)
        self.dram = {}

    def scratch(self, name, shape, dt):
        key = (name, tuple(shape), str(dt))
        if key not in self.dram:
            self.dram[key] = self.nc.dram_tensor(name, list(shape), dt).ap()
        return self.dram[key]

    def reset(self):
        self.A.off = 0
        self.A.marks = []


def DMA(out, in_, **kw):
    return lambda e: e.dma_start(out=out, in_=in_, **kw)


def make_ident(S, A):
    identf = A.alloc([128], F32)
    identb = A.alloc([128], BF16)
    bf, bb = Buf("identf"), Buf("identb")
    S.add("pool", lambda e: e.memset(identf, 0.0), writes=[bf])
    S.add("pool", lambda e: e.affine_select(out=identf, in_=identf, pattern=[[-1, 128]],
                                            compare_op=ALU.not_equal, fill=1.0, base=0,
                                            channel_multiplier=1), reads=[bf], writes=[bf])
    S.add("dve", lambda e: e.tensor_copy(out=identb, in_=identf), reads=[bf], writes=[bb])
    return identf, identb, bf, bb


def build_moe(ctx=None, io=None, zero_init=True, wr_sets=None, sets=None):
    fused16 = sets is not None
    sets = sets if fused16 else (0,)
    RW = 532 if fused16 else ROWW
    GW = 16 if fused16 else 8
    TOKW = 512 + GW
    if ctx is None:
        nc = bass.Bass("TRN2", target_bir_lowering=False)
        x = nc.dram_tensor("x", [NTOK, D], F32, kind="ExternalInput").ap()
        gn = nc.dram_tensor("gn", [1, D], F32, kind="ExternalInput").ap()
        wr = nc.dram_tensor("wr", [D, 16], F32, kind="ExternalInput").ap()
        wg = nc.dram_tensor("wg", [NE, D, DF], F32, kind="ExternalInput").ap()
        wu = nc.dram_tensor("wu", [NE, D, DF], F32, kind="ExternalInput").ap()
        wd = nc.dram_tensor("wd", [NE, DF, D], F32, kind="ExternalInput").ap()
        cG = nc.dram_tensor("cG", [128, 128], F32, kind="ExternalInput").ap()
        cL = nc.dram_tensor("cL", [128, 128], F32, kind="ExternalInput").ap()
        part = nc.dram_tensor("part", [NTOK, D], F32, kind="ExternalOutput").ap()
        hbuf = nc.dram_tensor("hbuf", [NTOK, ROWW], F32).ap()
        affd = nc.dram_tensor("affd", [16, NTOK], F32).ap()
        xs = [nc.dram_tensor(f"xs{i}", [CAP, ROWW], F32).ap() for i in range(NE)]
        S = Sched(nc)
        A = Arena(nc, "arena", 204 * 1024)
        ps = [nc.alloc_psum_tensor(f"ps{i}", [128, 512], F32) for i in range(8)]
        part_full, part_eoff = part, 0
    else:
        nc, S, A = ctx.nc, ctx.S, ctx.A
        ctx.reset()
        x, gn, wr, wg, wu, wd, cG, cL, part = (io[k] for k in ("x", "gn", "wr", "wg", "wu", "wd", "cG", "cL", "part"))
        part_full, part_eoff = io.get("part_full", part), io.get("part_eoff", 0)
        hbuf = ctx.scratch("hbuf16", [NTOK, RW], F32)
        affd = ctx.scratch("affd", [16, NTOK], F32)
        xs = [ctx.scratch(f"xs16_{i}", [CAP, RW], F32) for i in range(NE)]
        ps = [ctx.psall[:, i * 512:(i + 1) * 512] for i in range(8)]
    PB = [Buf(f"ps{i}") for i in range(8)]

    identf, identb, Bif, Bib = make_ident(S, A)
    G_sb = A.alloc([128], F32)
    L_sb = A.alloc([128], F32)
    gb = A.alloc([D], F32)
    gcol = A.alloc([8], F32)
    wr_sb = A.alloc([8, 16], F32)
    zero = A.alloc([D], F32)
    tokid = A.alloc([64], I32)
    Bc = Buf("consts")
    S.add("sp", DMA(G_sb, cG), pwrites=[Bc], dma=True)
    S.add("sp", DMA(L_sb, cL), pwrites=[Bc], dma=True)
    S.add("sp", DMA(gb, gn.partition_broadcast(128)), pwrites=[Bc], dma=True)
    S.add("sp", DMA(gcol, gn.rearrange("o (c p) -> p (o c)", p=128), allow_slow_non_contiguous=True),
          pwrites=[Bc], dma=True)
    wrv = wr.rearrange("(c p) e -> p c e", p=128)
    if wr_sets is None:
        S.add("sp", DMA(wr_sb, wrv), pwrites=[Bc], dma=True)
    else:
        own, oth = wr_sets
        S.add("sp", DMA(wr_sb[:, :, 0:8], wrv[:, :, own:own + 8]), pwrites=[Bc], dma=True)
        S.add("sp", DMA(wr_sb[:, :, 8:16], wrv[:, :, oth:oth + 8]), pwrites=[Bc], dma=True)
    S.add("dve", lambda e: e.memset(zero, 0.0), pwrites=[Bc])
    S.add("pool", lambda e: e.iota(tokid, pattern=[[128, 64]], base=0, channel_multiplier=1), pwrites=[Bc])
    Bpart = Buf("part")
    if zero_init:
        for T in range(NTOK // 128):
            S.add("sp", DMA(part[T * 128:(T + 1) * 128, :], zero), reads=[Bc], pwrites=[Bpart], dma=True)

    posT = A.alloc([4, 128], I32)
    A.mark()
    affT_sb = A.alloc([NTOK], F32)
    BaffT = Buf("affT")
    xts = [A.alloc([D], F32) for _ in range(3)]
    Bxt = [Buf() for _ in range(3)]
    xns = [A.alloc([D], F32) for _ in range(2)]
    Bxn = [Buf() for _ in range(2)]
    hTs = [A.alloc([8, 128], F32) for _ in range(2)]
    BhT = [Buf() for _ in range(2)]
    rts = [A.alloc([RW], F32) for _ in range(3)]
    Brt = [Buf() for _ in range(3)]
    sts = [A.alloc([8], F32) for _ in range(4)]
    Bst = [Buf() for _ in range(4)]
    exs = [A.alloc([16], F32) for _ in range(2)]
    Bex = [Buf() for _ in range(2)]
    affs = [A.alloc([16], F32) for _ in range(2)]
    Baf = [Buf() for _ in range(2)]
    junk = A.alloc([D], BF16)
    Bjunk = Buf()
    for T in range(NTOK // 128):
        xt, bxt = xts[T % 3], Bxt[T % 3]
        xn, bxn = xns[T % 2], Bxn[T % 2]
        hT, bhT = hTs[T % 2], BhT[T % 2]
        rt, brt = rts[T % 3], Brt[T % 3]
        st, bst = sts[T % 4], Bst[T % 4]
        ex, bex = exs[T % 2], Bex[T % 2]
        af, baf = affs[T % 2], Baf[T % 2]
        rt_bf = rt.bitcast(BF16)
        rt_i = rt.bitcast(I32)
        S.add("sp", DMA(xt, x[T * 128:(T + 1) * 128, :]), writes=[bxt], dma=True)
        S.add("act", lambda e, xt=xt, st=st: e.activation(out=junk, in_=xt, func=AF.Square, scale=1.0 / 32,
                                                          accum_out=st[:, 0:1]),
              reads=[bxt], writes=[Bjunk, bst])
        S.add("act", lambda e, st=st: e.activation(out=st[:, 1:2], in_=st[:, 0:1], func=AF.Sqrt, bias=EPS, scale=1.0),
              reads=[bst], pwrites=[bst])
        S.add("dve", lambda e, st=st: e.reciprocal(out=st[:, 2:3], in_=st[:, 1:2]), reads=[bst], pwrites=[bst])
        S.add("dve", lambda e, xn=xn, xt=xt, st=st: e.tensor_scalar_mul(out=xn, in0=xt, scalar1=st[:, 2:3]),
              reads=[bxt, bst], writes=[bxn])
        b0 = 2 * (T % 2)
        for c in range(8):
            pb = b0 + c // 4
            S.add("pe", lambda e, pb=pb, c=c, xn=xn: e.transpose(out=ps[pb][:, (c % 4) * 128:(c % 4 + 1) * 128],
                                                                 in_=xn[:, c * 128:(c + 1) * 128], identity=identf),
                  reads=[bxn, Bif], writes=[PB[pb]] if c % 4 == 0 else [], pwrites=[] if c % 4 == 0 else [PB[pb]])
        for c in range(8):
            pb = b0 + c // 4
            src = ps[pb][:, (c % 4) * 128:(c % 4 + 1) * 128]
            if c % 2 == 0:
                S.add("act", lambda e, src=src, c=c, hT=hT: e.activation(out=hT[:, c, :], in_=src, func=AF.Copy,
                                                                         scale=gcol[:, c:c + 1]),
                      reads=[PB[pb], Bc], writes=[bhT] if c == 0 else [], pwrites=[] if c == 0 else [bhT])
            else:
                S.add("dve", lambda e, src=src, c=c, hT=hT: e.tensor_scalar_mul(out=hT[:, c, :], in0=src,
                                                                                scalar1=gcol[:, c:c + 1]),
                      reads=[PB[pb], Bc], pwrites=[bhT])
        pl = 4 + T % 2
        for c in range(8):
            S.add("pe", lambda e, pl=pl, c=c, hT=hT: e.matmul(ps[pl][:, 0:16], lhsT=hT[:, c, :], rhs=wr_sb[:, c, :],
                                                              start=(c == 0), stop=(c == 7)),
                  reads=[bhT, Bc], writes=[PB[pl]] if c == 0 else [], pwrites=[] if c == 0 else [PB[pl]])
        S.add("dve", lambda e, pl=pl, st=st: e.reduce_max(out=st[:, 3:4], in_=ps[pl][:, 0:16], axis=AX.X),
              reads=[PB[pl]], pwrites=[bst])
        S.add("dve", lambda e, st=st: e.tensor_scalar_mul(out=st[:, 4:5], in0=st[:, 3:4], scalar1=-1.0),
              reads=[bst], pwrites=[bst])
        S.add("act", lambda e, pl=pl, st=st, ex=ex: e.activation(out=ex, in_=ps[pl][:, 0:16], func=AF.Exp,
                                                                 bias=st[:, 4:5], scale=1.0, accum_out=st[:, 5:6]),
              reads=[PB[pl], bst], writes=[bex], pwrites=[bst])
        S.add("dve", lambda e, st=st: e.reciprocal(out=st[:, 6:7], in_=st[:, 5:6]), reads=[bst], pwrites=[bst])
        S.add("dve", lambda e, af=af, ex=ex, st=st: e.tensor_scalar_mul(out=af, in0=ex, scalar1=st[:, 6:7]),
              reads=[bex, bst], writes=[baf])
        S.add("dve", lambda e, rt_bf=rt_bf, xn=xn: e.tensor_tensor(out=rt_bf[:, 0:D], in0=xn, in1=gb, op=ALU.mult),
              reads=[bxn, Bc], writes=[brt])
        S.add("act", lambda e, rt=rt, af=af: e.copy(out=rt[:, 512:512 + GW], in_=af[:, 0:GW]), reads=[baf], pwrites=[brt])
        S.add("dve", lambda e, rt_i=rt_i, T=T: e.tensor_copy(out=rt_i[:, TOKW:TOKW + 1], in_=tokid[:, T:T + 1]),
              reads=[Bc], pwrites=[brt])
        S.add("act", DMA(hbuf[T * 128:(T + 1) * 128, 0:TOKW + 1], rt[:, 0:TOKW + 1]), reads=[brt], dma=True)
        pa = 6 + T % 2
        S.add("pe", lambda e, pa=pa, af=af: e.transpose(out=ps[pa][0:16, 0:128], in_=af, identity=identf),
              reads=[baf, Bif], writes=[PB[pa]])
        S.add("act", lambda e, pa=pa, T=T: e.copy(out=affT_sb[0:16, T * 128:(T + 1) * 128], in_=ps[pa][0:16, 0:128]),
              reads=[PB[pa]], pwrites=[BaffT])

    Baffd = Buf("affd")
    S.add("sp", DMA(affd, affT_sb[0:16, :]), reads=[BaffT], writes=[Baffd], dma=True)
    S.barrier()
    A.release()
    for s_ in sets:
        eofs = 8 * s_ if fused16 else 0
        gofs = eofs
        prev_sc = []
        A.mark()
        a_sb = A.alloc([512], F32)
        Ba = Buf("a")
        S.add("sp", DMA(a_sb, affd[eofs:eofs + 8, :].rearrange("e (c j) -> (e c) j", c=16)), reads=[Baffd], writes=[Ba], dma=True)
        msk = A.alloc([512], F32)
        Bm = Buf("msk")
        sc = A.alloc([8], F32)
        Bs = Buf("sc")
        S.add("dve", lambda e: e.memset(sc, 0.0), writes=[Bs])
        pt = 0
        for k in range(30):
            dl = 2.0 ** -(k + 1)
            S.add("dve", lambda e, dl=dl: e.tensor_scalar_add(out=sc[:, 1:2], in0=sc[:, 0:1], scalar1=dl),
                  reads=[Bs], pwrites=[Bs])
            S.add("dve", lambda e: e.tensor_single_scalar(out=msk, in_=a_sb, scalar=sc[:, 1:2], op=ALU.is_ge),
                  reads=[Ba, Bs], writes=[Bm])
            S.add("dve", lambda e: e.reduce_sum(out=sc[:, 2:3], in_=msk, axis=AX.X), reads=[Bm], pwrites=[Bs])
            S.add("pe", lambda e: e.matmul(ps[pt][:, 0:1], lhsT=G_sb, rhs=sc[:, 2:3], start=True, stop=True),
                  reads=[Bs, Bc], writes=[PB[pt]])
            S.add("dve", lambda e: e.tensor_single_scalar(out=sc[:, 3:4], in_=ps[pt][:, 0:1], scalar=CAP - 0.5, op=ALU.is_ge),
                  reads=[PB[pt]], pwrites=[Bs])
            S.add("dve", lambda e, dl=dl: e.scalar_tensor_tensor(out=sc[:, 0:1], in0=sc[:, 3:4], scalar=dl, in1=sc[:, 0:1],
                                                                op0=ALU.mult, op1=ALU.add),
                  reads=[Bs], pwrites=[Bs])

        ones = A.alloc([512], F32)
        incl = A.alloc([512], F32)
        posm = A.alloc([512], F32)
        Bo, Bi, Bp, BpT = Buf(), Buf(), Buf(), Buf()
        S.add("dve", lambda e: e.memset(ones, 1.0), writes=[Bo])
        S.add("dve", lambda e: e.tensor_single_scalar(out=msk, in_=a_sb, scalar=sc[:, 0:1], op=ALU.is_ge),
              reads=[Ba, Bs], writes=[Bm])
        S.add("dve", lambda e: e.reduce_sum(out=sc[:, 2:3], in_=msk, axis=AX.X), reads=[Bm], pwrites=[Bs])
        S.add("pe", lambda e: e.matmul(ps[pt][:, 0:1], lhsT=L_sb, rhs=sc[:, 2:3], start=True, stop=True),
              reads=[Bs, Bc], writes=[PB[pt]])
        S.add("dve", lambda e: e.tensor_copy(out=sc[:, 4:5], in_=ps[pt][:, 0:1]), reads=[PB[pt]], pwrites=[Bs])
        S.add("dve", lambda e: e.tensor_tensor_scan(out=incl, data0=ones, data1=msk, initial=0.0, op0=ALU.mult, op1=ALU.add),
              reads=[Bo, Bm], writes=[Bi])
        S.add("dve", lambda e: e.tensor_tensor(out=incl, in0=incl, in1=msk, op=ALU.subtract), reads=[Bi, Bm], writes=[Bi])
        S.add("dve", lambda e: e.tensor_scalar(out=posm, in0=incl, scalar1=sc[:, 4:5], scalar2=-4096.0,
                                               op0=ALU.add, op1=ALU.add), reads=[Bi, Bs], writes=[Bp])
        S.add("dve", lambda e: e.tensor_tensor(out=posm, in0=posm, in1=msk, op=ALU.mult), reads=[Bp, Bm], writes=[Bp])
        S.add("dve", lambda e: e.tensor_scalar_add(out=posm, in0=posm, scalar1=4096.0), reads=[Bp], writes=[Bp])
        pq = 1
        for jb in range(4):
            S.add("pe", lambda e, jb=jb: e.transpose(out=ps[pq][:, jb * 128:(jb + 1) * 128],
                                                     in_=posm[:, jb * 128:(jb + 1) * 128], identity=identf),
                  reads=[Bp, Bif], writes=[PB[pq]] if jb == 0 else [], pwrites=[] if jb == 0 else [PB[pq]])
        S.add("dve", lambda e: e.tensor_copy(out=posT, in_=ps[pq][:, 0:512].rearrange("p (a b) -> p a b", a=4)),
              reads=[PB[pq]], writes=[BpT])

        S.barrier()
        A.release()
        A.mark()
        import os as _os
        if _os.environ.get("MOE_STOP") == "3":
            S.emit()
            return nc
        NRT = 6
        rtl = [A.alloc([RW], F32) for _ in range(NRT)]
        Brl = [Buf() for _ in range(NRT)]
        Bxs = [Buf(f"xs{e}") for e in range(NE)]
        NSTG = 4
        stg = [A.alloc([2048], F32) for _ in range(NSTG)]
        Bstg = [Buf() for _ in range(NSTG)]
        wgb = [A.alloc([8, 256], BF16) for _ in range(2)]
        wub = [A.alloc([8, 256], BF16) for _ in range(2)]
        Bwg = [Buf() for _ in range(2)]
        Bwu = [Buf() for _ in range(2)]
        wdb = A.alloc([16, D], BF16)
        Bwd = [Buf() for _ in range(8)]
        xsb = A.alloc([8, RW], F32)
        Bxsb = Buf("xsb")
        XT = A.alloc([8, CAP], BF16)
        BXT = Buf("XT")
        AT = A.alloc([16, CAP], BF16)
        BAT = [Buf() for _ in range(16)]
        sgs = [A.alloc([512], F32) for _ in range(2)]
        Bsg = [Buf() for _ in range(2)]
        yts = [A.alloc([D], F32) for _ in range(3)]
        Byt = [Buf() for _ in range(3)]
        pgs = [A.alloc([D], F32) for _ in range(3)]
        Bpg = [Buf() for _ in range(3)]
        pg_i = [0]
        stg_i = [0]
        rt_i_ = [0]
        cast_i = [0]
        sg_i = [0]
        yt_i = [0]
        prev_sc = []

        def cast(out, in_, reads, writes):
            eng = "dve" if cast_i[0] % 3 != 2 else "act"
            cast_i[0] += 1
            if eng == "dve":
                S.add("dve", lambda e: e.tensor_copy(out=out, in_=in_), reads=reads, writes=writes)
            else:
                S.add("act", lambda e: e.copy(out=out, in_=in_), reads=reads, writes=writes)

        def scatter_rows(e_, Ts=None):
            for T in (range(NTOK // 128) if Ts is None else Ts):
                c, jb = T // 4, T % 4
                k = rt_i_[0] % NRT
                rt_i_[0] += 1
                S.add("sp", DMA(rtl[k][:, 0:TOKW + 1], hbuf[T * 128:(T + 1) * 128, 0:TOKW + 1]), writes=[Brl[k]], dma=True)
                idx = posT[:, jb, e_ * 16 + c:e_ * 16 + c + 1]
                S.add("pool", lambda e, k=k, idx=idx, e_=e_: e.indirect_dma_start(
                    out=xs[e_], out_offset=bass.IndirectOffsetOnAxis(ap=idx, axis=0), in_=rtl[k], in_offset=None,
                    bounds_check=S.getreg(e, CAP - 1), oob_is_err=False), reads=[Brl[k], BpT], pwrites=[Bxs[e_]], dma=True)

        scatter_rows(0)
        for e_ in range(NE):
            nxt_T = list(range(NTOK // 128)) if e_ + 1 < NE else []
            S.add("sp", DMA(xsb, xs[e_].rearrange("(t p) c -> p t c", p=128)), reads=[Bxs[e_]], writes=[Bxsb], dma=True)
            xsb_bf = xsb.rearrange("p t c -> p (t c)").bitcast(BF16).rearrange("p (t c) -> p t c", t=8)
            xsb_i = xsb.rearrange("p t c -> p (t c)").bitcast(I32).rearrange("p (t c) -> p t c", t=8)
            for dc in range(8):
                for half in range(2):
                    pb = 6 + half
                    psb = ps[pb].bitcast(BF16)
                    for t4 in range(4):
                        t = half * 4 + t4
                        S.add("pe", lambda e, psb=psb, t4=t4, t=t, dc=dc: e.transpose(
                            out=psb[:, t4 * 128:(t4 + 1) * 128], in_=xsb_bf[:, t, dc * 128:(dc + 1) * 128], identity=identb),
                            reads=[Bxsb, Bib], writes=[PB[pb]] if t4 == 0 else [], pwrites=[] if t4 == 0 else [PB[pb]])
                    eng = "act" if (dc + half) % 2 == 0 else "dve"
                    dst = XT[:, dc, half * 512:(half + 1) * 512]
                    if eng == "act":
                        S.add("act", lambda e, psb=psb, dst=dst: e.copy(out=dst, in_=psb[:, 0:512]),
                              reads=[PB[pb]], writes=[BXT] if (dc == 0 and half == 0) else [],
                              pwrites=[] if (dc == 0 and half == 0) else [BXT])
                    else:
                        S.add("dve", lambda e, psb=psb, dst=dst: e.tensor_copy(out=dst, in_=psb[:, 0:512]),
                              reads=[PB[pb]], pwrites=[BXT])
            for fq in range(8):
                wsel = fq % 2
                for (wsrc, wdst, bw) in ((wg, wgb[wsel], Bwg[wsel]), (wu, wub[wsel], Bwu[wsel])):
                    k = stg_i[0] % NSTG
                    stg_i[0] += 1
                    sv = stg[k].rearrange("p (c f) -> p c f", c=8)
                    S.add("sp", DMA(sv, wsrc[eofs + e_].rearrange("(c p) f -> p c f", p=128)[:, :, fq * 256:(fq + 1) * 256]),
                          writes=[Bstg[k]], dma=True)
                    cast(wdst, sv, [Bstg[k]], [bw])
                for fl in range(2):
                    fc = fq * 2 + fl
                    bset = 0 if fc % 2 == 0 else 3
                    gb_ = [bset, bset + 1]
                    ub_ = [bset + 2, (bset + 3) if bset == 0 else 0]
                    if bset == 3:
                        gb_ = [4, 5]
                        ub_ = [6, 7]
                    else:
                        gb_ = [0, 1]
                        ub_ = [2, 3]
                    for half in range(2):
                        for (wsb, bw, bank) in ((wgb[wsel], Bwg[wsel], gb_[half]), (wub[wsel], Bwu[wsel], ub_[half])):
                            for dc in range(8):
                                S.add("pe", lambda e, wsb=wsb, bank=bank, dc=dc, fl=fl, half=half: e.matmul(
                                    ps[bank][:, 0:512], lhsT=wsb[:, dc, fl * 128:(fl + 1) * 128],
                                    rhs=XT[:, dc, half * 512:(half + 1) * 512], start=(dc == 0), stop=(dc == 7)),
                                    reads=[bw, BXT], writes=[PB[bank]] if dc == 0 else [],
                                    pwrites=[] if dc == 0 else [PB[bank]])
                        k = sg_i[0] % 2
                        sg_i[0] += 1
                        S.add("act", lambda e, k=k, bank=gb_[half]: e.activation(out=sgs[k], in_=ps[bank][:, 0:512], func=AF.Silu),
                              reads=[PB[gb_[half]]], writes=[Bsg[k]])
                        S.add("dve", lambda e, k=k, bank=ub_[half], fc=fc, half=half: e.tensor_tensor(
                            out=AT[:, fc, half * 512:(half + 1) * 512], in0=ps[bank][:, 0:512], in1=sgs[k], op=ALU.mult),
                            reads=[PB[ub_[half]], Bsg[k]], writes=[BAT[fc]] if half == 0 else [],
                            pwrites=[] if half == 0 else [BAT[fc]])
                    if nxt_T:
                        scatter_rows(e_ + 1, nxt_T[:4])
                        del nxt_T[:4]
            for q in range(8):
                k = stg_i[0] % NSTG
                stg_i[0] += 1
                sv = stg[k].rearrange("p (c f) -> p c f", c=2)
                S.add("sp", DMA(sv, wd[eofs + e_].rearrange("(c p) f -> p c f", p=128)[:, q * 2:(q + 1) * 2, :]),
                      writes=[Bstg[k]], dma=True)
                cast(wdb[:, q * 2:(q + 1) * 2, :], sv, [Bstg[k]], [Bwd[q]])
            cur_sc = []
            for t in range(8):
                k = yt_i[0] % 3
                yt_i[0] += 1
                for half in range(2):
                    bank = (t * 2 + half) % 6
                    for fc in range(16):
                        S.add("pe", lambda e, bank=bank, fc=fc, t=t, half=half: e.matmul(
                            ps[bank][:, 0:512], lhsT=AT[:, fc, t * 128:(t + 1) * 128],
                            rhs=wdb[:, fc, half * 512:(half + 1) * 512], start=(fc == 0), stop=(fc == 15)),
                            reads=[BAT[fc], Bwd[fc // 2]], writes=[PB[bank]] if fc == 0 else [],
                            pwrites=[] if fc == 0 else [PB[bank]])
                    gsc = xsb[:, t, 512 + gofs + e_:513 + gofs + e_]
                    if half == 0:
                        S.add("act", lambda e, k=k, bank=bank, gsc=gsc: e.activation(
                            out=yts[k][:, 0:512], in_=ps[bank][:, 0:512], func=AF.Copy, scale=gsc),
                            reads=[PB[bank], Bxsb], writes=[Byt[k]])
                    else:
                        S.add("dve", lambda e, k=k, bank=bank, gsc=gsc: e.tensor_scalar_mul(
                            out=yts[k][:, 512:1024], in0=ps[bank][:, 0:512], scalar1=gsc),
                            reads=[PB[bank], Bxsb], pwrites=[Byt[k]])
                idx = xsb_i[:, t, TOKW:TOKW + 1]
                kg_ = pg_i[0] % 3
                pg_i[0] += 1
                S.add("pool", lambda e, kg_=kg_, idx=idx: e.indirect_dma_start(
                    out=pgs[kg_], out_offset=None, in_=part_full,
                    in_offset=bass.IndirectOffsetOnAxis(ap=idx, axis=0), element_offset=part_eoff),
                    reads=[Bxsb, Bpart], writes=[Bpg[kg_]], dma=True, extra_deps=prev_sc)
                S.add("dve", lambda e, k=k, kg_=kg_: e.tensor_tensor(out=yts[k], in0=yts[k], in1=pgs[kg_], op=ALU.add),
                      reads=[Byt[k], Bpg[kg_]], writes=[Byt[k]])
                op = S.add("pool", lambda e, k=k, idx=idx: e.indirect_dma_start(
                    out=part_full, out_offset=bass.IndirectOffsetOnAxis(ap=idx, axis=0), in_=yts[k], in_offset=None,
                    element_offset=part_eoff),
                    reads=[Byt[k], Bxsb], pwrites=[Bpart], dma=True)
                cur_sc.append(op)
            prev_sc = cur_sc
        S.barrier()
        A.release()
    S.barrier()
    if ctx is not None:
        return None
    S.emit()
    return nc


import math


def t5_bucket_np(rel):
    rel = np.asarray(rel, np.int64)
    ret = np.where(rel > 0, 16, 0)
    n = np.abs(rel)
    nf = np.maximum(n, 1).astype(np.float32)
    large = 8 + (np.log(nf / np.float32(8)) / np.float32(math.log(128.0)) * np.float32(8)).astype(np.int32)
    large = np.minimum(large, 15)
    return ret + np.where(n < 8, n, large)


def band_consts(cfg):
    ohb = []
    meta = []
    for (d, radius, wins) in cfg["branches"]:
        for (off, bases) in wins:
            for base in bases:
                rho = np.arange(128)
                rel = rho + off - base + 128
                valid = np.abs(rel) <= radius
                bk = t5_bucket_np(rel * d)
                m = np.zeros((32, 128), np.float32)
                m[bk[valid], rho[valid]] = 1.0
                ohb.append(m)
    Z = np.zeros((128, 512), np.float32)
    Z[np.arange(128), np.arange(128) + 128] = 1.0
    p = np.arange(128)
    G = (p[:, None] // 64 == p[None, :] // 64).astype(np.float32)
    return np.stack(ohb), Z, G


def rope_consts():
    half = 32
    inv = (10000.0 ** (-np.arange(0, half, 2, dtype=np.float32) / half)).astype(np.float32)
    t = np.arange(8192)
    row, col = t // 64, t % 64
    cos = np.zeros((64, 8192), np.float32)
    sin = np.zeros((64, 8192), np.float32)
    for f in range(64):
        pos = row if f < 32 else col
        ang = pos.astype(np.float32) * inv[f % 16]
        cos[f] = np.cos(ang)
        sin[f] = np.sin(ang)
    cos = np.concatenate([cos, cos], 0)
    sin = np.concatenate([sin, sin], 0)
    Rl = np.zeros((128, 128), np.float32)
    for f0 in range(0, 128, 32):
        for i in range(16):
            Rl[f0 + 16 + i, f0 + i] = -1.0
            Rl[f0 + i, f0 + 16 + i] = 1.0
    return cos, sin, Rl


def build_attn(cfg, ctx=None, io=None):
    io = io or {}
    if ctx is None:
        nc = bass.Bass("TRN2", target_bir_lowering=False)
    else:
        nc = ctx.nc
        ctx.reset()
    n_ext, own_off, n_own, CH, n_add = cfg["n_ext"], cfg["own_off"], cfg["n_own"], cfg["CH"], cfg["n_add"]
    nh, nkv = cfg["nh"], cfg["nkv"]
    npair, ngrp = nh // 2, nkv // 2
    qmap, kmap, kvq = cfg["qmap"], cfg["kmap"], cfg["kv_of_q"]
    dense = cfg["dense"]
    sinkf = cfg["sink"]
    branches = cfg["branches"]
    npass = sum(len(b) for (_, _, wins) in branches for (_, b) in wins)
    nbw = sum(len(wins) for (_, _, wins) in branches)

    def din(name, shape, dt=F32):
        if name in io:
            return io[name]
        return nc.dram_tensor(name, list(shape), dt, kind="ExternalInput").ap()

    xa = din("xa", [n_ext, D])
    adds = [din(f"pa{i}", [n_ext, D]) for i in range(n_add)]
    gn = din("gn", [1, D])
    wqb = din("wqb", [D, npair * 128])
    wkb = din("wkb", [D, ngrp * 128])
    wvb = din("wvb", [D, nkv * 64])
    wo = din("wo", [D, D])
    gqb = din("gqb", [128, 1])
    gkb = din("gkb", [128, 1])
    gq1 = din("gq1", [1, 64])
    gk1 = din("gk1", [1, 64])
    tblT = din("tblT", [32, nh])
    tblB = din("tblB", [1, nh * 32])
    sink1 = din("sink1", [1, nh])
    cOH = din("cOH", [npass, 32, 128])
    cZ = din("cZ", [128, 512])
    cG = din("cG", [128, 128])
    valid2 = din("valid2", [128, n_ext // 128])
    if dense:
        xk = din("xk", [NTOK, D])
        wqd = din("wqd", [D, 512])
        wkd = din("wkd", [D, 128])
        wvd = din("wvd", [D, 128])
        gqd = din("gqd", [128, 1])
        gkd = din("gkd", [128, 1])
        gqd1 = din("gqd1", [1, 64])
        gkd1 = din("gkd1", [1, 64])
        cosk = din("cosk", [128, NTOK])
        sink_ = din("sink_", [128, NTOK])
        cosq = din("cosq", [128, n_own])
        sinq = din("sinq", [128, n_own])
        cR = din("cR", [128, 128])
    VW = nkv * 65
    OW = nh * 65
    if ctx is None:
        out = nc.dram_tensor("out", [n_own, D], F32, kind="ExternalOutput").ap()
        vbuf = nc.dram_tensor("vbuf", [n_ext, VW], BF16).ap()
        obuf = [nc.dram_tensor(f"obuf{i}", [n_own, OW], F32).ap() for i in range(len(branches))]
        xsum = nc.dram_tensor("xsum", [n_ext, D], F32).ap() if n_add else xa
        S = Sched(nc)
        A = Arena(nc, "arena", 204 * 1024)
        psall = nc.alloc_psum_tensor("psall", [128, 4096], F32)
    else:
        out = io["out"]
        tg = cfg["name"]
        vbuf = ctx.scratch(tg + "vbuf", [n_ext, VW], BF16)
        obuf = [ctx.scratch(tg + f"obuf{i}", [n_own, OW], F32) for i in range(len(branches))]
        xsum = ctx.scratch(tg + "xsum", [n_ext, D], F32) if n_add else xa
        S, A, psall = ctx.S, ctx.A, ctx.psall
    ps = [psall[:, i * 512:(i + 1) * 512] for i in range(8)]
    psb = [p.bitcast(BF16) for p in ps]
    PB = [Buf(f"ps{i}") for i in range(8)]

    identf, identb, Bif, Bib = make_ident(S, A)
    Bc = Buf("consts")

    def cload(shape, src, dt=F32, eng="sp", **kw):
        t = A.alloc(shape, dt)
        S.add(eng, DMA(t, src, **kw), pwrites=[Bc], dma=True)
        return t

    def wload(src, cols):
        t = A.alloc([8, cols], BF16)
        v = src.rearrange("(c p) f -> p c f", p=128)
        for c in range(8):
            S.add("pool", DMA(t[:, c, :], v[:, c, :]), pwrites=[Bc], dma=True)
        return t

    gcol = cload([8], gn.rearrange("o (c p) -> p (o c)", p=128), allow_slow_non_contiguous=True)
    G_bf = cload([128], cG, BF16, "pool")
    Z_bf = cload([512], cZ, BF16, "pool")
    wo_bf = wload(wo, D)
    val_sb = cload([n_ext // 128], valid2)
    gq_c = cload([1], gqb)
    gk_c = cload([1], gkb)
    gqB = cload([64], gq1.partition_broadcast(128))
    gkB = cload([64], gk1.partition_broadcast(128))
    tbB = cload([nh, 32], tblB.partition_broadcast(128))
    skB = cload([nh], sink1.partition_broadcast(128))
    tT = A.alloc([nh], F32)
    S.add("sp", DMA(tT[0:32, :], tblT), pwrites=[Bc], dma=True)
    cst = A.alloc([16], F32)
    MbB = A.alloc([nh], F32)
    SHB = A.alloc([nh], F32)
    skE = A.alloc([nh], F32)
    Bk = Buf("cst")

    def mk_mqk(gA, gB, o):
        S.add("dve", lambda e: e.reduce_max(out=cst[:, o:o + 1], in_=gA, axis=AX.X, apply_absolute_value=True),
              reads=[Bc], pwrites=[Bk])
        S.add("dve", lambda e: e.reduce_max(out=cst[:, o + 1:o + 2], in_=gB, axis=AX.X, apply_absolute_value=True),
              reads=[Bc], pwrites=[Bk])
        S.add("dve", lambda e: e.tensor_tensor(out=cst[:, o + 2:o + 3], in0=cst[:, o:o + 1], in1=cst[:, o + 1:o + 2],
                                               op=ALU.mult), reads=[Bk], pwrites=[Bk])
        S.add("dve", lambda e: e.tensor_scalar_mul(out=cst[:, o + 2:o + 3], in0=cst[:, o + 2:o + 3], scalar1=8.0),
              reads=[Bk], pwrites=[Bk])
        S.add("dve", lambda e: e.tensor_scalar_mul(out=cst[:, o + 3:o + 4], in0=cst[:, o + 2:o + 3], scalar1=-1.0),
              reads=[Bk], pwrites=[Bk])

    mk_mqk(gqB, gkB, 0)
    BMb, BskE = Buf("Mb"), Buf("skE")
    S.add("dve", lambda e: e.tensor_reduce(out=MbB, in_=tbB, axis=AX.X, op=ALU.max), reads=[Bc], writes=[BMb])
    BSH = Buf("SH")
    if sinkf:
        S.add("dve", lambda e: e.tensor_tensor(out=SHB, in0=skB, in1=MbB, op=ALU.subtract), reads=[Bc, BMb], writes=[BSH])
        S.add("dve", lambda e: e.tensor_scalar_add(out=SHB, in0=SHB, scalar1=cst[:, 3:4]), reads=[BSH, Bk], writes=[BSH])
        S.add("dve", lambda e: e.tensor_scalar_max(out=SHB, in0=SHB, scalar1=0.0), reads=[BSH], writes=[BSH])
        S.add("dve", lambda e: e.tensor_tensor(out=SHB, in0=SHB, in1=MbB, op=ALU.add), reads=[BSH, BMb], writes=[BSH])
        S.add("dve", lambda e: e.tensor_tensor(out=skE, in0=skB, in1=SHB, op=ALU.subtract), reads=[BSH, Bc],
              writes=[BskE])
        S.add("act", lambda e: e.activation(out=skE, in_=skE, func=AF.Exp, bias=cst[:, 3:4], scale=1.0),
              reads=[Bk, BskE], writes=[BskE])
    else:
        S.add("dve", lambda e: e.tensor_copy(out=SHB, in_=MbB), reads=[BMb], writes=[BSH])
    etb = A.alloc([nh], BF16)
    etf = A.alloc([nh], F32)
    Bet = Buf("etb")
    S.add("dve", lambda e: e.tensor_tensor(out=etf[0:32, :], in0=tT[0:32, :], in1=SHB[0:32, :], op=ALU.subtract),
          reads=[Bc, BSH], writes=[Bet])
    S.add("act", lambda e: e.activation(out=etb[0:32, :], in_=etf[0:32, :], func=AF.Exp), reads=[Bet], writes=[Bet])

    A.mark()
    oh_bf = A.alloc([npass, 128], BF16)
    S.add("pool", DMA(oh_bf[0:32], cOH.rearrange("n b r -> b n r")), pwrites=[Bc], dma=True)
    tab = A.alloc([npass, nh], BF16)
    Btab = Buf("tab")
    for pi in range(npass):
        S.add("pe", lambda e, pi=pi: e.matmul(ps[4][:, 0:nh], lhsT=oh_bf[0:32, pi, :], rhs=etb[0:32, :], start=True, stop=True),
              reads=[Bc, Bet], writes=[PB[4]])
        S.add("dve", lambda e, pi=pi: e.tensor_copy(out=tab[:, pi, :], in_=ps[4][:, 0:nh]), reads=[PB[4]], pwrites=[Btab])
    A.release()
    A.mark()
    oh_bf = A.alloc([npass, 128], BF16)
    tab = A.alloc([npass, nh], BF16)
    EB = [A.alloc([nh, 128], BF16) for _ in range(nbw)]
    BEB = [Buf(f"EB{i}") for i in range(nbw)]
    nbk = (128 * nh) // 512
    pi = 0
    bw = 0
    for (d, radius, wins) in branches:
        for (off, bases) in wins:
            for qq in range(128):
                for j, base in enumerate(bases):
                    o0 = qq * nh
                    S.add("pe", lambda e, o0=o0, base=base, qq=qq, pj=pi + j, j=j, nb=len(bases): e.matmul(
                        psall[:, o0:o0 + nh], lhsT=Z_bf[:, base - qq:base - qq + 128], rhs=tab[:, pj, :],
                        start=(j == 0), stop=(j == nb - 1)),
                        reads=[Bc, Btab], writes=[PB[k_] for k_ in range(nbk)] if (qq == 0 and j == 0) else [],
                        pwrites=[] if (qq == 0 and j == 0) else [PB[0]])
            S.add("dve", lambda e, bw=bw: e.tensor_copy(
                out=EB[bw], in_=psall[:, 0:128 * nh].rearrange("p (q h) -> p h q", h=nh)),
                reads=[PB[k_] for k_ in range(nbk)], writes=[BEB[bw]])
            pi += len(bases)
            bw += 1

    if cfg.get("stop") == "eb":
        S.barrier()
        S.emit()
        return nc
    NCH = CH // 128
    wq_bf = wload(wqb, npair * 128)
    wk_bf = wload(wkb, ngrp * 128)
    wv_bf = wload(wvb, nkv * 64)
    Kb = A.alloc([ngrp, n_ext], BF16)
    Qb = A.alloc([npair, n_own], BF16)
    BKb, BQb = Buf("Kb"), Buf("Qb")
    A.mark()
    xts = [A.alloc([D], F32) for _ in range(2)]
    Bxt = [Buf() for _ in range(2)]
    xad = [A.alloc([D], F32) for _ in range(2)]
    Bxa = [Buf() for _ in range(2)]
    xnb = [A.alloc([D], BF16) for _ in range(2)]
    Bxn = [Buf() for _ in range(2)]
    hTs = [A.alloc([8, CH], BF16) for _ in range(2)]
    BhT = [Buf() for _ in range(2)]
    sts = [A.alloc([4], F32) for _ in range(4)]
    Bst = [Buf() for _ in range(4)]
    junk = A.alloc([D], BF16)
    Bjunk = Buf()
    sqs = [A.alloc([CH], BF16) for _ in range(2)]
    Bsq = [Buf() for _ in range(2)]
    sds = [A.alloc([CH], F32) for _ in range(2)]
    Bsd = [Buf() for _ in range(2)]
    vsts = [A.alloc([nkv, 65], BF16) for _ in range(2)]
    Bvs = [Buf() for _ in range(2)]
    ctr = {"t": 0, "n": 0, "v": 0}
    Bvbuf = Buf("vbuf")
    Bxsum = Buf("xsum")

    def make_hT(src, tile0, slot, write_sum=False):
        hT, bh = hTs[slot], BhT[slot]
        for ti in range(NCH):
            T = tile0 + ti
            k = ctr["t"] % 2
            ctr["t"] += 1
            xt, bxt = xts[k], Bxt[k]
            st, bst = sts[ctr["t"] % 4], Bst[ctr["t"] % 4]
            S.add("sp", DMA(xt, src[T * 128:(T + 1) * 128, :]), writes=[bxt], dma=True)
            if write_sum and n_add:
                for ai, ad in enumerate(adds):
                    xa_, bxa = xad[ai % 2], Bxa[ai % 2]
                    S.add("act", DMA(xa_, ad[T * 128:(T + 1) * 128, :]), writes=[bxa], dma=True)
                    S.add("dve", lambda e, xt=xt, xa_=xa_: e.tensor_tensor(out=xt, in0=xt, in1=xa_, op=ALU.add),
                          reads=[bxt, bxa], writes=[bxt])
                S.add("sp", DMA(xsum[T * 128:(T + 1) * 128, :], xt), reads=[bxt], pwrites=[Bxsum], dma=True)
            S.add("act", lambda e, xt=xt, st=st, junk=junk: e.activation(out=junk, in_=xt, func=AF.Square, scale=1.0 / 32,
                                                                         accum_out=st[:, 0:1]),
                  reads=[bxt], writes=[Bjunk, bst])
            S.add("act", lambda e, st=st: e.activation(out=st[:, 1:2], in_=st[:, 0:1], func=AF.Sqrt, bias=EPS, scale=1.0),
                  reads=[bst], pwrites=[bst])
            S.add("dve", lambda e, st=st: e.reciprocal(out=st[:, 2:3], in_=st[:, 1:2]), reads=[bst], pwrites=[bst])
            xn, bxn = xnb[k], Bxn[k]
            S.add("dve", lambda e, xn=xn, xt=xt, st=st: e.tensor_scalar_mul(out=xn, in0=xt, scalar1=st[:, 2:3]),
                  reads=[bxt, bst], writes=[bxn])
            pb = k
            for c in range(8):
                S.add("pe", lambda e, pb=pb, c=c, xn=xn: e.transpose(out=psb[pb][:, c * 128:(c + 1) * 128],
                                                                     in_=xn[:, c * 128:(c + 1) * 128], identity=identb),
                      reads=[bxn, Bib], writes=[PB[pb]] if c == 0 else [], pwrites=[] if c == 0 else [PB[pb]])
            for c in range(8):
                src_ = psb[pb][:, c * 128:(c + 1) * 128]
                dst = hT[:, c, ti * 128:(ti + 1) * 128]
                first = (ti == 0 and c == 0)
                if c % 2 == 0:
                    S.add("act", lambda e, src_=src_, dst=dst, c=c: e.activation(out=dst, in_=src_, func=AF.Copy,
                                                                                 scale=gcol[:, c:c + 1]),
                          reads=[PB[pb], Bc], writes=[bh] if first else [], pwrites=[] if first else [bh])
                else:
                    S.add("dve", lambda e, src_=src_, dst=dst, c=c: e.tensor_scalar_mul(out=dst, in0=src_,
                                                                                        scalar1=gcol[:, c:c + 1]),
                          reads=[PB[pb], Bc], pwrites=[bh])
        return hT, bh

    def proj_fm(hT, bh, w_bf, col0, gcolumn, dst, bdst, rope=None):
        k = ctr["n"] % 2
        ctr["n"] += 1
        pq, pss = 2 + k, 4 + k
        for c in range(8):
            S.add("pe", lambda e, c=c: e.matmul(ps[pq][:, 0:CH], lhsT=w_bf[:, c, col0:col0 + 128], rhs=hT[:, c, :],
                                                start=(c == 0), stop=(c == 7)),
                  reads=[bh, Bc], writes=[PB[pq]] if c == 0 else [], pwrites=[] if c == 0 else [PB[pq]])
        sq, bsq, sd, bsd = sqs[k], Bsq[k], sds[k], Bsd[k]
        S.add("act", lambda e: e.activation(out=sq, in_=ps[pq][:, 0:CH], func=AF.Square), reads=[PB[pq]], writes=[bsq])
        S.add("pe", lambda e: e.matmul(ps[pss][:, 0:CH], lhsT=G_bf, rhs=sq, start=True, stop=True),
              reads=[bsq, Bc], writes=[PB[pss]])
        S.add("act", lambda e: e.activation(out=sd, in_=ps[pss][:, 0:CH], func=AF.Sqrt, bias=EPS, scale=1.0 / 64),
              reads=[PB[pss]], writes=[bsd])
        S.add("dve", lambda e: e.reciprocal(out=sd, in_=sd), reads=[bsd], writes=[bsd])
        if rope is None:
            S.add("dve", lambda e: e.scalar_tensor_tensor(out=dst, in0=ps[pq][:, 0:CH], scalar=gcolumn, in1=sd,
                                                          op0=ALU.mult, op1=ALU.mult),
                  reads=[PB[pq], bsd, Bc], pwrites=[bdst])
        else:
            cos_d, sin_d, c0 = rope
            qn, bqn = rp["qn"][k], rp["Bqn"][k]
            cs, bcs, sn, bsn = rp["cs"][k], rp["Bcs"][k], rp["sn"][k], rp["Bsn"][k]
            S.add("sp", DMA(cs, cos_d[:, c0:c0 + CH]), writes=[bcs], dma=True)
            S.add("sp", DMA(sn, sin_d[:, c0:c0 + CH]), writes=[bsn], dma=True)
            S.add("dve", lambda e: e.scalar_tensor_tensor(out=qn, in0=ps[pq][:, 0:CH], scalar=gcolumn, in1=sd,
                                                          op0=ALU.mult, op1=ALU.mult),
                  reads=[PB[pq], bsd, Bc], writes=[bqn])
            S.add("pe", lambda e: e.matmul(ps[6][:, 0:CH], lhsT=rp["R"], rhs=qn, start=True, stop=True),
                  reads=[bqn, Bc], writes=[PB[6]])
            S.add("dve", lambda e: e.tensor_tensor(out=cs, in0=qn, in1=cs, op=ALU.mult), reads=[bqn, bcs], writes=[bcs])
            S.add("dve", lambda e: e.tensor_tensor(out=sn, in0=ps[6][:, 0:CH], in1=sn, op=ALU.mult),
                  reads=[PB[6], bsn], writes=[bsn])
            S.add("dve", lambda e: e.tensor_tensor(out=dst, in0=cs, in1=sn, op=ALU.add), reads=[bcs, bsn], pwrites=[bdst])

    def proj_v(hT, bh, ti, w_bf, ncols, dst_fn):
        for c in range(8):
            S.add("pe", lambda e, c=c: e.matmul(ps[7][:, 0:ncols], lhsT=hT[:, c, ti * 128:(ti + 1) * 128],
                                                rhs=w_bf[:, c, 0:ncols], start=(c == 0), stop=(c == 7)),
                  reads=[bh, Bc], writes=[PB[7]] if c == 0 else [], pwrites=[] if c == 0 else [PB[7]])
        dst_fn(ps[7][:, 0:ncols])

    own_lo, own_hi = own_off, own_off + n_own
    for ch in range(n_ext // CH):
        hT, bh = make_hT(xa, ch * NCH, ch % 2, write_sum=True)
        for g in range(ngrp):
            proj_fm(hT, bh, wk_bf, g * 128, gk_c[:, 0:1], Kb[:, g, ch * CH:(ch + 1) * CH], BKb)
        for ti in range(NCH):
            T = ch * NCH + ti
            k = ctr["v"] % 2
            ctr["v"] += 1
            vs, bvs = vsts[k], Bvs[k]

            def put(psv, vs=vs, bvs=bvs, T=T):
                S.add("act", lambda e: e.copy(out=vs[:, :, 0:64], in_=psv.rearrange("p (h d) -> p h d", h=nkv)),
                      reads=[PB[7]], writes=[bvs])
                S.add("dve", lambda e: e.tensor_copy(out=vs[:, :, 64],
                                                     in_=val_sb[:, T:T + 1].to_broadcast([128, nkv])),
                      reads=[Bc], pwrites=[bvs])
                S.add("act", DMA(vbuf[T * 128:(T + 1) * 128, :], vs.rearrange("p h d -> p (h d)")), reads=[bvs],
                      pwrites=[Bvbuf], dma=True)
            proj_v(hT, bh, ti, wv_bf, nkv * 64, put)
        if own_lo <= ch * CH < own_hi:
            for p_ in range(npair):
                proj_fm(hT, bh, wq_bf, p_ * 128, gq_c[:, 0:1],
                        Qb[:, p_, ch * CH - own_lo:(ch + 1) * CH - own_lo], BQb)
    S.barrier()
    A.release()
    if cfg.get("stop") == "pass1":
        S.barrier()
        S.emit()
        return nc

    A.mark()
    NW = 6
    vws = [A.alloc([nkv, 65], BF16) for _ in range(NW)]
    Bvw = [Buf() for _ in range(NW)]
    pts = [A.alloc([512], BF16) for _ in range(3)]
    Bpt = [Buf() for _ in range(3)]
    PTs = [A.alloc([512], BF16) for _ in range(3)]
    BPT = [Buf() for _ in range(3)]
    osts = [A.alloc([nh, 65], F32) for _ in range(2)]
    Bos = [Buf() for _ in range(2)]
    Bob = [Buf(f"obuf{i}") for i in range(len(branches))]
    ngr = nh // 4
    cw = {"w": 0, "p": 0, "s": 0, "o": 0, "t": 0}
    bw0 = 0
    units = []
    for bi, (d, radius, wins) in enumerate(branches):
        nt = n_own // d // 128
        for r in range(d):
            for i in range(nt):
                if cfg.get("band_limit") is not None and cw["t"] >= cfg["band_limit"]:
                    break
                cw["t"] += 1
                tile = dict(bi=bi, d=d, wins=wins, s0=own_off // d + 128 * i, q0=r + d * 128 * i, r=r, bw0=bw0,
                            ko=cw["t"] % 2)
                for g in range(ngr):
                    for wi in range(len(wins)):
                        units.append((tile, g, wi))
        bw0 += len(wins)
    LAG = 2
    ust = {}
    for step in range(len(units) + LAG):
        if step < len(units):
            tile, g, wi = units[step]
            d, wins, q0 = tile["d"], tile["wins"], tile["q0"]
            if g == 0 and wi == 0:
                vt = []
                for (off, bases) in wins:
                    kw_ = cw["w"] % NW
                    cw["w"] += 1
                    u0 = tile["r"] + d * (tile["s0"] + off)
                    S.add("sp", DMA(vws[kw_].rearrange("p h d -> p (h d)"), vbuf[u0:u0 + d * 127 + 1:d, :]),
                          reads=[Bvbuf], writes=[Bvw[kw_]], dma=True)
                    vt.append((kw_, u0))
                tile["vt"] = vt
            if wi == 0:
                tile[("ob", g)] = 4 + cw["o"] % 4
                cw["o"] += 1
            kw_, u0 = tile["vt"][wi]
            sb = cw["s"] % 4
            cw["s"] += 1
            for j in range(4):
                hq = g * 4 + j
                pr, hf = qmap[hq]
                kg, khf = kmap[kvq[hq]]
                assert hf == khf
                S.add("pe", lambda e, sb=sb, j=j, hf=hf, kg=kg, pr=pr, u0=u0, q0=q0, d=d: e.matmul(
                    ps[sb][:, j * 128:(j + 1) * 128],
                    lhsT=Kb[hf * 64:(hf + 1) * 64, kg, u0:u0 + d * 127 + 1:d],
                    rhs=Qb[hf * 64:(hf + 1) * 64, pr, q0:q0 + d * 127 + 1:d], start=True, stop=True),
                    reads=[BKb, BQb], writes=[PB[sb]] if j == 0 else [], pwrites=[] if j == 0 else [PB[sb]])
            kp = cw["p"] % 3
            cw["p"] += 1
            S.add("act", lambda e, kp=kp, sb=sb: e.activation(out=pts[kp], in_=ps[sb][:, 0:512], func=AF.Exp,
                                                              bias=cst[:, 3:4], scale=0.125),
                  reads=[PB[sb], Bk], writes=[Bpt[kp]])
            ebv = EB[tile["bw0"] + wi][:, g * 4:(g + 1) * 4, :].rearrange("p h q -> p (h q)")
            S.add("dve", lambda e, kp=kp, ebv=ebv: e.tensor_tensor(out=PTs[kp], in0=pts[kp], in1=ebv, op=ALU.mult),
                  reads=[Bpt[kp], BEB[tile["bw0"] + wi]], writes=[BPT[kp]])
            ust[step] = kp
        if step >= LAG:
            tile, g, wi = units[step - LAG]
            kp = ust.pop(step - LAG)
            d, wins, q0 = tile["d"], tile["wins"], tile["q0"]
            kw_, u0 = tile["vt"][wi]
            ob = tile[("ob", g)]
            ost, bos = osts[tile["ko"]], Bos[tile["ko"]]
            for j in range(4):
                hq = g * 4 + j
                S.add("pe", lambda e, ob=ob, j=j, kp=kp, kw_=kw_, kv=kvq[hq], wi=wi, nw=len(wins): e.matmul(
                    ps[ob][:, j * 65:(j + 1) * 65], lhsT=PTs[kp][:, j * 128:(j + 1) * 128],
                    rhs=vws[kw_][:, kv, :], start=(wi == 0 and j == 0), stop=(wi == nw - 1)),
                    reads=[BPT[kp], Bvw[kw_]],
                    writes=[PB[ob]] if (wi == 0 and j == 0) else [],
                    pwrites=[] if (wi == 0 and j == 0) else [PB[ob]])
            if wi == len(wins) - 1:
                S.add("act", lambda e, ob=ob, g=g, ost=ost: e.copy(
                    out=ost[:, g * 4:(g + 1) * 4, :].rearrange("p h d -> p (h d)"), in_=ps[ob][:, 0:260]),
                    reads=[PB[ob]], writes=[bos] if g == 0 else [], pwrites=[] if g == 0 else [bos])
                if g == ngr - 1:
                    S.add("act", DMA(obuf[tile["bi"]][q0:q0 + d * 127 + 1:d, :], ost.rearrange("p h d -> p (h d)")),
                          reads=[bos], pwrites=[Bob[tile["bi"]]], dma=True)
    S.barrier()
    A.release()
    A.release()

    if cfg.get("stop") == "band":
        S.barrier()
        S.emit()
        return nc
    OA = A.alloc([4, D], BF16)
    BOA = Buf("OA")
    if dense:
        wqd_bf = wload(wqd, 512)
        wkd_bf = wload(wkd, 128)
        wvd_bf = wload(wvd, 128)
        gqd_c = cload([1], gqd)
        gkd_c = cload([1], gkd)
        gqdB = cload([64], gqd1.partition_broadcast(128))
        gkdB = cload([64], gkd1.partition_broadcast(128))
        R_bf = cload([128], cR, BF16, "pool")
        mk_mqk(gqdB, gkdB, 4)
        Kd = A.alloc([NTOK], BF16)
        Qd = A.alloc([4, n_own], BF16)
        V1d = A.alloc([NTOK // 128, 2, 65], BF16)
        BKd, BQd, BVd = Buf("Kd"), Buf("Qd"), Buf("V1d")
        S.add("dve", lambda e: e.memset(V1d[:, :, :, 64], 1.0), pwrites=[BVd])
        A.mark()
        xts[:] = [A.alloc([D], F32) for _ in range(2)]
        xnb[:] = [A.alloc([D], BF16) for _ in range(2)]
        hTs[:] = [A.alloc([8, CH], BF16) for _ in range(2)]
        sts[:] = [A.alloc([4], F32) for _ in range(4)]
        junk = A.alloc([D], BF16)
        sqs[:] = [A.alloc([CH], BF16) for _ in range(2)]
        sds[:] = [A.alloc([CH], F32) for _ in range(2)]
        rp = {"qn": [A.alloc([CH], BF16) for _ in range(2)], "Bqn": [Buf(), Buf()],
              "cs": [A.alloc([CH], F32) for _ in range(2)], "Bcs": [Buf(), Buf()],
              "sn": [A.alloc([CH], F32) for _ in range(2)], "Bsn": [Buf(), Buf()], "R": R_bf}
        saved_nadd = n_add
        for ch in range(n_own // CH):
            hT, bh = make_hT(xsum, (own_off + ch * CH) // 128, ch % 2)
            for p_ in range(4):
                proj_fm(hT, bh, wqd_bf, p_ * 128, gqd_c[:, 0:1], Qd[:, p_, ch * CH:(ch + 1) * CH], BQd,
                        rope=(cosq, sinq, ch * CH))
        for ch in range(NTOK // CH):
            hT, bh = make_hT(xk, ch * NCH, ch % 2)
            proj_fm(hT, bh, wkd_bf, 0, gkd_c[:, 0:1], Kd[:, ch * CH:(ch + 1) * CH], BKd, rope=(cosk, sink_, ch * CH))
            for ti in range(NCH):
                T = ch * NCH + ti

                def putd(psv, T=T):
                    S.add("act", lambda e: e.copy(out=V1d[:, T, :, 0:64], in_=psv.rearrange("p (h d) -> p h d", h=2)),
                          reads=[PB[7]], pwrites=[BVd])
                proj_v(hT, bh, ti, wvd_bf, 128, putd)
        S.barrier()
        A.release()

    if cfg.get("stop") == "densep":
        S.barrier()
        S.emit()
        return nc
    A.mark()
    pts2 = [A.alloc([512], BF16) for _ in range(4)]
    Bpt2 = [Buf() for _ in range(4)]
    obl = [[A.alloc([nh, 65], F32) for _ in range(len(branches))] for _ in range(2)]
    Bol = [[Buf() for _ in range(len(branches))] for _ in range(2)]
    rcs = [A.alloc([16], F32) for _ in range(2)]
    Brc = [Buf() for _ in range(2)]
    OT = A.alloc([8, 512], BF16)
    BOT = Buf("OT")
    xrs = [A.alloc([D], F32) for _ in range(2)]
    Bxr = [Buf() for _ in range(2)]
    cd = {"s": 0, "p": 0, "o": 0, "l": 0, "x": 0}
    band_col0 = cfg["band_col0"]
    for chq in range(n_own // 512):
        first_oa = [True]

        def oa_w():
            if first_oa[0]:
                first_oa[0] = False
                return dict(writes=[BOA])
            return dict(pwrites=[BOA])
        if dense:
            NK = NTOK // 128
            units = [(hq, kt) for hq in range(8) for kt in range(NK)]
            LAG = 2
            ust = {}
            obh = {}
            for step in range(len(units) + LAG):
                if step < len(units):
                    hq, kt = units[step]
                    pr, hf = hq % 4, hq // 4
                    if kt == 0:
                        obh[hq] = 4 + cd["o"] % 2
                        cd["o"] += 1
                    sb = cd["s"] % 4
                    cd["s"] += 1
                    S.add("pe", lambda e, sb=sb, hf=hf, pr=pr, kt=kt, chq=chq: e.matmul(
                        ps[sb][:, 0:512], lhsT=Kd[hf * 64:(hf + 1) * 64, kt * 128:(kt + 1) * 128],
                        rhs=Qd[hf * 64:(hf + 1) * 64, pr, chq * 512:(chq + 1) * 512], start=True, stop=True),
                        reads=[BKd, BQd], writes=[PB[sb]])
                    kp = cd["p"] % 4
                    cd["p"] += 1
                    S.add("act", lambda e, kp=kp, sb=sb: e.activation(out=pts2[kp], in_=ps[sb][:, 0:512], func=AF.Exp,
                                                                      bias=cst[:, 7:8], scale=0.125),
                          reads=[PB[sb], Bk], writes=[Bpt2[kp]])
                    ust[step] = kp
                if step >= LAG:
                    hq, kt = units[step - LAG]
                    pr, hf = hq % 4, hq // 4
                    kp = ust.pop(step - LAG)
                    ob = obh[hq]
                    for sub in range(4):
                        S.add("pe", lambda e, ob=ob, sub=sub, kp=kp, kt=kt, hf=hf: e.matmul(
                            ps[ob][:, sub * 65:(sub + 1) * 65], lhsT=pts2[kp][:, sub * 128:(sub + 1) * 128],
                            rhs=V1d[:, kt, hf, :], start=(kt == 0 and sub == 0), stop=(kt == NK - 1)),
                            reads=[Bpt2[kp], BVd],
                            writes=[PB[ob]] if (kt == 0 and sub == 0) else [],
                            pwrites=[] if (kt == 0 and sub == 0) else [PB[ob]])
                    if kt == NK - 1:
                        kr = cd["l"] % 2
                        cd["l"] += 1
                        ov = ps[ob][:, 0:260].rearrange("p (s d) -> p s d", s=4)
                        S.add("dve", lambda e, kr=kr, ov=ov: e.reciprocal(out=rcs[kr][:, 0:4], in_=ov[:, :, 64]),
                              reads=[PB[ob]], writes=[Brc[kr]])
                        col = cfg["dense_col0"] + hq * 64
                        S.add("dve", lambda e, kr=kr, ov=ov, col=col: e.tensor_tensor(
                            out=OA[:, :, col:col + 64], in0=ov[:, :, 0:64],
                            in1=rcs[kr][:, 0:4].unsqueeze(2).to_broadcast([128, 4, 64]), op=ALU.mult),
                            reads=[PB[ob], Brc[kr]], **oa_w())
        for sub in range(4):
            t = chq * 4 + sub
            ko = cd["x"] % 2
            cd["x"] += 1
            for bi in range(len(branches)):
                S.add("sp", DMA(obl[ko][bi].rearrange("p h d -> p (h d)"), obuf[bi][t * 128:(t + 1) * 128, :]),
                      reads=[Bob[bi]], writes=[Bol[ko][bi]], dma=True)
            o0, b0_ = obl[ko][0], Bol[ko][0]
            for bi in range(1, len(branches)):
                S.add("dve", lambda e, o0=o0, o1=obl[ko][bi]: e.tensor_tensor(out=o0, in0=o0, in1=o1, op=ALU.add),
                      reads=[b0_, Bol[ko][bi]], writes=[b0_])
            if sinkf:
                S.add("dve", lambda e, o0=o0: e.tensor_tensor(out=o0[:, :, 64], in0=o0[:, :, 64], in1=skE, op=ALU.add),
                      reads=[b0_, BSH, BskE], writes=[b0_])
            kr = cd["l"] % 2
            cd["l"] += 1
            S.add("dve", lambda e, kr=kr, o0=o0: e.reciprocal(out=rcs[kr][:, 0:nh], in_=o0[:, :, 64]),
                  reads=[b0_], writes=[Brc[kr]])
            S.add("dve", lambda e, kr=kr, o0=o0, sub=sub: e.tensor_tensor(
                out=OA[:, sub, band_col0:band_col0 + nh * 64].rearrange("p (h d) -> p h d", h=nh), in0=o0[:, :, 0:64],
                in1=rcs[kr][:, 0:nh].unsqueeze(2).to_broadcast([128, nh, 64]), op=ALU.mult),
                reads=[b0_, Brc[kr]], **oa_w())
        for c8 in range(8):
            pb = 6 + c8 % 2
            for sub in range(4):
                S.add("pe", lambda e, pb=pb, sub=sub, c8=c8: e.transpose(
                    out=psb[pb][:, sub * 128:(sub + 1) * 128], in_=OA[:, sub, c8 * 128:(c8 + 1) * 128], identity=identb),
                    reads=[BOA, Bib], writes=[PB[pb]] if sub == 0 else [], pwrites=[] if sub == 0 else [PB[pb]])
            if c8 % 2 == 0:
                S.add("act", lambda e, pb=pb, c8=c8: e.copy(out=OT[:, c8, :], in_=psb[pb][:, 0:512]),
                      reads=[PB[pb]], writes=[BOT] if c8 == 0 else [], pwrites=[] if c8 == 0 else [BOT])
            else:
                S.add("dve", lambda e, pb=pb, c8=c8: e.tensor_copy(out=OT[:, c8, :], in_=psb[pb][:, 0:512]),
                      reads=[PB[pb]], pwrites=[BOT])
        for sub in range(4):
            t = chq * 4 + sub
            kx = (chq * 4 + sub) % 2
            xr, bxr = xrs[kx], Bxr[kx]
            S.add("sp", DMA(xr, xsum[own_off + t * 128:own_off + (t + 1) * 128, :]), reads=[Bxsum], writes=[bxr], dma=True)
            for hf in range(2):
                pb = (sub * 2 + hf) % 4
                for c8 in range(8):
                    S.add("pe", lambda e, pb=pb, c8=c8, sub=sub, hf=hf: e.matmul(
                        ps[pb][:, 0:512], lhsT=OT[:, c8, sub * 128:(sub + 1) * 128],
                        rhs=wo_bf[:, c8, hf * 512:(hf + 1) * 512], start=(c8 == 0), stop=(c8 == 7)),
                        reads=[BOT, Bc], writes=[PB[pb]] if c8 == 0 else [], pwrites=[] if c8 == 0 else [PB[pb]])
                S.add("dve", lambda e, pb=pb, xr=xr, hf=hf: e.tensor_tensor(
                    out=xr[:, hf * 512:(hf + 1) * 512], in0=ps[pb][:, 0:512], in1=xr[:, hf * 512:(hf + 1) * 512],
                    op=ALU.add), reads=[PB[pb], bxr], writes=[bxr])
            S.add("sp", DMA(out[t * 128:(t + 1) * 128, :], xr), reads=[bxr], dma=True)
    S.barrier()
    if ctx is not None:
        return None
    S.emit()
    return nc


def build_add3(n_rows):
    nc = bass.Bass("TRN2", target_bir_lowering=False)
    srcs = [nc.dram_tensor(n, [n_rows, D], F32, kind="ExternalInput").ap() for n in ("xa", "p0", "p1")]
    out = nc.dram_tensor("out", [n_rows, D], F32, kind="ExternalOutput").ap()
    S = Sched(nc)
    A = Arena(nc, "arena", 64 * 1024)
    bufs = [[A.alloc([D], F32) for _ in range(3)] for _ in range(3)]
    Bb = [[Buf() for _ in range(3)] for _ in range(3)]
    for T in range(n_rows // 128):
        k = T % 3
        for i, eng in enumerate(("sp", "act", "sp")):
            S.add(eng, DMA(bufs[k][i], srcs[i][T * 128:(T + 1) * 128, :]), writes=[Bb[k][i]], dma=True)
        S.add("dve", lambda e, k=k: e.tensor_tensor(out=bufs[k][0], in0=bufs[k][0], in1=bufs[k][1], op=ALU.add),
              reads=[Bb[k][0], Bb[k][1]], writes=[Bb[k][0]])
        S.add("dve", lambda e, k=k: e.tensor_tensor(out=bufs[k][0], in0=bufs[k][0], in1=bufs[k][2], op=ALU.add),
              reads=[Bb[k][0], Bb[k][2]], writes=[Bb[k][0]])
        S.add("act", DMA(out[T * 128:(T + 1) * 128, :], bufs[k][0]), reads=[Bb[k][0]], dma=True)
    S.barrier()
    S.emit()
    return nc


A_WINS = [(-64, [128]), (64, [255])]
CFG_L0 = dict(name="l0", n_ext=6144, own_off=1024, n_own=4096, CH=512, n_add=0, nh=8, nkv=8,
              qmap=[(h % 4, h // 4) for h in range(8)], kmap=[(h % 4, h // 4) for h in range(8)],
              kv_of_q=list(range(8)),
              branches=[(1, 64, A_WINS), (4, 64, A_WINS), (16, 64, A_WINS)],
              sink=False, dense=True, band_col0=0, dense_col0=512)
CFG_L1 = dict(name="l1", n_ext=4352, own_off=128, n_own=4096, CH=128, n_add=2, nh=16, nkv=4,
              qmap=[(((hq // 4) // 2) * 4 + hq % 4, (hq // 4) % 2) for hq in range(16)],
              kmap=[(i // 2, i % 2) for i in range(4)],
              kv_of_q=[hq // 4 for hq in range(16)],
              branches=[(1, 128, [(-128, [128]), (0, [255, 127]), (128, [255])])],
              sink=True, dense=None, band_col0=0, dense_col0=0)


def _ext_rows(a, t0, own_off, n_ext):
    lo = t0 - own_off
    out = np.zeros((n_ext, a.shape[1]), a.dtype)
    s, e = max(lo, 0), min(lo + n_ext, a.shape[0])
    out[s - lo:e - lo] = a[s:e]
    return out


def _valid2(t0, own_off, n_ext):
    u = np.arange(n_ext) + t0 - own_off
    v = ((u >= 0) & (u < NTOK)).astype(np.float32)
    return np.ascontiguousarray(v.reshape(n_ext // 128, 128).T)


def attn_inputs(cfg, consts, half, xa_full, adds_full, gn, wq, wk, wv, wo, gq, gk, tbl, sink, dense_in=None):
    t0 = half * 4096
    n_ext, own_off = cfg["n_ext"], cfg["own_off"]
    nh = cfg["nh"]
    npair = nh // 2
    wqb = np.zeros((D, npair * 128), np.float32)
    for hq, (p_, s_) in enumerate(cfg["qmap"]):
        wqb[:, p_ * 128 + s_ * 64:p_ * 128 + s_ * 64 + 64] = wq[:, hq * 64:(hq + 1) * 64]
    wkb = np.zeros((D, (cfg["nkv"] // 2) * 128), np.float32)
    for kv, (g_, s_) in enumerate(cfg["kmap"]):
        wkb[:, g_ * 128 + s_ * 64:g_ * 128 + s_ * 64 + 64] = wk[:, kv * 64:(kv + 1) * 64]
    ohb, Z, G = consts
    m = dict(xa=_ext_rows(xa_full, t0, own_off, n_ext), gn=gn[None], wqb=wqb, wkb=wkb,
             wvb=np.ascontiguousarray(wv), wo=np.ascontiguousarray(wo),
             gqb=np.tile(gq, 2)[:, None].copy(), gkb=np.tile(gk, 2)[:, None].copy(), gq1=gq[None].copy(), gk1=gk[None].copy(),
             tblT=np.ascontiguousarray(tbl[:nh].T), tblB=np.ascontiguousarray(tbl[:nh].reshape(1, nh * 32)),
             sink1=(sink[None].copy() if sink is not None else np.zeros((1, nh), np.float32)),
             cOH=ohb, cZ=Z, cG=G, valid2=_valid2(t0, own_off, n_ext))
    for i, a in enumerate(adds_full):
        m[f"pa{i}"] = _ext_rows(a, t0, own_off, n_ext)
    if dense_in is not None:
        xk, wqd_, wkd_, wvd_, gqd_, gkd_, (cos, sin, Rl) = dense_in
        wqd = np.zeros((D, 512), np.float32)
        for hq in range(8):
            r_, i_ = hq % 4, hq // 4
            wqd[:, (r_ * 2 + i_) * 64:(r_ * 2 + i_) * 64 + 64] = wqd_[:, hq * 64:(hq + 1) * 64]
        m.update(xk=xk, wqd=wqd, wkd=np.ascontiguousarray(wkd_), wvd=np.ascontiguousarray(wvd_),
                 gqd=np.tile(gqd_, 2)[:, None].copy(), gkd=np.tile(gkd_, 2)[:, None].copy(),
                 gqd1=gqd_[None].copy(), gkd1=gkd_[None].copy(), cosk=cos, sink_=sin,
                 cosq=np.ascontiguousarray(cos[:, t0:t0 + 4096]), sinq=np.ascontiguousarray(sin[:, t0:t0 + 4096]), cR=Rl)
    return m


def moe_inputs(half, x_full, gn, router, wg, wu, wd):
    p = np.arange(128)
    cG = (p[:, None] // 16 == p[None, :] // 16).astype(np.float32)
    cL = ((p[:, None] // 16 == p[None, :] // 16) & (p[:, None] < p[None, :])).astype(np.float32)
    perm = np.concatenate([np.arange(8 * half, 8 * half + 8), np.arange(8 * (1 - half), 8 * (1 - half) + 8)])
    return dict(x=x_full, gn=gn[None], wr=np.ascontiguousarray(router[:, perm]),
                wg=np.ascontiguousarray(wg[8 * half:8 * half + 8]), wu=np.ascontiguousarray(wu[8 * half:8 * half + 8]),
                wd=np.ascontiguousarray(wd[8 * half:8 * half + 8]), cG=cG, cL=cL)


CFG_L1F = dict(CFG_L1, n_add=1)


def fused_host_inputs(inp, b):
    x = inp["x"][b]
    m = {}
    xpad = np.zeros((NTOK + 2048, D), np.float32)
    xpad[1024:1024 + NTOK] = x
    m["xpad"] = xpad
    cons0, cons1 = band_consts(CFG_L0), band_consts(CFG_L1)
    cos, sin, Rl = rope_consts()
    w0, w1 = inp["l0_w_in"], inp["l1_w_in"]
    zx = np.zeros((NTOK, D), np.float32)
    for h in range(2):
        a0 = attn_inputs(CFG_L0, cons0, h, zx, [], inp["l0_norm_attn"], w0[:, 0:512], w0[:, 512:1024], w0[:, 1024:1536],
                         inp["l0_w_out"], inp["l0_a_qnorm"], inp["l0_a_knorm"], inp["rel_bias"], None,
                         dense_in=(zx, w0[:, 1536:2048], w0[:, 2048:2176], w0[:, 2176:2304], inp["l0_b_qnorm"],
                                   inp["l0_b_knorm"], (cos, sin, Rl)))
        a1 = attn_inputs(CFG_L1F, cons1, h, zx, [zx], inp["l1_norm_attn"], w1[:, 0:1024], w1[:, 1024:1280],
                         w1[:, 1280:1536], inp["l1_w_out"], inp["l1_c_qnorm"], inp["l1_c_knorm"], inp["rel_bias"],
                         inp["l1_sink"])
        m[f"a0_valid2_{h}"] = a0["valid2"]
        m[f"a1_valid2_{h}"] = a1["valid2"]
        if h == 0:
            for k, v in a0.items():
                if k not in ("xa", "xk", "valid2", "cosq", "sinq"):
                    m["a0_" + k] = v
            for k, v in a1.items():
                if k not in ("xa", "pa0", "valid2"):
                    m["a1_" + k] = v
    p = np.arange(128)
    m["cG16"] = (p[:, None] // 16 == p[None, :] // 16).astype(np.float32)
    m["cL16"] = ((p[:, None] // 16 == p[None, :] // 16) & (p[:, None] < p[None, :])).astype(np.float32)
    for l in range(2):
        m[f"m{l}_gn"] = inp[f"l{l}_norm_ffn"][None]
        m[f"m{l}_wr"] = inp[f"l{l}_router"]
        m[f"m{l}_wg"] = inp[f"l{l}_w_gate"]
        m[f"m{l}_wu"] = inp[f"l{l}_w_up"]
        m[f"m{l}_wd"] = inp[f"l{l}_w_down"]
    return {k: np.ascontiguousarray(v) for k, v in m.items()}


def build_fused(tmpl):
    ctx = Ctx()
    nc, S, A = ctx.nc, ctx.S, ctx.A
    npdt = {np.dtype(np.float32): F32, np.dtype(np.int32): I32}
    E = {k: nc.dram_tensor(k, list(v.shape), npdt[v.dtype], kind="ExternalInput").ap() for k, v in tmpl.items()}
    out = nc.dram_tensor("out", [NTOK, D], F32, kind="ExternalOutput").ap()
    x1p = ctx.scratch("x1p", [NTOK + 256, D], F32)
    p0p = ctx.scratch("p0p", [NTOK + 256, D], F32)
    x3 = ctx.scratch("x3", [NTOK, D], F32)
    p1 = ctx.scratch("p1", [NTOK, D], F32)
    zt = A.alloc([D], F32)
    Bz = Buf()
    S.add("dve", lambda e: e.memset(zt, 0.0), writes=[Bz])
    for t in (x1p, p0p):
        S.add("sp", DMA(t[0:128, :], zt), reads=[Bz], dma=True)
        S.add("sp", DMA(t[128 + NTOK:256 + NTOK, :], zt), reads=[Bz], dma=True)
    S.barrier()
    xpad = E["xpad"]
    a0 = {k[3:]: v for k, v in E.items() if k.startswith("a0_")}
    a1 = {k[3:]: v for k, v in E.items() if k.startswith("a1_")}
    for h in range(2):
        io = dict(a0)
        io.update(xa=xpad[h * 4096:h * 4096 + 6144, :], xk=xpad[1024:1024 + NTOK, :], valid2=a0[f"valid2_{h}"],
                  cosq=a0["cosk"][:, h * 4096:(h + 1) * 4096], sinq=a0["sink_"][:, h * 4096:(h + 1) * 4096],
                  out=x1p[128 + h * 4096:128 + (h + 1) * 4096, :])
        build_attn(CFG_L0, ctx, io)
    build_moe(ctx, dict(x=x1p[128:128 + NTOK, :], gn=E["m0_gn"], wr=E["m0_wr"], wg=E["m0_wg"], wu=E["m0_wu"],
                        wd=E["m0_wd"], cG=E["cG16"], cL=E["cL16"], part=p0p[128:128 + NTOK, :], part_full=p0p,
                        part_eoff=128 * D), zero_init=True, sets=(0, 1))
    for h in range(2):
        io = dict(a1)
        io.update(xa=x1p[h * 4096:h * 4096 + 4352, :], pa0=p0p[h * 4096:h * 4096 + 4352, :],
                  valid2=a1[f"valid2_{h}"], out=x3[h * 4096:(h + 1) * 4096, :])
        build_attn(CFG_L1F, ctx, io)
    build_moe(ctx, dict(x=x3, gn=E["m1_gn"], wr=E["m1_wr"], wg=E["m1_wg"], wu=E["m1_wu"], wd=E["m1_wd"],
                        cG=E["cG16"], cL=E["cL16"], part=p1), zero_init=True, sets=(0, 1))
    ctx.reset()
    bufs = [[A.alloc([D], F32) for _ in range(2)] for _ in range(3)]
    Bb = [[Buf() for _ in range(2)] for _ in range(3)]
    for T in range(NTOK // 128):
        k = T % 3
        S.add("sp", DMA(bufs[k][0], x3[T * 128:(T + 1) * 128, :]), writes=[Bb[k][0]], dma=True)
        S.add("act", DMA(bufs[k][1], p1[T * 128:(T + 1) * 128, :]), writes=[Bb[k][1]], dma=True)
        S.add("dve", lambda e, k=k: e.tensor_tensor(out=bufs[k][0], in0=bufs[k][0], in1=bufs[k][1], op=ALU.add),
              reads=[Bb[k][0], Bb[k][1]], writes=[Bb[k][0]])
        S.add("sp", DMA(out[T * 128:(T + 1) * 128, :], bufs[k][0]), reads=[Bb[k][0]], dma=True)
    S.barrier()
    S.emit()
    return nc


def kernel(**inp):
    inp = {k: np.asarray(v) for k, v in inp.items()}
    nb = inp["x"].shape[0]
    maps = [fused_host_inputs(inp, b) for b in range(nb)]
    nc = build_fused(maps[0])
    res = run_bass_kernel_spmd(nc, maps, core_ids=list(range(nb)))
    return np.stack([res.results[b]["out"] for b in range(nb)]).astype(np.float32)
```
